# Optimizing a Trainium2 kernel written in Bass

```python
import math
import jax, jax.numpy as jnp
from jax import lax
import numpy as np

D_MODEL = 1024
BATCH = 2
SEQ = 8192
DEPTH = 2

EPS = 1e-6
MEM_LEN = 256
CONV_W = 4
N_EVEN = (DEPTH + 1) // 2
N_ODD = DEPTH // 2
RG_WIDTH = D_MODEL
RG_BLOCKS = 8
RG_BW = RG_WIDTH // RG_BLOCKS
RG_C = 8.0
NSA_HEADS = 8
NSA_KV_GROUPS = 2
NSA_REP = NSA_HEADS // NSA_KV_GROUPS
NSA_DK = 128
NSA_DV = NSA_DK
CMP_LEN = 32
CMP_STRIDE = 16
CMP_HID = 256
SLC_LEN = 64
SLC_TOPN = 16
WINDOW = 512
NSA_QBLOCK = 64
FORCE_BONUS = 100.0
EV_IN = 2 * RG_WIDTH + NSA_HEADS * NSA_DK + 6 * NSA_KV_GROUPS * NSA_DK + 3 * NSA_HEADS
EV_MIX = RG_WIDTH + NSA_HEADS * NSA_DV
D_INNER = 2 * D_MODEL
SSM_HEADDIM = 64
SSM_HEADS = D_INNER // SSM_HEADDIM
SSM_GROUPS = 4
SSM_REP = SSM_HEADS // SSM_GROUPS
SSM_STATE = 128
SSD_CHUNK = 128
CONV_CH = D_INNER + 2 * SSM_GROUPS * SSM_STATE
OD_IN = D_INNER + CONV_CH + SSM_HEADS
X_HEADS = 4
X_HEADDIM = 128
X_INNER = X_HEADS * X_HEADDIM
FF_DENSE = 2816
N_EXPERTS = 8
TOP_K = 2
FF_EXPERT = 3584

kernel_name = "hybrid_rglru_nsa_ssd_moe_block"


def rmsnorm(x, g):
    xf = x.astype(jnp.float32)
    y = xf * lax.rsqrt(jnp.mean(xf * xf, axis=-1, keepdims=True) + EPS)
    return (y * g.astype(jnp.float32)).astype(x.dtype)


def masked_softmax(s, mask):
    s = jnp.where(mask, s.astype(jnp.float32), -jnp.inf)
    m = jnp.max(s, axis=-1, keepdims=True)
    m = jnp.where(jnp.isfinite(m), m, 0.0)
    e = jnp.where(mask, jnp.exp(s - m), 0.0)
    return e / jnp.maximum(jnp.sum(e, axis=-1, keepdims=True), 1e-30)


def split_cols(x, sizes):
    return jnp.split(x, np.cumsum(sizes)[:-1].tolist(), axis=-1)


def causal_dwconv(x, w, b):
    k = w.shape[0]
    s = x.shape[1]
    xp = jnp.pad(x, ((0, 0), (k - 1, 0), (0, 0)))
    y = b
    for j in range(k):
        y = y + xp[:, j:j + s] * w[j]
    return y


def rglru(x, wa, ba, wx, bx, lam):
    b_, s_, c_ = x.shape
    xb = x.reshape(b_, s_, RG_BLOCKS, RG_BW)
    r = jax.nn.sigmoid(jnp.einsum("bshi,hij->bshj", xb, wa).reshape(b_, s_, c_) + ba)
    ig = jax.nn.sigmoid(jnp.einsum("bshi,hij->bshj", xb, wx).reshape(b_, s_, c_) + bx)
    log_a = -RG_C * r * jax.nn.softplus(-lam)
    a = jnp.exp(log_a)
    u = jnp.sqrt(-jnp.expm1(2.0 * log_a)) * (ig * x)

    def combine(lhs, rhs):
        a1, b1 = lhs
        a2, b2 = rhs
        return a1 * a2, a2 * b1 + b2

    _, h = lax.associative_scan(combine, (a, u), axis=1)
    return h


def nsa_compress(k, pos, w1, w2):
    b_, s_, g_, d_ = k.shape
    n_cmp = (s_ - CMP_LEN) // CMP_STRIDE + 1
    idx = jnp.arange(n_cmp)[:, None] * CMP_STRIDE + jnp.arange(CMP_LEN)[None, :]
    kb = k[:, idx] + pos[:, None, :]
    kb = kb.transpose(0, 1, 3, 2, 4).reshape(b_, n_cmp, g_, CMP_LEN * d_)
    return jax.nn.gelu(kb @ w1) @ w2


def nsa(q, kc, vc, ks, vs, kw, vw, gates, q_norm, k_norm, cmp_pos, ck_w1, ck_w2, cv_w1, cv_w2):
    b_, s_ = q.shape[:2]
    G, R, DK = NSA_KV_GROUPS, NSA_REP, NSA_DK
    scale = DK ** -0.5
    qr = rmsnorm(q, q_norm).reshape(b_, s_, G, R, DK)
    kcmp = rmsnorm(nsa_compress(kc, cmp_pos, ck_w1, ck_w2), k_norm[0])
    vcmp = nsa_compress(vc, cmp_pos, cv_w1, cv_w2)
    n_cmp = kcmp.shape[1]
    n_slc = s_ // SLC_LEN
    n_sel = min(SLC_TOPN, n_slc)
    ks_blk = rmsnorm(ks, k_norm[1]).reshape(b_, n_slc, SLC_LEN, G, DK).transpose(0, 3, 1, 2, 4)
    vs_blk = vs.reshape(b_, n_slc, SLC_LEN, G, NSA_DV).transpose(0, 3, 1, 2, 4)
    pad = ((0, 0), (WINDOW, 0), (0, 0), (0, 0))
    kw_pad = jnp.pad(rmsnorm(kw, k_norm[2]), pad)
    vw_pad = jnp.pad(vw, pad)
    g = jax.nn.sigmoid(gates).reshape(b_, s_, G, R, 3)
    cmp_start = jnp.arange(n_cmp) * CMP_STRIDE
    cmp_end = cmp_start + CMP_LEN - 1
    slc_start = jnp.arange(n_slc) * SLC_LEN
    overlap = ((cmp_start[:, None] <= slc_start[None, :] + SLC_LEN - 1)
               & (cmp_end[:, None] >= slc_start[None, :])).astype(jnp.float32)
    gather = jax.vmap(jax.vmap(lambda blocks, i: blocks[i]))
    j = jnp.arange(n_slc)

    def query_block(qb):
        t0 = qb * NSA_QBLOCK
        t = t0 + jnp.arange(NSA_QBLOCK)
        qblk = lax.dynamic_slice_in_dim(qr, t0, NSA_QBLOCK, axis=1)
        s = jnp.einsum("bqgrd,bngd->bgrqn", qblk, kcmp) * scale
        p_cmp = masked_softmax(s, cmp_end[None, :] <= t[:, None])
        o_cmp = jnp.einsum("bgrqn,bngd->bqgrd", p_cmp.astype(vcmp.dtype), vcmp)
        imp = jnp.einsum("bgrqn,nj->bgqj", p_cmp, overlap)
        cur = t // SLC_LEN
        forced = (j[None, :] == 0) | (j[None, :] == cur[:, None]) | (j[None, :] == cur[:, None] - 1)
        valid = slc_start[None, :] <= t[:, None]
        score = jnp.where(valid, imp + FORCE_BONUS * forced, -1.0)
        _, sel = lax.top_k(score, n_sel)
        ksel = gather(ks_blk, sel).reshape(b_, G, NSA_QBLOCK, n_sel * SLC_LEN, DK)
        vsel = gather(vs_blk, sel).reshape(b_, G, NSA_QBLOCK, n_sel * SLC_LEN, NSA_DV)
        key_pos = (sel[..., None] * SLC_LEN + jnp.arange(SLC_LEN)).reshape(b_, G, NSA_QBLOCK, n_sel * SLC_LEN)
        s = jnp.einsum("bqgrd,bgqkd->bgrqk", qblk, ksel) * scale
        p = masked_softmax(s, key_pos[:, :, None] <= t[:, None])
        o_slc = jnp.einsum("bgrqk,bgqkd->bqgrd", p.astype(vsel.dtype), vsel)
        kwb = lax.dynamic_slice_in_dim(kw_pad, t0, WINDOW + NSA_QBLOCK, axis=1)
        vwb = lax.dynamic_slice_in_dim(vw_pad, t0, WINDOW + NSA_QBLOCK, axis=1)
        kpos = t0 - WINDOW + jnp.arange(WINDOW + NSA_QBLOCK)
        diff = t[:, None] - kpos[None, :]
        s = jnp.einsum("bqgrd,bkgd->bgrqk", qblk, kwb) * scale
        p = masked_softmax(s, (diff >= 0) & (diff < WINDOW) & (kpos[None, :] >= 0))
        o_win = jnp.einsum("bgrqk,bkgd->bqgrd", p.astype(vwb.dtype), vwb)
        gb = lax.dynamic_slice_in_dim(g, t0, NSA_QBLOCK, axis=1)
        return gb[..., 0:1] * o_cmp + gb[..., 1:2] * o_slc + gb[..., 2:3] * o_win

    out = lax.map(query_block, jnp.arange(s_ // NSA_QBLOCK))
    return out.transpose(1, 0, 2, 3, 4, 5).reshape(b_, s_, NSA_HEADS * NSA_DV)


def even_mixer(h, w_in, conv_w, conv_b, wa, ba, wx, bx, lam, gate_b, q_norm, k_norm,
               cmp_pos, ck_w1, ck_w2, cv_w1, cv_w2, w_out):
    b_, s_, _ = h.shape
    gdk = NSA_KV_GROUPS * NSA_DK
    rg_x, rg_g, q, kc, vc, ks, vs, kw, vw, gates = split_cols(
        h @ w_in, [RG_WIDTH, RG_WIDTH, NSA_HEADS * NSA_DK] + [gdk] * 6 + [3 * NSA_HEADS])
    rg = jax.nn.gelu(rg_g) * rglru(causal_dwconv(rg_x, conv_w, conv_b), wa, ba, wx, bx, lam)

    def kv4(t):
        return t.reshape(b_, s_, NSA_KV_GROUPS, NSA_DK)

    att = nsa(q.reshape(b_, s_, NSA_HEADS, NSA_DK), kv4(kc), kv4(vc), kv4(ks), kv4(vs), kv4(kw), kv4(vw),
              gates + gate_b, q_norm, k_norm, cmp_pos, ck_w1, ck_w2, cv_w1, cv_w2)
    return jnp.concatenate([rg, att], axis=-1) @ w_out


def ssd(x, a, bm, cm):
    b_, s_, g_, r_, p_ = x.shape
    q_ = SSD_CHUNK
    c_ = s_ // q_
    x = x.reshape(b_, c_, q_, g_, r_, p_)
    bm = bm.reshape(b_, c_, q_, g_, -1)
    cm = cm.reshape(b_, c_, q_, g_, -1)
    a = a.reshape(b_, c_, q_, g_, r_).transpose(0, 3, 4, 1, 2)
    a_cs = jnp.cumsum(a, axis=-1)
    causal = jnp.tril(jnp.ones((q_, q_), dtype=bool))
    decay = jnp.exp(jnp.where(causal, a_cs[..., :, None] - a_cs[..., None, :], -jnp.inf))
    y_diag = jnp.einsum("bcqgn,bcsgn,bgrcqs,bcsgrp->bcqgrp", cm, bm, decay, x)
    decay_states = jnp.exp(a_cs[..., -1:] - a_cs)
    states = jnp.einsum("bcsgn,bgrcs,bcsgrp->bcgrpn", bm, decay_states, x)
    chunk_decay = jnp.exp(a_cs[..., -1])

    def step(hc, inp):
        st, dec = inp
        return dec[..., None, None] * hc + st, hc

    _, prev = lax.scan(step, jnp.zeros_like(states[:, 0]),
                       (jnp.moveaxis(states, 1, 0), jnp.moveaxis(chunk_decay, -1, 0)))
    y_off = jnp.einsum("bcqgn,cbgrpn,bgrcq->bcqgrp", cm, prev, jnp.exp(a_cs))
    return (y_diag + y_off).reshape(b_, s_, g_, r_, p_)


def mamba2(h, w_in, conv_w, conv_b, dt_bias, a_log, d_skip, norm_g, w_out):
    b_, s_, _ = h.shape
    gn = SSM_GROUPS * SSM_STATE
    z, xbc, dt = split_cols(h @ w_in, [D_INNER, CONV_CH, SSM_HEADS])
    xbc = jax.nn.silu(causal_dwconv(xbc, conv_w, conv_b))
    xs, bm, cm = split_cols(xbc, [D_INNER, gn, gn])
    xs = xs.reshape(b_, s_, SSM_GROUPS, SSM_REP, SSM_HEADDIM).astype(jnp.float32)
    bm = bm.reshape(b_, s_, SSM_GROUPS, SSM_STATE).astype(jnp.float32)
    cm = cm.reshape(b_, s_, SSM_GROUPS, SSM_STATE).astype(jnp.float32)
    dt = jax.nn.softplus(dt.astype(jnp.float32) + dt_bias.astype(jnp.float32)).reshape(b_, s_, SSM_GROUPS, SSM_REP)
    a = -jnp.exp(a_log.astype(jnp.float32)).reshape(SSM_GROUPS, SSM_REP)
    y = ssd(xs * dt[..., None], dt * a, bm, cm) + d_skip.astype(jnp.float32).reshape(SSM_GROUPS, SSM_REP, 1) * xs
    y = y.reshape(b_, s_, D_INNER).astype(h.dtype) * jax.nn.silu(z)
    y = rmsnorm(y.reshape(b_, s_, SSM_GROUPS, D_INNER // SSM_GROUPS),
                norm_g.reshape(SSM_GROUPS, D_INNER // SSM_GROUPS)).reshape(b_, s_, D_INNER)
    return y @ w_out


def cross_attn(xn, memn, wq, wkv, qn, kn, wo):
    b_, s_, _ = xn.shape
    m_ = memn.shape[1]
    q = rmsnorm((xn @ wq).reshape(b_, s_, X_HEADS, X_HEADDIM), qn)
    k, v = jnp.split(memn @ wkv, 2, axis=-1)
    k = rmsnorm(k.reshape(b_, m_, X_HEADS, X_HEADDIM), kn)
    v = v.reshape(b_, m_, X_HEADS, X_HEADDIM)
    s = jnp.einsum("bqhd,bmhd->bhqm", q, k).astype(jnp.float32) * (X_HEADDIM ** -0.5)
    p = jax.nn.softmax(s, axis=-1).astype(v.dtype)
    o = jnp.einsum("bhqm,bmhd->bqhd", p, v).reshape(b_, s_, X_INNER)
    return o @ wo


def swiglu(h, w13, w2):
    up, gate = jnp.split(h @ w13, 2, axis=-1)
    return (jax.nn.silu(gate) * up) @ w2


def moe(h, router, w13, w2):
    logits = (h @ router).astype(jnp.float32)
    top_v, top_i = lax.top_k(logits, TOP_K)
    w = jax.nn.softmax(top_v, axis=-1)
    gate = jnp.sum(jax.nn.one_hot(top_i, N_EXPERTS, dtype=jnp.float32) * w[..., None], axis=-2)
    y = jnp.zeros_like(h)
    for e in range(N_EXPERTS):
        y = y + gate[..., e:e + 1].astype(h.dtype) * swiglu(h, w13[e], w2[e])
    return y


def setup_inputs(seed: int = 0) -> dict:
    key = jax.random.key(seed)
    keys = iter(jax.random.split(key, 64))
    f32 = jnp.float32

    def nrm(shape, fan_in):
        return jax.random.normal(next(keys), shape, f32) * (fan_in ** -0.5)

    def gain(shape):
        return 1.0 + 0.02 * jax.random.normal(next(keys), shape, f32)

    def bias(shape):
        return 0.01 * jax.random.normal(next(keys), shape, f32)

    a0 = jax.random.uniform(next(keys), (N_EVEN, RG_WIDTH), f32, minval=0.9, maxval=0.999)
    s0 = a0 ** (1.0 / RG_C)
    lam = jnp.log(s0) - jnp.log1p(-s0)
    dt0 = jnp.exp(jax.random.uniform(next(keys), (N_ODD, SSM_HEADS), f32,
                                     minval=math.log(1e-3), maxval=math.log(1e-1)))
    dt_bias = dt0 + jnp.log(-jnp.expm1(-dt0))
    a_log = jnp.log(jax.random.uniform(next(keys), (N_ODD, SSM_HEADS), f32, minval=1.0, maxval=16.0))
    return {
        "x": jax.random.normal(next(keys), (BATCH, SEQ, D_MODEL), f32),
        "mem": jax.random.normal(next(keys), (BATCH, MEM_LEN, D_MODEL), f32),
        "norm_mix": gain((DEPTH, D_MODEL)),
        "norm_cross": gain((DEPTH, D_MODEL)),
        "norm_mem": gain((DEPTH, D_MODEL)),
        "norm_ffn": gain((DEPTH, D_MODEL)),
        "ev_w_in": nrm((N_EVEN, D_MODEL, EV_IN), D_MODEL),
        "ev_rg_conv_w": nrm((N_EVEN, CONV_W, RG_WIDTH), CONV_W),
        "ev_rg_conv_b": bias((N_EVEN, RG_WIDTH)),
        "ev_rg_wa": nrm((N_EVEN, RG_BLOCKS, RG_BW, RG_BW), RG_BW),
        "ev_rg_ba": bias((N_EVEN, RG_WIDTH)),
        "ev_rg_wx": nrm((N_EVEN, RG_BLOCKS, RG_BW, RG_BW), RG_BW),
        "ev_rg_bx": bias((N_EVEN, RG_WIDTH)),
        "ev_rg_lambda": lam,
        "ev_nsa_gate_b": bias((N_EVEN, 3 * NSA_HEADS)),
        "ev_q_norm": gain((N_EVEN, NSA_DK)),
        "ev_k_norm": gain((N_EVEN, 3, NSA_DK)),
        "ev_cmp_pos": 0.1 * jax.random.normal(next(keys), (N_EVEN, CMP_LEN, NSA_DK), f32),
        "ev_cmp_k_w1": nrm((N_EVEN, CMP_LEN * NSA_DK, CMP_HID), CMP_LEN * NSA_DK),
        "ev_cmp_k_w2": nrm((N_EVEN, CMP_HID, NSA_DK), CMP_HID),
        "ev_cmp_v_w1": nrm((N_EVEN, CMP_LEN * NSA_DK, CMP_HID), CMP_LEN * NSA_DK),
        "ev_cmp_v_w2": nrm((N_EVEN, CMP_HID, NSA_DV), CMP_HID),
        "ev_w_out": nrm((N_EVEN, EV_MIX, D_MODEL), EV_MIX),
        "od_w_in": nrm((N_ODD, D_MODEL, OD_IN), D_MODEL),
        "od_conv_w": nrm((N_ODD, CONV_W, CONV_CH), CONV_W),
        "od_conv_b": bias((N_ODD, CONV_CH)),
        "od_dt_bias": dt_bias,
        "od_a_log": a_log,
        "od_d_skip": gain((N_ODD, SSM_HEADS)),
        "od_norm": gain((N_ODD, D_INNER)),
        "od_w_out": nrm((N_ODD, D_INNER, D_MODEL), D_INNER),
        "x_wq": nrm((DEPTH, D_MODEL, X_INNER), D_MODEL),
        "x_wkv": nrm((DEPTH, D_MODEL, 2 * X_INNER), D_MODEL),
        "x_q_norm": gain((DEPTH, X_HEADDIM)),
        "x_k_norm": gain((DEPTH, X_HEADDIM)),
        "x_wo": nrm((DEPTH, X_INNER, D_MODEL), X_INNER),
        "ff_w13": nrm((N_EVEN, D_MODEL, 2 * FF_DENSE), D_MODEL),
        "ff_w2": nrm((N_EVEN, FF_DENSE, D_MODEL), FF_DENSE),
        "moe_router": nrm((N_ODD, D_MODEL, N_EXPERTS), D_MODEL),
        "moe_w13": nrm((N_ODD, N_EXPERTS, D_MODEL, 2 * FF_EXPERT), D_MODEL),
        "moe_w2": nrm((N_ODD, N_EXPERTS, FF_EXPERT, D_MODEL), FF_EXPERT),
    }


def reference(x, mem, norm_mix, norm_cross, norm_mem, norm_ffn,
              ev_w_in, ev_rg_conv_w, ev_rg_conv_b, ev_rg_wa, ev_rg_ba, ev_rg_wx, ev_rg_bx, ev_rg_lambda,
              ev_nsa_gate_b, ev_q_norm, ev_k_norm, ev_cmp_pos, ev_cmp_k_w1, ev_cmp_k_w2, ev_cmp_v_w1,
              ev_cmp_v_w2, ev_w_out,
              od_w_in, od_conv_w, od_conv_b, od_dt_bias, od_a_log, od_d_skip, od_norm, od_w_out,
              x_wq, x_wkv, x_q_norm, x_k_norm, x_wo,
              ff_w13, ff_w2, moe_router, moe_w13, moe_w2):
    for layer in range(DEPTH):
        i = layer // 2
        h = rmsnorm(x, norm_mix[layer])
        if layer % 2 == 0:
            mix = even_mixer(h, ev_w_in[i], ev_rg_conv_w[i], ev_rg_conv_b[i], ev_rg_wa[i], ev_rg_ba[i],
                             ev_rg_wx[i], ev_rg_bx[i], ev_rg_lambda[i], ev_nsa_gate_b[i], ev_q_norm[i],
                             ev_k_norm[i], ev_cmp_pos[i], ev_cmp_k_w1[i], ev_cmp_k_w2[i], ev_cmp_v_w1[i],
                             ev_cmp_v_w2[i], ev_w_out[i])
        else:
            mix = mamba2(h, od_w_in[i], od_conv_w[i], od_conv_b[i], od_dt_bias[i], od_a_log[i],
                         od_d_skip[i], od_norm[i], od_w_out[i])
        x = x + mix
        x = x + cross_attn(rmsnorm(x, norm_cross[layer]), rmsnorm(mem, norm_mem[layer]),
                           x_wq[layer], x_wkv[layer], x_q_norm[layer], x_k_norm[layer], x_wo[layer])
        h = rmsnorm(x, norm_ffn[layer])
        if layer % 2 == 0:
            x = x + swiglu(h, ff_w13[i], ff_w2[i])
        else:
            x = x + moe(h, moe_router[i], moe_w13[i], moe_w2[i])
    return x
```

```python
import numpy as np
import ml_dtypes
import concourse.bass as bass
import concourse.mybir as mybir
from concourse.bass_utils import run_bass_kernel_spmd

F32 = mybir.dt.float32
BF16 = mybir.dt.bfloat16
I32 = mybir.dt.int32
AF = mybir.ActivationFunctionType
ALU = mybir.AluOpType
AX = mybir.AxisListType
NPBF = ml_dtypes.bfloat16

SEM_ROLL = 30000
NUM_DEV = None


class V:
    __slots__ = ("tile", "ap")

    def __init__(self, tile, ap):
        self.tile = tile
        self.ap = ap

    def __getitem__(self, idx):
        return V(self.tile, self.ap[idx])

    def re(self, pat, **kw):
        return V(self.tile, self.ap.rearrange(pat, **kw))

    def bc(self, shape):
        return V(self.tile, self.ap.to_broadcast(shape))


class Tile:
    def __init__(self, ctx, h, name, space):
        self.ctx = ctx
        self.h = h
        self.name = name
        self.space = space
        self.last_w = None
        self.reads = []
        self.dsem = None
        self.dcnt = 0

    def __getitem__(self, idx):
        return V(self, self.h[idx])

    @property
    def v(self):
        return V(self, self.h[:])


class Eng:
    def __init__(self, ctx, name, h):
        self.ctx = ctx
        self.name = name
        self.h = h
        self.sem = None
        self.cnt = 0
        self.waited = {}
        self.n = 0

    def newsem(self):
        self.sem = self.ctx.nc.alloc_semaphore(f"s_{self.name}_{self.ctx.nsem}")
        self.ctx.nsem += 1
        self.ctx.sems[id(self.sem)] = self.sem
        self.cnt = 0


class Ctx:
    def __init__(self):
        self.nc = bass.Bass("TRN2", target_bir_lowering=False, num_devices=NUM_DEV)
        nc = self.nc
        self.nsem = 0
        self.sems = {}
        self.E = {
            "pe": Eng(self, "pe", nc.tensor),
            "act": Eng(self, "act", nc.scalar),
            "dve": Eng(self, "dve", nc.vector),
            "pool": Eng(self, "pool", nc.gpsimd),
            "sp": Eng(self, "sp", nc.sync),
        }
        for e in self.E.values():
            e.newsem()
        self.ntile = 0
        self._all_tiles = []
        self._scopes = []
        self.free_dsems = []
        self.out_tiles = []
        self.sb_bytes = 0

    def sb(self, shape, dt, name=None):
        name = f"sb{self.ntile}_{name or 't'}"
        self.ntile += 1
        if self._scopes:
            h = self._scopes[-1][0].enter_context(self.nc.sbuf_tensor(name, list(shape), dt))
        else:
            h = self.nc.alloc_sbuf_tensor(name, list(shape), dt)
        t = Tile(self, h, name, "sb")
        self._all_tiles.append(t)
        if self._scopes:
            self._scopes[-1][1].append(t)
        return t

    def scope(self):
        from contextlib import contextmanager, ExitStack

        @contextmanager
        def _cm():
            st = ExitStack()
            tiles = []
            self._scopes.append((st, tiles))
            try:
                yield
            finally:
                self.barrier()
                self._scopes.pop()
                st.close()
                for t in tiles:
                    if t.dsem is not None:
                        self.free_dsems.append((t.dsem, t.dcnt))
                        t.dsem = None
                dead = set(id(t) for t in tiles)
                self._all_tiles = [t for t in self._all_tiles if id(t) not in dead]
                self.out_tiles = [t for t in self.out_tiles if id(t) not in dead]
        return _cm()

    def sb_scoped(self, stack, shape, dt, name=None):
        name = f"sb{self.ntile}_{name or 't'}"
        self.ntile += 1
        h = stack.enter_context(self.nc.sbuf_tensor(name, list(shape), dt))
        t = Tile(self, h, name, "sb")
        self._all_tiles.append(t)
        return t

    def ps(self, shape, dt=F32, name=None):
        name = f"ps{self.ntile}_{name or 'p'}"
        self.ntile += 1
        if self._scopes:
            h = self._scopes[-1][0].enter_context(self.nc.psum_tensor(name, list(shape), dt))
        else:
            h = self.nc.alloc_psum_tensor(name, list(shape), dt)
        return Tile(self, h, name, "ps")

    def din(self, name, shape, dt):
        return self.nc.dram_tensor(name, list(shape), dt, kind="ExternalInput").ap()

    def dout(self, name, shape, dt):
        return self.nc.dram_tensor(name, list(shape), dt, kind="ExternalOutput").ap()

    def _wait(self, eng, ev):
        sem_id, val = ev[0], ev[1]
        if eng.waited.get(sem_id, 0) >= val:
            return
        eng.h.wait_ge(self.sems[sem_id], val)
        eng.waited[sem_id] = val

    def _sync(self, eng, reads, writes, is_mm=False):
        for t in reads:
            if t.last_w is not None:
                self._wait(eng, t.last_w)
            if t.space == "ps":
                for r in t.reads:
                    if r[2] != eng.name:
                        self._wait(eng, r)
        for t in writes:
            if t.last_w is not None:
                lw = t.last_w
                if not (is_mm and lw[3] and lw[2] == eng.name):
                    if lw[2] != eng.name or True:
                        self._wait(eng, lw)
            for r in t.reads:
                if is_mm and r[2] == eng.name:
                    continue
                self._wait(eng, r)

    def _done(self, eng, ins, reads, writes, is_mm=False):
        if eng.cnt >= SEM_ROLL:
            eng.newsem()
        eng.cnt += 1
        eng.n += 1
        ins.then_inc(eng.sem, 1)
        ev = (id(eng.sem), eng.cnt, eng.name, is_mm)
        for t in writes:
            t.last_w = ev
            t.reads = []
        for t in reads:
            if t not in writes:
                t.reads.append(ev)
                if len(t.reads) > 12:
                    best = {}
                    for r in t.reads:
                        if r[0] not in best or best[r[0]][1] < r[1]:
                            best[r[0]] = r
                    t.reads = list(best.values())

    def _op(self, engname, fn, reads, writes, is_mm=False):
        eng = self.E[engname]
        reads = [v.tile for v in reads if isinstance(v, V)]
        writes = [v.tile for v in writes if isinstance(v, V)]
        self._sync(eng, reads, writes, is_mm)
        ins = fn(eng.h)
        self._done(eng, ins, reads, writes, is_mm)

    def dma(self, out, in_, q="sp", **kw):
        eng = self.E[q]
        if isinstance(out, V):
            t = out.tile
            lw = t.last_w
            if lw is not None and lw[2] == "dma":
                t.last_w = None
            self._sync(eng, [], [t])
            t.last_w = lw
            o, i = out.ap, in_
            wr = True
        else:
            t = in_.tile
            self._sync(eng, [t], [])
            o, i = out, in_.ap
            wr = False
        if t.dsem is None:
            while self.free_dsems and self.free_dsems[-1][1] >= SEM_ROLL - 2000:
                self.free_dsems.pop()
            if self.free_dsems:
                t.dsem, t.dcnt = self.free_dsems.pop()
            else:
                t.dsem = self.nc.alloc_semaphore(f"d_{t.name}")
                self.sems[id(t.dsem)] = t.dsem
        if t.dcnt >= SEM_ROLL:
            self._wait(eng, (id(t.dsem), t.dcnt))
            t.dsem = self.nc.alloc_semaphore(f"d_{t.name}_{self.nsem}")
            self.nsem += 1
            self.sems[id(t.dsem)] = t.dsem
            t.dcnt = 0
        ins = eng.h.dma_start(out=o, in_=i, **kw)
        t.dcnt += 16
        ins.then_inc(t.dsem, 16)
        ev = (id(t.dsem), t.dcnt, "dma", False)
        if wr:
            t.last_w = ev
            t.reads = []
        else:
            t.reads.append(ev)
            self.out_tiles.append(t)

    def finish(self):
        eng = self.E["sp"]
        seen = set()
        for t in self.out_tiles:
            if id(t) in seen:
                continue
            seen.add(id(t))
            self._wait(eng, (id(t.dsem), t.dcnt))

    def mm(self, out, lhsT, rhs, start=True, stop=True):
        self._op("pe", lambda h: h.matmul(out.ap, lhsT.ap, rhs.ap, start=start, stop=stop),
                 [lhsT, rhs], [out], is_mm=True)

    def tr(self, out, in_, ident):
        self._op("pe", lambda h: h.transpose(out.ap, in_.ap, ident.ap), [in_, ident], [out], is_mm=True)

    def act(self, out, in_, func, bias=None, scale=None, accum_out=None, eng="act"):
        kw = {}
        rd = [in_]
        if bias is not None:
            kw["bias"] = bias.ap if isinstance(bias, V) else bias
            rd.append(bias)
        if scale is not None:
            kw["scale"] = scale.ap if isinstance(scale, V) else scale
            rd.append(scale)
        wr = [out]
        if accum_out is not None:
            kw["accum_out"] = accum_out.ap
            wr.append(accum_out)
        self._op("act", lambda h: h.activation(out.ap, in_.ap, func, **kw), rd, wr)

    def tt(self, out, in0, in1, op, eng="dve"):
        self._op(eng, lambda h: h.tensor_tensor(out.ap, in0.ap, in1.ap, op), [in0, in1], [out])

    def ts(self, out, in0, s1, op0, s2=None, op1=None, eng="dve", accum_out=None):
        a1 = s1.ap if isinstance(s1, V) else s1
        a2 = s2.ap if isinstance(s2, V) else s2
        kw = {}
        wr = [out]
        if op1 is not None:
            kw["op1"] = op1
        if accum_out is not None:
            kw["accum_out"] = accum_out.ap
            wr.append(accum_out)
        self._op(eng, lambda h: h.tensor_scalar(out.ap, in0.ap, a1, a2, op0, **kw), [in0, s1, s2], wr)

    def stt(self, out, in0, scalar, in1, op0, op1):
        a = scalar.ap if isinstance(scalar, V) else scalar
        self._op("dve", lambda h: h.scalar_tensor_tensor(out.ap, in0.ap, a, in1.ap, op0, op1),
                 [in0, scalar, in1], [out])

    def scan(self, out, d0, d1, initial, op0=ALU.mult, op1=ALU.add):
        a = initial.ap if isinstance(initial, V) else initial
        self._op("dve", lambda h: h.tensor_tensor_scan(out.ap, d0.ap, d1.ap, a, op0, op1),
                 [d0, d1, initial], [out])

    def copy(self, out, in_, eng="dve"):
        if eng == "act":
            self._op("act", lambda h: h.copy(out.ap, in_.ap), [in_], [out])
        else:
            self._op(eng, lambda h: h.tensor_copy(out.ap, in_.ap), [in_], [out])

    def memset(self, out, val, eng="dve"):
        self._op(eng, lambda h: h.memset(out.ap, val), [], [out])

    def recip(self, out, in_):
        self._op("dve", lambda h: h.reciprocal(out.ap, in_.ap), [in_], [out])

    def max8(self, out, in_):
        self._op("dve", lambda h: h.max(out.ap, in_.ap), [in_], [out])

    def match_replace(self, out, to_rep, vals, imm):
        self._op("dve", lambda h: h.match_replace(out.ap, to_rep.ap, vals.ap, imm), [to_rep, vals], [out])

    def run(self, in_maps, n=8):
        self.finish()
        return run_bass_kernel_spmd(self.nc, in_maps, core_ids=list(range(n)))


def _barrier(self):
    evs = [(id(e.sem), e.cnt) for e in self.E.values() if e.cnt > 0]
    dm = {}
    for t in self._all_tiles:
        if t.dsem is not None and t.dcnt > 0:
            dm[id(t.dsem)] = max(dm.get(id(t.dsem), 0), t.dcnt)
    evs += list(dm.items())
    for e in self.E.values():
        for ev in evs:
            self._wait(e, ev)


Ctx.barrier = _barrier


EPS = 1e-6
TB = 512


class Pools:
    def __init__(self, c, nps=8, nw=2, wcols=28 * 512):
        self.c = c
        self.ps = [c.ps([128, 512], F32, name=f"psb{i}") for i in range(nps)]
        self.pi = 0
        self.w = [c.sb([128, wcols], BF16, name=f"wb{i}") for i in range(nw)]
        self.wi = 0

    def psum(self):
        t = self.ps[self.pi % len(self.ps)]
        self.pi += 1
        return t

    def wbuf(self):
        t = self.w[self.wi % len(self.w)]
        self.wi += 1
        return t


def load_consts(c, ident_d=None):
    K = {}
    K["ones_mean"] = c.sb([128, 128], BF16, name="ones_mean")
    c.memset(K["ones_mean"].v, 1.0 / 1024)
    K["ones_mean128"] = c.sb([128, 128], BF16, name="ones_mean128")
    c.memset(K["ones_mean128"].v, 1.0 / 128)
    K["ones"] = c.sb([128, 128], BF16, name="ones")
    c.memset(K["ones"].v, 1.0)
    K["eps"] = c.sb([128, 1], F32, name="epsc")
    c.memset(K["eps"].v, EPS)
    return K


def rmsnorm_fm(c, P, K, x, g, hn, kc_n, tb, sq, rstd, ones_key="ones_mean"):
    c.act(sq, x, AF.Square)
    ps = P.psum()
    for k in range(kc_n):
        c.mm(ps[:, :tb], K[ones_key].v, sq[:, k, :], start=(k == 0), stop=(k == kc_n - 1))
    c.act(rstd, ps[:, :tb], AF.Ln, bias=K["eps"].v)
    c.act(rstd, rstd, AF.Exp, scale=-0.5)
    for k in range(kc_n):
        c.stt(hn[:, k, :], x[:, k, :], g[:, k:k + 1], rstd, ALU.mult, ALU.mult)


def gemm(c, P, W, KC, cols, rhs_fn, out_fn, tb, NP=512, q="sp"):
    c0, n = cols
    nchunks = n // 128
    ci = 0
    p0 = 0
    while p0 < n:
        npan = min(NP, n - p0)
        wt = P.wbuf()
        wv = V(wt, wt.h[:, :KC * npan].rearrange("p (k n) -> p k n", k=KC))
        c.dma(wv, W[:, c0 + p0:c0 + p0 + npan].rearrange("(k p) n -> p k n", p=128), q=q)
        for j in range(npan // 128):
            ps = P.psum()
            for k in range(KC):
                c.mm(ps[:, :tb], wv[:, k, j * 128:(j + 1) * 128], rhs_fn(k), start=(k == 0), stop=(k == KC - 1))
            out_fn(ci, ps[:, :tb])
            ci += 1
        p0 += npan


def build_cast(F, CH=8192, c0=None, io=None):
    c = c0 or Ctx()

    def IN(name, shape, dt):
        if io is not None and name in io:
            ap = io[name]
            assert list(ap.shape) == list(shape), (name, ap.shape, shape)
            return ap
        assert io is None, name
        return c.din(name, shape, dt)

    def OUT(name, shape, dt):
        if io is not None and name in io:
            ap = io[name]
            assert list(ap.shape) == list(shape), (name, ap.shape, shape)
            return ap
        assert io is None, name
        return c.dout(name, shape, dt)
    xin = IN("w32", [128, F], F32)
    out = OUT("w16", [128, F], BF16)
    a = [c.sb([128, CH], F32, name=f"ca{i}") for i in range(2)]
    b = [c.sb([128, CH], BF16, name=f"cb{i}") for i in range(2)]
    engs = ["dve", "pool", "act"]
    i = 0
    p = 0
    while p < F:
        n = min(CH, F - p)
        ta, tb_ = a[i % 2], b[i % 2]
        c.dma(ta[:, :n], xin[:, p:p + n], q="sp")
        c.copy(tb_[:, :n], ta[:, :n], eng=engs[i % 3])
        c.dma(out[:, p:p + n], tb_[:, :n], q="act" if False else "sp")
        p += n
        i += 1
    return c


def mem_kv(c, P, K, memT_d, norm_mem_d, wkv_d, kn_d, Kt, Vt):
    with c.scope():
        _mem_kv_body(c, P, K, memT_d, norm_mem_d, wkv_d, kn_d, Kt, Vt)


def _mem_kv_body(c, P, K, memT_d, norm_mem_d, wkv_d, kn_d, Kt, Vt):
    xm = c.sb([128, 8, 256], F32, name="xm")
    c.dma(xm.v, memT_d.rearrange("(k p) m -> p k m", p=128))
    g = c.sb([128, 8], F32, name="gmem")
    c.dma(g.v, norm_mem_d)
    kn = c.sb([128, 1], F32, name="kn")
    c.dma(kn.v, kn_d)
    hm = c.sb([128, 8, 256], BF16, name="hm")
    sq = c.sb([128, 8, 256], BF16, name="sqm")
    rstd = c.sb([128, 256], F32, name="rstdm")
    rmsnorm_fm(c, P, K, xm.v, g.v, hm.v, 8, 256, sq.v, rstd.v)
    kraw = c.sb([128, 4, 256], F32, name="kraw")

    def out_k(ci, ps):
        c.copy(kraw[:, ci, :], ps, eng="act")
    gemm(c, P, wkv_d, 8, (0, 512), lambda k: hm[:, k, :], out_k, 256)
    ksq = c.sb([128, 4, 256], BF16, name="ksq")
    c.act(ksq.v, kraw.v, AF.Square)
    for h in range(4):
        ps = P.psum()
        c.mm(ps[:, :256], K["ones_mean128"].v, ksq[:, h, :])
        c.act(rstd.v, ps[:, :256], AF.Ln, bias=K["eps"].v)
        c.act(rstd.v, rstd.v, AF.Exp, scale=-0.5)
        c.stt(Kt[:, h, :], kraw[:, h, :], kn[:, 0:1], rstd.v, ALU.mult, ALU.mult)
    wt = P.wbuf()
    wv = V(wt, wt.h[:, :8 * 512].rearrange("p (k n) -> p k n", k=8))
    c.dma(wv, wkv_d[:, 512:1024].rearrange("(k p) n -> p k n", p=128))
    for mt in range(2):
        ps = P.psum()
        for k in range(8):
            c.mm(ps.v, hm[:, k, mt * 128:(mt + 1) * 128], wv[:, k, :], start=(k == 0), stop=(k == 7))
        c.copy(Vt[:, mt, :], ps.v, eng="act")


def cross_attn_blk(c, P, K, x, hn, sq, rstd, gcross, wq_d, qn, Kt, Vt, wo_d, S):
    rmsnorm_fm(c, P, K, x, gcross, hn, 8, TB, sq, rstd)
    qraw, qn_bf, pT, oT = S["qraw"], S["qn_bf"], S["pT"], S["oT"]

    def out_q(ci, ps):
        c.copy(qraw[:, ci, :], ps, eng="act")
    gemm(c, P, wq_d, 8, (0, 512), lambda k: hn[:, k, :], out_q, TB)
    c.act(S["qsq"].v, qraw.v, AF.Square)
    scale = 128 ** -0.5
    for h in range(4):
        ps = P.psum()
        c.mm(ps.v, K["ones_mean128"].v, S["qsq"][:, h, :])
        c.act(rstd, ps.v, AF.Ln, bias=K["eps"].v)
        c.act(rstd, rstd, AF.Exp, scale=-0.5)
        c.stt(qn_bf[:, h, :], qraw[:, h, :], qn[:, 0:1], rstd, ALU.mult, ALU.mult)
        for mt in range(2):
            ps2 = P.psum()
            c.mm(ps2.v, Kt[:, h, mt * 128:(mt + 1) * 128], qn_bf[:, h, :])
            c.act(pT[:, mt, :], ps2.v, AF.Exp, scale=scale)
        pso = P.psum()
        psd = P.psum()
        for mt in range(2):
            c.mm(pso.v, Vt[:, mt, h * 128:(h + 1) * 128], pT[:, mt, :], start=(mt == 0), stop=(mt == 1))
        for mt in range(2):
            c.mm(psd.v, K["ones"].v, pT[:, mt, :], start=(mt == 0), stop=(mt == 1))
        c.act(rstd, psd.v, AF.Ln)
        c.act(rstd, rstd, AF.Exp, scale=-1.0)
        c.tt(oT[:, h, :], pso.v, rstd, ALU.mult)

    def out_o(ci, ps):
        c.tt(x[:, ci, :], ps, x[:, ci, :], ALU.add)
    gemm(c, P, wo_d, 4, (0, 1024), lambda k: oT[:, k, :], out_o, TB)


def cross_scratch(c):
    S = {}
    S["qraw"] = c.sb([128, 4, TB], F32, name="qraw")
    S["qsq"] = c.sb([128, 4, TB], BF16, name="qsq")
    S["qn_bf"] = c.sb([128, 4, TB], BF16, name="qn_bf")
    S["pT"] = c.sb([128, 2, TB], BF16, name="pT")
    S["oT"] = c.sb([128, 4, TB], BF16, name="oT")
    return S


def build_stage_c(NT=2048, c0=None, io=None):
    c = c0 or Ctx()

    def IN(name, shape, dt):
        if io is not None and name in io:
            ap = io[name]
            assert list(ap.shape) == list(shape), (name, ap.shape, shape)
            return ap
        assert io is None, name
        return c.din(name, shape, dt)

    def OUT(name, shape, dt):
        if io is not None and name in io:
            ap = io[name]
            assert list(ap.shape) == list(shape), (name, ap.shape, shape)
            return ap
        assert io is None, name
        return c.dout(name, shape, dt)
    xT = IN("xT", [1024, NT], F32)
    mixT = IN("mixT", [2048, NT], BF16)
    memT = IN("memT", [1024, 256], F32)
    w_out = IN("w_out", [2048, 1024], BF16)
    wq = IN("wq", [1024, 512], BF16)
    wkv = IN("wkv", [1024, 1024], BF16)
    wo = IN("wo", [512, 1024], BF16)
    w13 = IN("w13", [1024, 5632], BF16)
    w2 = IN("w2", [2816, 1024], BF16)
    norm_cross = IN("norm_cross", [128, 8], F32)
    norm_mem = IN("norm_mem", [128, 8], F32)
    norm_ffn = IN("norm_ffn", [128, 8], F32)
    qn_d = IN("qn", [128, 1], F32)
    kn_d = IN("kn", [128, 1], F32)
    outT = OUT("outT", [1024, NT], F32)

    P = Pools(c, nw=3, wcols=22 * 512)
    K = load_consts(c)
    Kt = c.sb([128, 4, 256], BF16, name="Kt")
    Vt = c.sb([128, 2, 512], BF16, name="Vt")
    mem_kv(c, P, K, memT, norm_mem, wkv, kn_d, Kt, Vt)
    gc = c.sb([128, 8], F32, name="gc")
    c.dma(gc.v, norm_cross)
    gf = c.sb([128, 8], F32, name="gf")
    c.dma(gf.v, norm_ffn)
    qn = c.sb([128, 1], F32, name="qn")
    c.dma(qn.v, qn_d)
    S = cross_scratch(c)
    xs = [c.sb([128, 8, TB], F32, name=f"x{i}") for i in range(2)]
    mixs = [c.sb([128, 16, TB], BF16, name=f"mix{i}") for i in range(2)]
    hn = c.sb([128, 8, TB], BF16, name="hn")
    rstd = c.sb([128, TB], F32, name="rstd")
    hff = c.sb([128, 22, TB], BF16, name="hff")
    sq = hff[:, 0:8, :]
    sg = c.sb([128, 4, TB], BF16, name="sg")

    def loadC(blk):
        tsl_ = slice(blk * TB, (blk + 1) * TB)
        c.dma(xs[blk % 2].v, xT[:, tsl_].rearrange("(k p) t -> p k t", p=128), q="act")
        c.dma(mixs[blk % 2].v, mixT[:, tsl_].rearrange("(k p) t -> p k t", p=128), q="act")
    loadC(0)
    for blk in range(NT // TB):
        x = xs[blk % 2]
        mix = mixs[blk % 2]
        ts_ = slice(blk * TB, (blk + 1) * TB)
        if blk + 1 < NT // TB:
            loadC(blk + 1)

        def out_mix(ci, ps):
            c.tt(x[:, ci, :], ps, x[:, ci, :], ALU.add)
        gemm(c, P, w_out, 16, (0, 1024), lambda k: mix[:, k, :], out_mix, TB)
        cross_attn_blk(c, P, K, x.v, hn.v, sq, rstd.v, gc.v, wq, qn, Kt, Vt, wo, S)
        rmsnorm_fm(c, P, K, x.v, gf.v, hn.v, 8, TB, sq, rstd.v)
        for p0 in range(0, 2816, 512):
            n = min(512, 2816 - p0)

            def out_gate(ci, ps):
                c.act(sg[:, ci, :], ps, AF.Silu)

            def out_up(ci, ps, p0=p0):
                c.tt(hff[:, p0 // 128 + ci, :], ps, sg[:, ci, :], ALU.mult)
            gemm(c, P, w13, 8, (2816 + p0, n), lambda k: hn[:, k, :], out_gate, TB)
            gemm(c, P, w13, 8, (p0, n), lambda k: hn[:, k, :], out_up, TB)

        def out_dn(ci, ps):
            c.tt(x[:, ci, :], ps, x[:, ci, :], ALU.add)
        gemm(c, P, w2, 22, (0, 1024), lambda k: hff[:, k, :], out_dn, TB)
        c.dma(outT[:, ts_].rearrange("(k p) t -> p k t", p=128), x.v, q="act")
    return c


def build_stage_a(S=8192, CT=2048, c0=None, io=None):
    c = c0 or Ctx()

    def IN(name, shape, dt):
        if io is not None and name in io:
            ap = io[name]
            assert list(ap.shape) == list(shape), (name, ap.shape, shape)
            return ap
        assert io is None, name
        return c.din(name, shape, dt)

    def OUT(name, shape, dt):
        if io is not None and name in io:
            ap = io[name]
            assert list(ap.shape) == list(shape), (name, ap.shape, shape)
            return ap
        assert io is None, name
        return c.dout(name, shape, dt)
    xT = IN("xT", [1024, S], F32)
    gmix_d = IN("gmix", [128, 8], F32)
    wA_d = IN("wA", [1024, 512], BF16)
    cw_d = IN("cw", [128, 8], F32)
    cb_d = IN("cb", [128, 2], F32)
    wa_d = IN("wa", [128, 256], BF16)
    wx_d = IN("wx", [128, 256], BF16)
    ba_d = IN("ba", [128, 2], F32)
    bx_d = IN("bx", [128, 2], F32)
    lam_d = IN("lam", [128, 2], F32)
    rgT = OUT("rgT", [256, S], BF16)

    P = Pools(c, nw=1, wcols=8 * 512)
    K = load_consts(c)
    gm = c.sb([128, 8], F32, name="gm"); c.dma(gm.v, gmix_d)
    cw = c.sb([128, 8], F32, name="cw"); c.dma(cw.v, cw_d)
    cb = c.sb([128, 2], F32, name="cb"); c.dma(cb.v, cb_d)
    wa = c.sb([128, 256], BF16, name="wa"); c.dma(wa.v, wa_d)
    wx = c.sb([128, 256], BF16, name="wx"); c.dma(wx.v, wx_d)
    ba = c.sb([128, 2], F32, name="ba"); c.dma(ba.v, ba_d)
    bx = c.sb([128, 2], F32, name="bx"); c.dma(bx.v, bx_d)
    lam = c.sb([128, 2], F32, name="lam"); c.dma(lam.v, lam_d)
    one = c.sb([128, 1], F32, name="one"); c.memset(one.v, 1.0)
    n8 = c.sb([128, 2], F32, name="n8")
    c.act(n8.v, lam.v, AF.Exp, scale=-1.0)
    c.act(n8.v, n8.v, AF.Ln, bias=one.v)
    c.ts(n8.v, n8.v, -8.0, ALU.mult)
    wA = c.sb([128, 8, 512], BF16, name="wA")
    c.dma(wA.v, wA_d.rearrange("(k p) n -> p k n", p=128))

    xs = [c.sb([128, 8, TB], F32, name=f"x{i}") for i in range(2)]
    hns = [c.sb([128, 8, TB], BF16, name=f"hn{i}") for i in range(2)]
    sqs = [c.sb([128, 8, TB], BF16, name=f"sq{i}") for i in range(2)]
    rstd = c.sb([128, TB], F32, name="rstd")
    rgx = c.sb([128, 2, CT + 3], F32, name="rgx")
    gg = c.sb([128, 2, CT], BF16, name="gg")
    xc = c.sb([128, CT], F32, name="xc")
    xcb = c.sb([128, CT], BF16, name="xcb")
    Rb = c.sb([128, CT], F32, name="Rb")
    IG = c.sb([128, CT], F32, name="IG")
    Ab = c.sb([128, CT], F32, name="Ab")
    Ub = c.sb([128, CT], F32, name="Ub")
    Hc = [c.sb([128, CT], F32, name=f"Hc{i}") for i in range(2)]
    ob = c.sb([128, CT], BF16, name="ob")
    carry = c.sb([128, 2], F32, name="carry")
    c.memset(rgx[:, :, 0:3], 0.0)
    c.memset(carry.v, 0.0)

    def preA(blk):
        x = xs[blk % 2]
        c.dma(x.v, xT[:, blk * TB:(blk + 1) * TB].rearrange("(k p) t -> p k t", p=128), q="sp")
        rmsnorm_fm(c, P, K, x.v, gm.v, hns[blk % 2].v, 8, TB, sqs[blk % 2].v, rstd.v)
    preA(0)
    for ch in range(S // CT):
        for sb_ in range(CT // TB):
            blk = ch * (CT // TB) + sb_
            if blk + 1 < S // TB:
                preA(blk + 1)
            hn = hns[blk % 2]
            for j in range(4):
                ps = P.psum()
                for k in range(8):
                    c.mm(ps.v, wA[:, k, j * 128:(j + 1) * 128], hn[:, k, :], start=(k == 0), stop=(k == 7))
                if j < 2:
                    c.copy(rgx[:, j, 3 + sb_ * TB:3 + (sb_ + 1) * TB], ps.v, eng="act")
                else:
                    c.act(gg[:, j - 2, sb_ * TB:(sb_ + 1) * TB], ps.v, AF.Gelu)
        for b2 in range(2):
            c.act(xc.v, rgx[:, b2, 0:CT], AF.Identity, scale=cw[:, b2 * 4:b2 * 4 + 1], bias=cb[:, b2:b2 + 1])
            for j in range(1, 4):
                c.stt(xc.v, rgx[:, b2, j:j + CT], cw[:, b2 * 4 + j:b2 * 4 + j + 1], xc.v, ALU.mult, ALU.add)
            c.copy(xcb.v, xc.v, eng="pool")
            for s2 in range(CT // TB):
                sl = slice(s2 * TB, (s2 + 1) * TB)
                ps = P.psum()
                c.mm(ps.v, wa[:, b2 * 128:(b2 + 1) * 128], xcb[:, sl])
                c.act(Rb[:, sl], ps.v, AF.Sigmoid, bias=ba[:, b2:b2 + 1])
                ps = P.psum()
                c.mm(ps.v, wx[:, b2 * 128:(b2 + 1) * 128], xcb[:, sl])
                c.act(IG[:, sl], ps.v, AF.Sigmoid, bias=bx[:, b2:b2 + 1])
            c.act(Ab.v, Rb.v, AF.Exp, scale=n8[:, b2:b2 + 1])
            c.tt(Ub.v, Ab.v, Ab.v, ALU.mult, eng="pool")
            c.ts(Ub.v, Ub.v, -1.0, ALU.mult, 1.0, ALU.add, eng="pool")
            c.act(Ub.v, Ub.v, AF.Sqrt)
            c.tt(IG.v, IG.v, xc.v, ALU.mult)
            c.tt(Ub.v, Ub.v, IG.v, ALU.mult)
            H = Hc[b2]
            c.scan(H.v, Ab.v, Ub.v, carry[:, b2:b2 + 1])
            c.copy(carry[:, b2:b2 + 1], H[:, CT - 1:CT])
            c.tt(ob.v, H.v, gg[:, b2, :], ALU.mult)
            c.dma(rgT[b2 * 128:(b2 + 1) * 128, ch * CT:(ch + 1) * CT], ob.v, q="act")
        c.copy(rgx[:, :, 0:3], rgx[:, :, CT:CT + 3])
    return c


def bcl(v, n):
    shp = list(v.ap.shape) + [n]
    return V(v.tile, v.ap.unsqueeze(len(shp) - 1).to_broadcast(shp))


def build_stage_d(S=8192, dbg=False, c0=None, io=None, mode="full", st_d=None, tok0=None, qidx=None, hook=None):
    c = c0 or Ctx()

    def IN(name, shape, dt):
        if io is not None and name in io:
            ap = io[name]
            assert list(ap.shape) == list(shape), (name, ap.shape, shape)
            return ap
        assert io is None, name
        return c.din(name, shape, dt)

    def OUT(name, shape, dt):
        if io is not None and name in io:
            ap = io[name]
            assert list(ap.shape) == list(shape), (name, ap.shape, shape)
            return ap
        assert io is None, name
        return c.dout(name, shape, dt)
    DBG = {}

    def dump(name, v, shape, dt):
        if not dbg:
            return
        o = c.dout("dbg_" + name, shape, dt)
        c.dma(o, v, q="sp")
    FULL = mode != "state"
    nblk = {"full": S // TB, "state": 12, "own": 4}[mode]
    xT = IN("xT", [1024, S], F32)
    gmix_d = IN("gmix", [128, 8], F32)
    wD_d = IN("wD", [1024, 1280], BF16)
    wdt_d = IN("wdt", [1024, 8], BF16)
    cw_d = IN("cw", [128, 24], F32)
    cb_d = IN("cb", [128, 6], F32)
    dtb_d = IN("dtb", [128, 8], F32)
    alog_d = IN("alog", [128, 8], F32)
    dsk_d = IN("dsk", [128, 8], F32)
    gn_d = IN("gn", [128, 4], F32)
    wo_d = IN("wo", [512, 1024], BF16)
    identb_d = IN("identb", [128, 128], BF16)
    identf_d = IN("identf", [128, 128], F32)
    U_d = IN("U", [128, 128], F32)
    NM_d = IN("NM", [128, 128], F32)
    onesf_d = IN("onesf", [128, 128], F32)
    outT = OUT("outT", [1024, nblk * TB], F32) if FULL else None

    P = Pools(c, nps=5, nw=1, wcols=16)
    K = load_consts(c)
    pso = c.ps([128, 512], F32, name="pso")
    psy = c.ps([128, 512], F32, name="psy")

    def ld(name, d, shape, dt):
        t = c.sb(shape, dt, name=name)
        c.dma(t.v, d)
        return t
    gm = ld("gm", gmix_d, [128, 8], F32)
    cw = ld("cw", cw_d, [128, 24], F32)
    cb = ld("cb", cb_d, [128, 6], F32)
    dtb = ld("dtb", dtb_d, [128, 8], F32)
    Aneg = ld("alog", alog_d, [128, 8], F32)
    dsk = ld("dsk", dsk_d, [128, 8], F32)
    gn = ld("gn", gn_d, [128, 4], F32)
    identb = ld("identb", identb_d, [128, 128], BF16)
    identf = ld("identf", identf_d, [128, 128], F32)
    U = ld("U", U_d, [128, 128], F32)
    NM = ld("NM", NM_d, [128, 128], F32)
    onesf = ld("onesf", onesf_d, [128, 128], F32)
    one = c.sb([128, 1], F32, name="one"); c.memset(one.v, 1.0)
    eps = K["eps"]
    c.act(Aneg.v, Aneg.v, AF.Exp)
    c.ts(Aneg.v, Aneg.v, -1.0, ALU.mult)
    wD = c.sb([128, 8, 1280], BF16, name="wD")
    c.dma(wD.v, wD_d.rearrange("(k p) n -> p k n", p=128))
    wdt = c.sb([128, 8, 8], BF16, name="wdt")
    c.dma(wdt.v, wdt_d.rearrange("(k p) n -> p k n", p=128))
    wo = c.sb([128, 4, 1024], BF16, name="wo")
    c.dma(wo.v, wo_d.rearrange("(k p) n -> p k n", p=128))

    xs_in = [c.sb([128, 8, TB], F32, name=f"x{i}") for i in range(2)]
    hns = [c.sb([128, 8, TB], BF16, name=f"hn{i}") for i in range(2)]
    sqs = [c.sb([128, 8, TB], BF16, name=f"sq{i}") for i in range(2)]
    rstd = c.sb([128, TB], F32, name="rstd")
    raw = c.sb([128, 6, TB + 3], F32, name="raw")
    cacc = c.sb([128, TB], F32, name="cacc")
    xact = c.sb([128, 6, TB], BF16, name="xact")
    sz = c.sb([128, 4, 512], BF16, name="sz")
    dt_t = c.sb([128, 4, 8], F32, name="dt_t")
    a_t = c.sb([128, 4, 8], F32, name="a_t")
    xs_tm = c.sb([128, 512], BF16, name="xs_tm")
    B_tm = c.sb([128, 128], BF16, name="B_tm")
    acs = c.sb([128, 8], F32, name="acs")
    nacs = c.sb([128, 8], F32, name="nacs")
    eacs = c.sb([128, 8], F32, name="eacs")
    dst = c.sb([128, 8], F32, name="dst")
    cdec = c.sb([128, 8], F32, name="cdec")
    abc = c.sb([128, 8, 128], F32, name="abc")
    xdt = c.sb([128, 512], BF16, name="xdt")
    xd = c.sb([128, 512], BF16, name="xd")
    GT = c.sb([128, 128], BF16, name="GT")
    LT = [c.sb([128, 128], BF16, name=f"LT{i}") for i in range(3)]
    MT = [c.sb([128, 128], BF16, name=f"MT{i}") for i in range(3)]
    prev = c.sb([128, 512], F32, name="prev")
    prevb = c.sb([128, 512], BF16, name="prevb")
    t1 = c.sb([128, 512], F32, name="t1")
    t2 = c.sb([128, 512], F32, name="t2")
    ysq = c.sb([128, 512], BF16, name="ysq")
    ss = c.sb([128, 1], F32, name="ss")
    yn = c.sb([128, 512], BF16, name="yn")
    ynT = c.sb([128, 4, TB], BF16, name="ynT")
    obuf = c.sb([128, 8, TB], F32, name="obuf")
    psT = c.ps([128, 512], BF16, name="psT")
    if mode == "own":
        stv = st_d[bass.ds(qidx, 1)].rearrange("o p n -> (o p) n")
        c.dma(prev.v, stv[:, 0:512], q="sp")
        c.dma(raw[:, :, 0:3], stv[:, 512:530].rearrange("p (a b) -> p a b", b=3), q="sp")
        c.copy(prevb.v, prev.v)
    else:
        c.memset(raw[:, :, 0:3], 0.0)
        c.memset(prev.v, 0.0)
        c.memset(prevb.v, 0.0)
        if mode == "state":
            c.dma(st_d[0][:, 0:512], prev.v, q="sp")
            c.dma(st_d[0][:, 512:530].rearrange("p (a b) -> p a b", b=3), raw[:, :, 0:3], q="sp")

    def preD(blk):
        x = xs_in[blk % 2]
        xcols = slice(blk * TB, (blk + 1) * TB) if mode != "own" else bass.ds(tok0 + blk * TB, TB)
        c.dma(x.v, xT[:, xcols].rearrange("(k p) t -> p k t", p=128), q="sp")
        rmsnorm_fm(c, P, K, x.v, gm.v, hns[blk % 2].v, 8, TB, sqs[blk % 2].v, rstd.v)
    preD(0)
    for blk in range(nblk):
        if blk + 1 < nblk:
            preD(blk + 1)
        hn = hns[blk % 2]
        for c6 in range(6):
            ps = P.psum()
            for k in range(8):
                c.mm(ps.v, wD[:, k, 512 + c6 * 128:512 + (c6 + 1) * 128], hn[:, k, :], start=(k == 0), stop=(k == 7))
            c.copy(raw[:, c6, 3:3 + TB], ps.v, eng="act")
        for tt_ in range(4):
            tsl = slice(tt_ * 128, (tt_ + 1) * 128)
            if FULL:
                ps = P.psum()
                for k in range(8):
                    c.mm(ps.v, hn[:, k, tsl], wD[:, k, 0:512], start=(k == 0), stop=(k == 7))
                c.act(sz[:, tt_, :], ps.v, AF.Silu)
            ps = P.psum()
            for k in range(8):
                c.mm(ps[:, 0:8], hn[:, k, tsl], wdt[:, k, :], start=(k == 0), stop=(k == 7))
            c.tt(dt_t[:, tt_, :], ps[:, 0:8], dtb.v, ALU.add)
        c.act(dt_t.v, dt_t.v, AF.Exp)
        c.act(dt_t.v, dt_t.v, AF.Ln, bias=one.v)
        c.tt(a_t.v, dt_t.v, V(Aneg, Aneg.h[:].unsqueeze(1).to_broadcast([128, 4, 8])), ALU.mult)
        for c6 in range(6):
            c.act(cacc.v, raw[:, c6, 0:TB], AF.Identity, scale=cw[:, c6 * 4:c6 * 4 + 1], bias=cb[:, c6:c6 + 1])
            for j in range(1, 4):
                c.stt(cacc.v, raw[:, c6, j:j + TB], cw[:, c6 * 4 + j:c6 * 4 + j + 1], cacc.v, ALU.mult, ALU.add)
            c.act(xact[:, c6, :], cacc.v, AF.Silu)
        c.copy(raw[:, :, 0:3], raw[:, :, TB:TB + 3])
        for tt_ in range(4):
            tsl = slice(tt_ * 128, (tt_ + 1) * 128)
            for c4 in range(4):
                c.tr(psT[:, c4 * 128:(c4 + 1) * 128], xact[:, c4, tsl], identb.v)
            c.copy(xs_tm.v, psT.v, eng="act")
            c.tr(psT[:, 0:128], xact[:, 4, tsl], identb.v)
            c.copy(B_tm.v, psT[:, 0:128], eng="act")
            a = a_t[:, tt_, :]
            dt = dt_t[:, tt_, :]
            if hook is not None:
                hook()
            ps = P.psum()
            c.mm(ps[:, 0:8], U.v, a)
            c.copy(acs.v, ps[:, 0:8])
            if FULL:
                c.ts(nacs.v, ps[:, 0:8], -1.0, ALU.mult)
                c.act(eacs.v, ps[:, 0:8], AF.Exp)
            ps2 = P.psum()
            c.mm(ps2[:, 0:8], onesf.v, a)
            c.act(cdec.v, ps2[:, 0:8], AF.Exp)
            c.tt(dst.v, ps2[:, 0:8], acs.v, ALU.subtract)
            c.act(dst.v, dst.v, AF.Exp)
            if FULL:
                c.copy(abc.v, bcl(a, 128), eng="pool")
            c.tt(xdt.v.re("p (h e) -> p h e", h=8), xs_tm.v.re("p (h e) -> p h e", h=8), bcl(dt, 64), ALU.mult)
            c.tt(xd.v.re("p (h e) -> p h e", h=8), xdt.v.re("p (h e) -> p h e", h=8), bcl(dst.v, 64), ALU.mult, eng="pool")
            if FULL:
                psg = P.psum()
                c.mm(psg[:, 0:128], xact[:, 4, tsl], xact[:, 5, tsl])
                c.copy(GT.v, psg[:, 0:128], eng="act")
                c.mm(pso.v, xact[:, 5, tsl], prevb.v)
                def hfront(h):
                    psd = P.psum()
                    c.mm(psd[:, 0:128], abc[:, h, :], U.v, start=True, stop=False)
                    c.mm(psd[:, 0:128], identf.v, NM.v, start=False, stop=True)
                    L = LT[h % 3]
                    M = MT[h % 3]
                    c.act(L.v, psd[:, 0:128], AF.Exp, bias=nacs[:, h:h + 1])
                    c.tt(M.v, L.v, GT.v, ALU.mult)

                def hback(h):
                    c.mm(psy[:, h * 64:(h + 1) * 64], MT[h % 3].v, xdt[:, h * 64:(h + 1) * 64])
                hfront(0)
                hfront(1)
                for h in range(8):
                    hback(h)
                    if h + 2 < 8:
                        hfront(h + 2)
                c.tt(t1.v.re("p (h e) -> p h e", h=8), pso.v.re("p (h e) -> p h e", h=8), bcl(eacs.v, 64), ALU.mult)
                c.tt(t1.v, t1.v, psy.v, ALU.add)
                c.tt(t2.v.re("p (h e) -> p h e", h=8), xs_tm.v.re("p (h e) -> p h e", h=8), bcl(dsk.v, 64), ALU.mult, eng="pool")
                c.tt(t1.v, t1.v, t2.v, ALU.add)
                c.tt(t1.v, t1.v, sz[:, tt_, :], ALU.mult)
                c.act(ysq.v, t1.v, AF.Square, accum_out=ss.v)
                c.ts(ss.v, ss.v, 1.0 / 512, ALU.mult, EPS, ALU.add)
                c.act(ss.v, ss.v, AF.Sqrt)
                c.recip(ss.v, ss.v)
                c.ts(yn.v, t1.v, ss[:, 0:1], ALU.mult)
                for c4 in range(4):
                    c.tr(psT[:, c4 * 128:(c4 + 1) * 128], yn[:, c4 * 128:(c4 + 1) * 128], identb.v)
                for c4 in range(4):
                    c.ts(ynT[:, c4, tsl], psT[:, c4 * 128:(c4 + 1) * 128], gn[:, c4:c4 + 1], ALU.mult)
            pss = P.psum()
            c.mm(pss.v, B_tm.v, xd.v)
            c.tt(prev.v.re("p (h e) -> p h e", h=8), prev.v.re("p (h e) -> p h e", h=8), bcl(cdec.v, 64), ALU.mult)
            c.tt(prev.v, prev.v, pss.v, ALU.add)
            if FULL:
                c.copy(prevb.v, prev.v, eng="pool")
            if FULL and blk == 0 and tt_ == 1:
                dump("dt", dt_t.v, [128, 4, 8], F32)
                dump("a", a_t.v, [128, 4, 8], F32)
                dump("xact", xact.v, [128, 6, TB], BF16)
                dump("sz", sz.v, [128, 4, 512], BF16)
                dump("xs_tm", xs_tm.v, [128, 512], BF16)
                dump("B_tm", B_tm.v, [128, 128], BF16)
                dump("acs", acs.v, [128, 8], F32)
                dump("dst", dst.v, [128, 8], F32)
                dump("cdec", cdec.v, [128, 8], F32)
                dump("GT", GT.v, [128, 128], BF16)
                dump("LT", LT[1].v, [128, 128], BF16)
                dump("MT", MT[1].v, [128, 128], BF16)
                dump("t1", t1.v, [128, 512], F32)
                dump("yn", yn.v, [128, 512], BF16)
                dump("prev", prev.v, [128, 512], F32)
                dump("xdt", xdt.v, [128, 512], BF16)
                dump("ss", ss.v, [128, 1], F32)
        if mode == "state" and blk % 4 == 3:
            qq = (blk + 1) // 4
            c.dma(st_d[qq][:, 0:512], prev.v, q="sp")
            c.dma(st_d[qq][:, 512:530].rearrange("p (a b) -> p a b", b=3), raw[:, :, 0:3], q="sp")
        if not FULL:
            continue
        for n in range(8):
            ps = P.psum()
            for k in range(4):
                c.mm(ps.v, wo[:, k, n * 128:(n + 1) * 128], ynT[:, k, :], start=(k == 0), stop=(k == 3))
            c.copy(obuf[:, n, :], ps.v, eng="act")
        c.dma(outT[:, blk * TB:(blk + 1) * TB].rearrange("(k p) t -> p k t", p=128), obuf.v, q="act")
    return c


def build_stage_e(NT=2048, NE=8, FE=3584, c0=None, io=None, tok0=None):
    c = c0 or Ctx()

    def IN(name, shape, dt):
        if io is not None and name in io:
            ap = io[name]
            assert list(ap.shape) == list(shape), (name, ap.shape, shape)
            return ap
        assert io is None, name
        return c.din(name, shape, dt)

    def OUT(name, shape, dt):
        if io is not None and name in io:
            ap = io[name]
            assert list(ap.shape) == list(shape), (name, ap.shape, shape)
            return ap
        assert io is None, name
        return c.dout(name, shape, dt)
    SX = NT if tok0 is None else 8192
    xT = IN("xT", [1024, SX], F32)
    pT_d = IN("pT", [4, 1024, NT], F32)
    memT = IN("memT", [1024, 256], F32)
    wq = IN("wq", [1024, 512], BF16)
    wkv = IN("wkv", [1024, 1024], BF16)
    wo = IN("wo", [512, 1024], BF16)
    norm_cross = IN("norm_cross", [128, 8], F32)
    norm_mem = IN("norm_mem", [128, 8], F32)
    norm_ffn = IN("norm_ffn", [128, 8], F32)
    qn_d = IN("qn", [128, 1], F32)
    kn_d = IN("kn", [128, 1], F32)
    router_d = IN("router", [1024, NE], F32)
    w13 = IN("w13", [NE, 1024, 2 * FE], BF16)
    w2 = IN("w2", [NE, FE, 1024], BF16)
    identf_d = IN("identf", [128, 128], F32)
    sel_d = IN("sel", [NE, NE * 128], F32)
    outT = OUT("outT", [1024, NT], F32)
    KF = FE // 128

    P = Pools(c, nw=3, wcols=KF * 512)
    K = load_consts(c)
    Kt = c.sb([128, 4, 256], BF16, name="Kt")
    Vt = c.sb([128, 2, 512], BF16, name="Vt")
    mem_kv(c, P, K, memT, norm_mem, wkv, kn_d, Kt, Vt)
    gc = c.sb([128, 8], F32, name="gc"); c.dma(gc.v, norm_cross)
    gf = c.sb([128, 8], F32, name="gf"); c.dma(gf.v, norm_ffn)
    qn = c.sb([128, 1], F32, name="qn"); c.dma(qn.v, qn_d)
    identf = c.sb([128, 128], F32, name="identf"); c.dma(identf.v, identf_d)
    sel = c.sb([NE, NE * 128], F32, name="sel"); c.dma(sel.v, sel_d)
    rt = c.sb([128, 8, NE], F32, name="rt"); c.dma(rt.v, router_d.rearrange("(k p) e -> p k e", p=128))
    S = cross_scratch(c)
    x = c.sb([128, 8, TB], F32, name="x")
    pin = [c.sb([128, 8, TB], F32, name=f"pin{i}") for i in range(1)]
    hn = c.sb([128, 8, TB], BF16, name="hn")
    hn32 = pin[0]
    rstd = c.sb([128, TB], F32, name="rstd")
    hff = c.sb([128, KF, TB], BF16, name="hff")
    sq = hff[:, 0:8, :]
    sg = c.sb([128, 4, TB], BF16, name="sg")
    lg = c.sb([128, NE], F32, name="lg")
    m8 = c.sb([128, 8], F32, name="m8")
    w1 = c.sb([128, 1], F32, name="w1")
    w2s = c.sb([128, 1], F32, name="w2s")
    g1 = c.sb([128, NE], F32, name="g1")
    g2 = c.sb([128, NE], F32, name="g2")
    gT = c.sb([NE, TB], F32, name="gT")
    gb = c.sb([128, TB], F32, name="gb")
    tmp = c.sb([128, TB], F32, name="tmp")
    assert NE == 8

    for blk in range(NT // TB):
        ts_ = slice(blk * TB, (blk + 1) * TB)
        tin = ts_ if tok0 is None else bass.ds(tok0 + blk * TB, TB)
        qin = "act" if tok0 is None else "sp"
        c.dma(x.v, xT[:, tin].rearrange("(k p) t -> p k t", p=128), q=qin)
        for i in range(4):
            pb = pin[0]
            c.dma(pb.v, pT_d[i, :, ts_].rearrange("(k p) t -> p k t", p=128), q=qin)
            c.tt(x.v, x.v, pb.v, ALU.add, eng="pool" if i % 2 else "dve")
        cross_attn_blk(c, P, K, x.v, hn.v, sq, rstd.v, gc.v, wq, qn, Kt, Vt, wo, S)
        rmsnorm_fm(c, P, K, x.v, gf.v, hn.v, 8, TB, sq, rstd.v)
        for k in range(8):
            c.stt(hn32[:, k, :], x[:, k, :], gf[:, k:k + 1], rstd.v, ALU.mult, ALU.mult)
        for tt_ in range(TB // 128):
            tsl = slice(tt_ * 128, (tt_ + 1) * 128)
            ps = P.psum()
            for k in range(8):
                c.mm(ps[:, 0:NE], hn32[:, k, tsl], rt[:, k, :], start=(k == 0), stop=(k == 7))
            c.copy(lg.v, ps[:, 0:NE])
            c.max8(m8.v, lg.v)
            c.tt(w1.v, m8[:, 1:2], m8[:, 0:1], ALU.subtract)
            c.act(w1.v, w1.v, AF.Exp)
            c.ts(w1.v, w1.v, 1.0, ALU.add)
            c.recip(w1.v, w1.v)
            c.ts(w2s.v, w1.v, -1.0, ALU.mult, 1.0, ALU.add)
            c.ts(g1.v, lg.v, m8[:, 0:1], ALU.is_equal, w1[:, 0:1], ALU.mult)
            c.ts(g2.v, lg.v, m8[:, 1:2], ALU.is_equal, w2s[:, 0:1], ALU.mult)
            c.tt(g1.v, g1.v, g2.v, ALU.add)
            pst = P.psum()
            c.tr(pst[0:NE, 0:128], g1.v, identf.v)
            c.copy(gT[:, tsl], pst[0:NE, 0:128], eng="act")
        for e in range(NE):
            psg = P.psum()
            c.mm(psg.v, sel[:, e * 128:(e + 1) * 128], gT.v)
            c.copy(gb.v, psg.v, eng="act")
            for p0 in range(0, FE, 512):
                def out_gate(ci, ps):
                    c.act(sg[:, ci, :], ps, AF.Silu)

                def out_up(ci, ps, p0=p0):
                    c.tt(hff[:, p0 // 128 + ci, :], ps, sg[:, ci, :], ALU.mult)
                gemm(c, P, w13[e], 8, (FE + p0, 512), lambda k: hn[:, k, :], out_gate, TB)
                gemm(c, P, w13[e], 8, (p0, 512), lambda k: hn[:, k, :], out_up, TB)

            def out_dn(ci, ps):
                c.tt(tmp.v, ps, gb.v, ALU.mult)
                c.tt(x[:, ci, :], tmp.v, x[:, ci, :], ALU.add, eng="pool")
            gemm(c, P, w2[e], KF, (0, 1024), lambda k: hff[:, k, :], out_dn, TB)
        c.dma(outT[:, ts_].rearrange("(k p) t -> p k t", p=128), x.v, q="act")
    return c


def bcm(v, n):
    shp = [v.ap.shape[0], n] + list(v.ap.shape[1:])
    return V(v.tile, v.ap.unsqueeze(1).to_broadcast(shp))


def build_stage_b(S=8192, NQ=4096, dbg=False, lim=99, nqt_lim=None, c0=None, io=None, att_dst=None,
                  kv_save=None, kv_load=None):
    from contextlib import ExitStack
    c = c0 or Ctx()

    def IN(name, shape, dt):
        if io is not None and name in io:
            ap = io[name]
            assert list(ap.shape) == list(shape), (name, ap.shape, shape)
            return ap
        assert io is None, name
        return c.din(name, shape, dt)

    def OUT(name, shape, dt):
        if io is not None and name in io:
            ap = io[name]
            assert list(ap.shape) == list(shape), (name, ap.shape, shape)
            return ap
        assert io is None, name
        return c.dout(name, shape, dt)
    NKT = S // 128
    NQT = NQ // 128
    xT = IN("xT", [1024, S], F32)
    xqT = IN("xqT", [1024, NQ], F32)
    tq_d = IN("tq", [128, NQT], F32)
    tqb_d = IN("tqb", [128, NQ], F32)
    gmix_d = IN("gmix", [128, 8], F32)
    wq_d = IN("wq", [1024, 512], BF16)
    wkv_d = IN("wkv", [1024, 768], BF16)
    wg_d = IN("wg", [1024, 12], BF16)
    gateb_d = IN("gateb", [12, 1], F32)
    qnorm_d = IN("qnorm", [128, 1], F32)
    knorm_d = IN("knorm", [128, 3], F32)
    posT_d = IN("posT", [128, 32], BF16)
    ckw1_d = IN("ckw1", [4096, 256], BF16)
    ckw2_d = IN("ckw2", [256, 128], BF16)
    cvw1_d = IN("cvw1", [4096, 256], BF16)
    cvw2_d = IN("cvw2", [256, 128], BF16)
    identb_d = IN("identb", [128, 128], BF16)
    E_d = IN("E", [128, S], BF16)
    ov_d = IN("ov", [128, 4, 129], BF16)
    sel12_d = IN("sel12", [12, 12 * 128], BF16)
    keypos_d = IN("keypos", [128, NKT], F32)
    cmpend_d = IN("cmpend", [128, 4], F32)
    j64_d = IN("j64", [128, 128], F32)
    e0_d = IN("e0", [128, 128], F32)
    attT = OUT("attT", [4, 128, NQ], BF16) if att_dst is None else None
    scale = 128 ** -0.5

    def dump(name, v, shape, dt):
        if dbg:
            o = c.dout("dbg_" + name, shape, dt)
            c.dma(o, v, q="sp")

    P = Pools(c, nps=3, nw=1, wcols=16)
    K = load_consts(c)
    psO = c.ps([128, 512], F32, name="psO")
    psD = c.ps([128, 512], F32, name="psD")
    psM = [c.ps([128, 512], F32, name=f"psM{i}") for i in range(2)]
    psX = c.ps([128, 512], F32, name="psX")

    def ld(name, d, shape, dt, q="sp"):
        t = c.sb(shape, dt, name=name)
        c.dma(t.v, d, q=q)
        return t
    gm = ld("gm", gmix_d, [128, 8], F32)
    gateb = ld("gateb", gateb_d, [12, 1], F32)
    qnorm = ld("qnorm", qnorm_d, [128, 1], F32)
    knorm = ld("knorm", knorm_d, [128, 3], F32)
    posT = ld("posT", posT_d, [128, 32], BF16)
    identb = ld("identb", identb_d, [128, 128], BF16)
    ov = ld("ov", ov_d, [128, 4, 129], BF16)
    sel12 = ld("sel12", sel12_d, [12, 12 * 128], BF16)
    keypos = ld("keypos", keypos_d, [128, NKT], F32)
    cmpend = ld("cmpend", cmpend_d, [128, 4], F32)
    j64 = ld("j64", j64_d, [128, 128], F32)
    e0 = ld("e0", e0_d, [128, 128], F32)
    tq = ld("tq", tq_d, [128, NQT], F32)
    wq = c.sb([128, 8, 512], BF16, name="wq"); c.dma(wq.v, wq_d.rearrange("(k p) n -> p k n", p=128))
    wkv = c.sb([128, 8, 768], BF16, name="wkv"); c.dma(wkv.v, wkv_d.rearrange("(k p) n -> p k n", p=128))
    wg = c.sb([128, 8, 12], BF16, name="wg"); c.dma(wg.v, wg_d.rearrange("(k p) n -> p k n", p=128))
    ckw2 = c.sb([128, 2, 128], BF16, name="ckw2"); c.dma(ckw2.v, ckw2_d.rearrange("(k p) n -> p k n", p=128))
    cvw2 = c.sb([128, 2, 128], BF16, name="cvw2"); c.dma(cvw2.v, cvw2_d.rearrange("(k p) n -> p k n", p=128))

    ksnT = c.sb([128, S], BF16, name="ksnT")
    kwnT = c.sb([128, S], BF16, name="kwnT")
    vs_tm = c.sb([128, NKT, 128], BF16, name="vs_tm")
    vw_tm = c.sb([128, NKT, 128], BF16, name="vw_tm")
    qnT = c.sb([128, 4, NQ], BF16, name="qnT")
    Gs = c.sb([12, NQ], BF16, name="Gs")
    kcmpT = c.sb([128, 512], BF16, name="kcmpT")
    vcmp = c.sb([128, 4, 128], BF16, name="vcmp")
    rstd = c.sb([128, TB], F32, name="rstd")
    rstdn = c.sb([128, TB], F32, name="rstdn")
    tmpf = c.sb([128, TB], F32, name="tmpf")
    tmpb = c.sb([128, TB], BF16, name="tmpb")

    with ExitStack() as st:
        kcT = c.sb_scoped(st, [128, S + 16], BF16, name="kcT")
        vcT = c.sb_scoped(st, [128, S + 16], BF16, name="vcT")
        st2 = ExitStack()
        x = c.sb_scoped(st2, [128, 8, TB], F32, name="x")
        hn = c.sb_scoped(st2, [128, 8, TB], BF16, name="hn")
        sq = c.sb_scoped(st2, [128, 8, TB], BF16, name="sq")
        c.memset(kcT[:, S:S + 16], 0.0)
        c.memset(vcT[:, S:S + 16], 0.0)
        qflat = qnT.h[:].rearrange("p a b -> p (a b)")
        xB = [x.v, V(qnT, qflat[:, 0:8192].bitcast(F32).rearrange("p (k t) -> p k t", k=8))]
        hB = [hn.v, V(qnT, qflat[:, 8192:12288].rearrange("p (k t) -> p k t", k=8))]
        sB = [sq.v, V(qnT, qflat[:, 12288:16384].rearrange("p (k t) -> p k t", k=8))]

        def pre1(blk):
            bs_ = slice(blk * TB, (blk + 1) * TB)
            c.dma(xB[blk % 2], xT[:, bs_].rearrange("(k p) t -> p k t", p=128), q="sp")
            rmsnorm_fm(c, P, K, xB[blk % 2], gm.v, hB[blk % 2], 8, TB, sB[blk % 2], rstdn.v)
        if kv_load is None:
            pre1(0)
        for blk in range(S // TB if kv_load is None else 0):
            bs = slice(blk * TB, (blk + 1) * TB)
            if blk + 1 < S // TB:
                pre1(blk + 1)
            hn = hB[blk % 2]
            for j in range(4):
                ps = P.psum()
                for k in range(8):
                    c.mm(ps.v, wkv[:, k, j * 128:(j + 1) * 128], hn[:, k, :], start=(k == 0), stop=(k == 7))
                if j == 0:
                    c.copy(kcT[:, bs], ps.v, eng="act")
                elif j == 1:
                    c.copy(vcT[:, bs], ps.v, eng="act")
                else:
                    dst = ksnT if j == 2 else kwnT
                    c.copy(tmpf.v, ps.v, eng="act")
                    c.act(tmpb.v, ps.v, AF.Square)
                    ps2 = P.psum()
                    c.mm(ps2.v, K["ones_mean128"].v, tmpb.v)
                    c.act(rstd.v, ps2.v, AF.Ln, bias=K["eps"].v)
                    c.act(rstd.v, rstd.v, AF.Exp, scale=-0.5)
                    c.stt(dst[:, bs], tmpf.v, knorm[:, j - 1:j], rstd.v, ALU.mult, ALU.mult)
            for tt_ in range(4):
                tsl = slice(tt_ * 128, (tt_ + 1) * 128)
                ps = P.psum()
                for k in range(8):
                    c.mm(ps[:, 0:256], hn[:, k, tsl], wkv[:, k, 512:768], start=(k == 0), stop=(k == 7))
                kt = blk * 4 + tt_
                c.copy(vs_tm[:, kt, :], ps[:, 0:128], eng="act")
                c.copy(vw_tm[:, kt, :], ps[:, 128:256])
        hn = hB[0]
        for blk in range(NQ // TB if lim >= 2 else 0):
            bs = slice(blk * TB, (blk + 1) * TB)
            c.dma(x.v, xqT[:, bs].rearrange("(k p) t -> p k t", p=128), q="sp")
            rmsnorm_fm(c, P, K, x.v, gm.v, hn, 8, TB, sq.v, rstd.v)
            for j in range(4):
                ps = P.psum()
                for k in range(8):
                    c.mm(ps.v, wq[:, k, j * 128:(j + 1) * 128], hn[:, k, :], start=(k == 0), stop=(k == 7))
                c.copy(tmpf.v, ps.v, eng="act")
                c.act(tmpb.v, ps.v, AF.Square)
                ps2 = P.psum()
                c.mm(ps2.v, K["ones_mean128"].v, tmpb.v)
                c.act(rstd.v, ps2.v, AF.Ln, bias=K["eps"].v)
                c.act(rstd.v, rstd.v, AF.Exp, scale=-0.5)
                c.stt(qnT[:, j, bs], tmpf.v, qnorm[:, 0:1], rstd.v, ALU.mult, ALU.mult)
            ps = P.psum()
            for k in range(8):
                c.mm(ps[0:12, :], wg[:, k, :], hn[:, k, :], start=(k == 0), stop=(k == 7))
            c.act(Gs[:, bs], ps[0:12, :], AF.Sigmoid, bias=gateb.v)
        c.barrier()
        st2.close()
        w1 = c.sb_scoped(st, [128, 32, 256], BF16, name="w1")
        hk = c.sb_scoped(st, [128, 2, 512], BF16, name="hk")
        pb = c.sb_scoped(st, [128, 2], F32, name="pb")
        for which in range(2):
            src = kcT if which == 0 else vcT
            if lim < 3 or kv_load is not None:
                break
            w1src = (ckw1_d if which == 0 else cvw1_d).rearrange("(l p) h -> p l h", p=128)
            for l4 in range(4):
                c.dma(w1[:, l4 * 8:(l4 + 1) * 8, :], w1src[:, l4 * 8:(l4 + 1) * 8, :], q="sp")
            srcv = src.v.re("p (n s) -> p n s", s=16)
            for hc in range(2):
                ps = P.psum()
                for l in range(32):
                    c.mm(ps[:, 0:1], w1[:, l, hc * 128:(hc + 1) * 128], posT[:, l:l + 1], start=(l == 0), stop=(l == 31))
                c.copy(pb[:, hc:hc + 1], ps[:, 0:1])
            for hc in range(2):
                ps = P.psum()
                for l in range(32):
                    c.mm(ps.v, w1[:, l, hc * 128:(hc + 1) * 128], srcv[:, l // 16:l // 16 + 512, l % 16],
                         start=(l == 0), stop=(l == 31))
                c.act(hk[:, hc, :], ps.v, AF.Gelu, bias=pb[:, hc:hc + 1])
            if which == 0:
                ps = P.psum()
                for hc in range(2):
                    c.mm(ps.v, ckw2[:, hc, :], hk[:, hc, :], start=(hc == 0), stop=(hc == 1))
                c.copy(tmpf.v, ps.v, eng="act")
                c.act(tmpb.v, ps.v, AF.Square)
                ps2 = P.psum()
                c.mm(ps2.v, K["ones_mean128"].v, tmpb.v)
                c.act(rstd.v, ps2.v, AF.Ln, bias=K["eps"].v)
                c.act(rstd.v, rstd.v, AF.Exp, scale=-0.5)
                c.stt(kcmpT.v, tmpf.v, knorm[:, 0:1], rstd.v, ALU.mult, ALU.mult)
            else:
                for nt in range(4):
                    ps = P.psum()
                    for hc in range(2):
                        c.mm(ps[:, 0:128], hk[:, hc, nt * 128:(nt + 1) * 128], cvw2[:, hc, :], start=(hc == 0), stop=(hc == 1))
                    c.copy(vcmp[:, nt, :], ps[:, 0:128], eng="act")
        kvt = dict(ksnT=ksnT, kwnT=kwnT, vs_tm=vs_tm, vw_tm=vw_tm, kcmpT=kcmpT, vcmp=vcmp)
        if kv_load is not None:
            for i_, (nm, t_) in enumerate(kvt.items()):
                c.dma(t_.v, kv_load[nm], q="sp" if i_ % 2 == 0 else "act")
        if kv_save is not None:
            for i_, (nm, t_) in enumerate(kvt.items()):
                c.dma(kv_save[nm], t_.v, q="sp" if i_ % 2 == 0 else "act")
        dump("kcmpT", kcmpT.v, [128, 512], BF16)
        dump("vcmp", vcmp.v, [128, 4, 128], BF16)
        dump("ksnT", ksnT.v, [128, S], BF16)
        dump("qnT", qnT.v, [128, 4, NQ], BF16)
        dump("Gs", Gs.v, [12, NQ], BF16)
        dump("vw_tm", vw_tm.v, [128, NKT, 128], BF16)
        c.barrier()

    E = ld("E", E_d, [128, S], BF16)
    tqb = ld("tqb", tqb_d, [128, NQ], F32, q="act")
    eC = c.sb([128, 4, 512], BF16, name="eC")
    NR = 4
    eS = [c.sb([128, 512], BF16, name=f"eS{i}") for i in range(NR)]
    pS = [c.sb([128, 512], BF16, name=f"pS{i}") for i in range(NR)]
    mk = [c.sb([128, 128], F32, name=f"mk{i}") for i in range(6)]
    mk2 = [c.sb([128, 128], F32, name=f"mkb{i}") for i in range(6)]
    mkb = [c.sb([128, 128], BF16, name=f"mkc{i}") for i in range(6)]
    imp = c.sb([128, 128], F32, name="imp")
    rec1 = [c.sb([128, 1], F32, name=f"rec1{i}") for i in range(2)]
    nd = c.sb([128, 128], F32, name="nd")
    vv = c.sb([128, 128], F32, name="vv")
    ff = c.sb([128, 128], F32, name="ff")
    sc = c.sb([128, 128], F32, name="sc")
    sc2 = c.sb([128, 128], F32, name="sc2")
    m8a = c.sb([128, 8], F32, name="m8a")
    m8b = c.sb([128, 8], F32, name="m8b")
    selb = c.sb([128, 128], BF16, name="selb")
    selT = c.sb([128, 128], BF16, name="selT")
    rec = c.sb([128, 512], F32, name="rec")
    oacc = c.sb([128, 512], F32, name="oacc")
    otmp = c.sb([128, 512], F32, name="otmp")
    osb = c.sb([128, 512], F32, name="osb")
    obf = [c.sb([128, 512], BF16, name=f"obf{i}") for i in range(2)]
    XT = V(psX, psX.h[:].bitcast(BF16))
    cnt = [0, 0]

    def pipe(n, front, back, depth=2):
        for i in range(min(depth, n)):
            front(i)
        for i in range(n):
            back(i)
            if i + depth < n:
                front(i + depth)

    def finish_branch(k, br, first):
        qs = slice(k * 128, (k + 1) * 128)
        for r in range(4):
            cidx = r * 3 + br
            c.mm(psX[:, r * 128:(r + 1) * 128], sel12[:, cidx * 128:(cidx + 1) * 128], Gs[:, qs])
        c.copy(osb.v, psO.v, eng="act")
        if br == 0 and k == 0:
            c.ts(rec.v, psD.v, 1e-30, ALU.max)
            c.act(rec.v, rec.v, AF.Ln)
        else:
            c.act(rec.v, psD.v, AF.Ln)
        c.act(rec.v, rec.v, AF.Exp, scale=-1.0)
        c.tt(rec.v, rec.v, psX.v, ALU.mult)
        if first:
            c.tt(oacc.v, osb.v, rec.v, ALU.mult)
        else:
            c.tt(otmp.v, osb.v, rec.v, ALU.mult)
            c.tt(oacc.v, oacc.v, otmp.v, ALU.add, eng="pool")

    for k in range(NQT if lim >= 4 else 0):
        if nqt_lim is not None and k >= nqt_lim:
            break
        qs = slice(k * 128, (k + 1) * 128)
        Q = qnT[:, :, qs]
        tqk = tq[:, k:k + 1]

        def cfront(nt):
            ps = P.psum()
            c.mm(ps.v.re("p (r q) -> p r q", r=4), kcmpT[:, nt * 128:(nt + 1) * 128], Q)
            c.act(eC[:, nt, :], ps.v, AF.Exp, scale=scale)
            m = mk[nt % 6]
            c.ts(m.v, tqb[:, qs], cmpend[:, nt:nt + 1], ALU.is_ge)
            c.tt(eC[:, nt, :].re("p (r q) -> p r q", r=4), eC[:, nt, :].re("p (r q) -> p r q", r=4), bcm(m.v, 4), ALU.mult)

        def cback(nt):
            c.mm(psO.v, vcmp[:, nt, :], eC[:, nt, :], start=(nt == 0), stop=(nt == 3))
            c.mm(psD.v, K["ones"].v, eC[:, nt, :], start=(nt == 0), stop=(nt == 3))
        pipe(4, cfront, cback)
        for r in range(4):
            psI = P.psum()
            for nt in range(4):
                c.mm(psI[:, 0:129], eC[:, nt, r * 128:(r + 1) * 128], ov[:, nt, :], start=(nt == 0), stop=(nt == 3))
            r1 = rec1[r % 2]
            c.ts(r1.v, psI[:, 128:129], 1e-30, ALU.max)
            c.recip(r1.v, r1.v)
            if r == 0:
                c.ts(imp.v, psI[:, 0:128], r1[:, 0:1], ALU.mult)
            else:
                c.stt(imp.v, psI[:, 0:128], r1[:, 0:1], imp.v, ALU.mult, ALU.add)
        finish_branch(k, 0, True)
        c.ts(nd.v, j64.v, tqk, ALU.subtract)
        c.ts(vv.v, nd.v, 0.0, ALU.is_le)
        c.ts(ff.v, nd.v, -128.0, ALU.is_gt)
        c.tt(ff.v, ff.v, vv.v, ALU.mult)
        c.tt(ff.v, ff.v, e0.v, ALU.add)
        c.stt(sc.v, ff.v, 100.0, imp.v, ALU.mult, ALU.add)
        c.tt(sc.v, sc.v, vv.v, ALU.mult)
        c.ts(ff.v, vv.v, -1.0, ALU.add)
        c.tt(sc.v, sc.v, ff.v, ALU.add)
        c.max8(m8a.v, sc.v)
        c.match_replace(sc2.v, m8a.v, sc.v, -2.0)
        c.max8(m8b.v, sc2.v)
        c.ts(sc2.v, sc.v, m8b[:, 7:8], ALU.is_ge)
        c.tt(selb.v, sc2.v, vv.v, ALU.mult)
        c.tr(XT[:, 0:128], selb.v, identb.v)
        c.copy(selT.v, XT[:, 0:128], eng="act")
        if dbg and k in (1, 20):
            dump(f"imp{k}", imp.v, [128, 128], F32)
            dump(f"sel{k}", selb.v, [128, 128], BF16)
        wkts = [kt for kt in range(2 * k - 4, 2 * k + 2) if kt >= 0]
        nkt = 2 * k + 2
        items = [("win", kt, ii, len(wkts)) for ii, kt in enumerate(wkts)] + \
                [("slc", kt, kt, nkt) for kt in range(nkt)]
        bufs = {}

        def front(i):
            kind, kt, ii, nn = items[i]
            ks_ = slice(kt * 128, (kt + 1) * 128)
            b3 = cnt[0] % NR
            cnt[0] += 1
            bufs[i] = b3
            ps = P.psum()
            c.mm(ps.v.re("p (r q) -> p r q", r=4), (kwnT if kind == "win" else ksnT)[:, ks_], Q)
            c.act(eS[b3].v, ps.v, AF.Exp, scale=scale)
            m3 = cnt[1] % 6
            cnt[1] += 1
            if kind == "win":
                m, m2, mb = mk[m3], mk2[m3], mkb[m3]
                c.ts(m.v, tqb[:, qs], keypos[:, kt:kt + 1], ALU.subtract)
                c.ts(m2.v, m.v, 0.0, ALU.is_ge)
                c.ts(m.v, m.v, 512.0, ALU.is_lt)
                c.tt(mb.v, m.v, m2.v, ALU.mult)
                msk = mb.v
            else:
                pm = psM[m3 % 2]
                c.mm(pm[:, 0:128], E[:, ks_], selT.v)
                if kt >= 2 * k:
                    m = mk[m3]
                    c.ts(m.v, tqb[:, qs], keypos[:, kt:kt + 1], ALU.is_ge)
                    c.tt(m.v, m.v, pm[:, 0:128], ALU.mult)
                    msk = m.v
                else:
                    msk = pm[:, 0:128]
            c.tt(pS[b3].v.re("p (r q) -> p r q", r=4), eS[b3].v.re("p (r q) -> p r q", r=4), bcm(msk, 4), ALU.mult)

        def back(i):
            kind, kt, ii, nn = items[i]
            b3 = bufs[i]
            vt = vw_tm if kind == "win" else vs_tm
            c.mm(psO.v, vt[:, kt, :], pS[b3].v, start=(ii == 0), stop=(ii == nn - 1))
            c.mm(psD.v, K["ones"].v, pS[b3].v, start=(ii == 0), stop=(ii == nn - 1))
            if ii == nn - 1:
                finish_branch(k, 2 if kind == "win" else 1, False)
        pipe(len(items), front, back, depth=3)
        ob = obf[k % 2]
        c.copy(ob.v, oacc.v, eng="act")
        dst = attT.rearrange("r d q -> d r q")[:, :, qs] if att_dst is None else att_dst(k)
        c.dma(dst, ob.v.re("p (r q) -> p r q", r=4), q="act")
    return c


_CACHE = {}


def _get(name, fn, *a):
    key = (name,) + tuple(a)
    if key not in _CACHE:
        _CACHE[key] = fn(*a)
    return _CACHE[key]


def _vec8(v):
    return np.ascontiguousarray(np.asarray(v, np.float32).reshape(8, 128).T)


def _col(v):
    return np.ascontiguousarray(np.asarray(v, np.float32).reshape(128, 1))


def _bcrow(v):
    v = np.asarray(v, np.float32)
    return np.ascontiguousarray(np.broadcast_to(v[None, :], (128, v.shape[0])))


def _bfc(a):
    return np.asarray(a, dtype=np.float32).astype(NPBF)


CAST_NAMES = ["ev_w_in", "ev_rg_wa", "ev_rg_wx", "ev_cmp_pos", "ev_cmp_k_w1", "ev_cmp_k_w2", "ev_cmp_v_w1",
              "ev_cmp_v_w2", "ev_w_out", "od_w_in", "od_w_out", "x_wq", "x_wkv", "x_wo", "ff_w13", "ff_w2",
              "moe_w13", "moe_w2"]


def cast_weights(inp):
    flats = [np.asarray(inp[n], np.float32).reshape(-1) for n in CAST_NAMES]
    tot = sum(f.size for f in flats)
    per = 8 * 128
    F = (tot + per - 1) // per
    F = ((F + 7) // 8) * 8
    buf = np.zeros(per * F, np.float32)
    o = 0
    offs = {}
    for n, f in zip(CAST_NAMES, flats):
        buf[o:o + f.size] = f
        offs[n] = (o, f.size)
        o += f.size
    buf = buf.reshape(8, 128, F)
    c = _get("cast", build_cast, F)
    res = c.run([{"w32": buf[i]} for i in range(8)])
    out = np.concatenate([np.asarray(res.results[i]["w16"]).reshape(-1) for i in range(8)])
    W = {}
    for n in CAST_NAMES:
        o, sz = offs[n]
        W[n] = out[o:o + sz].reshape(np.asarray(inp[n]).shape)
    return W


def a_inputs(inp, W, b, bp):
    Wi = W["ev_w_in"][0]
    blks = [2 * bp, 2 * bp + 1]
    cols = np.concatenate([np.arange(k * 128, (k + 1) * 128) for k in blks] +
                          [1024 + np.arange(k * 128, (k + 1) * 128) for k in blks])

    def pb(v):
        v = np.asarray(v, np.float32)
        return np.ascontiguousarray(np.stack([v[k * 128:(k + 1) * 128] for k in blks], axis=1))
    cwv = np.asarray(inp["ev_rg_conv_w"][0], np.float32)
    cw = np.stack([cwv[j, k * 128:(k + 1) * 128] for k in blks for j in range(4)], axis=1)
    return dict(gmix=_vec8(inp["norm_mix"][0]), wA=np.ascontiguousarray(Wi[:, cols]),
                cw=np.ascontiguousarray(cw), cb=pb(inp["ev_rg_conv_b"][0]),
                wa=np.ascontiguousarray(np.concatenate([W["ev_rg_wa"][0][k] for k in blks], axis=1)),
                wx=np.ascontiguousarray(np.concatenate([W["ev_rg_wx"][0][k] for k in blks], axis=1)),
                ba=pb(inp["ev_rg_ba"][0]), bx=pb(inp["ev_rg_bx"][0]), lam=pb(inp["ev_rg_lambda"][0]))


def b_consts(S=8192):
    j = np.arange(128)
    n = np.arange(512)
    key = np.arange(S)
    E = (key[None, :] // 64 == j[:, None]).astype(np.float32)
    cs = n * 16
    ce = cs + 31
    ss = j * 64
    ovm = ((cs[:, None] <= ss[None, :] + 63) & (ce[:, None] >= ss[None, :])).astype(np.float32)
    ovm[511] = 0
    ov = np.concatenate([ovm, np.ones((512, 1), np.float32)], axis=1).reshape(4, 128, 129).transpose(1, 0, 2)
    sel12 = np.zeros((12, 12 * 128), np.float32)
    for i in range(12):
        sel12[i, i * 128:(i + 1) * 128] = 1
    keypos = (np.arange(64)[None, :] * 128 + np.arange(128)[:, None]).astype(np.float32)
    cmpend = (16 * (np.arange(4)[None, :] * 128 + np.arange(128)[:, None]) + 31).astype(np.float32)
    cmpend[127, 3] = 1e9
    j64 = np.broadcast_to((64 * j)[None, :], (128, 128)).astype(np.float32)
    e0 = np.zeros((128, 128), np.float32)
    e0[:, 0] = 1
    return dict(identb=_bfc(np.eye(128)), E=_bfc(E), ov=_bfc(np.ascontiguousarray(ov)), sel12=_bfc(sel12),
                keypos=np.ascontiguousarray(keypos), cmpend=np.ascontiguousarray(cmpend),
                j64=np.ascontiguousarray(j64), e0=e0)


def b_tok(qh):
    tiles = np.arange(32) * 2 + qh
    return tiles[:, None] * 128 + np.arange(128)[None, :]


def b_inputs(inp, W, xb, xbT, g, qh, consts):
    Wi = W["ev_w_in"][0]
    qcols = 2048 + np.arange(g * 512, (g + 1) * 512)
    base = 2048 + 1024

    def kvc(i):
        return base + i * 256 + np.arange(g * 128, (g + 1) * 128)
    kvcols = np.concatenate([kvc(0), kvc(1), kvc(2), kvc(4), kvc(3), kvc(5)])
    gcols = base + 6 * 256 + np.arange(g * 12, (g + 1) * 12)
    tok = b_tok(qh)
    m = dict(xT=xbT, xqT=np.ascontiguousarray(xb[tok.reshape(-1)].T),
             tq=np.ascontiguousarray(tok.T).astype(np.float32),
             tqb=np.ascontiguousarray(np.broadcast_to(tok.reshape(1, -1), (128, 4096))).astype(np.float32),
             gmix=_vec8(inp["norm_mix"][0]), wq=np.ascontiguousarray(Wi[:, qcols]),
             wkv=np.ascontiguousarray(Wi[:, kvcols]), wg=np.ascontiguousarray(Wi[:, gcols]),
             gateb=np.ascontiguousarray(np.asarray(inp["ev_nsa_gate_b"][0], np.float32)[g * 12:(g + 1) * 12].reshape(12, 1)),
             qnorm=_col(inp["ev_q_norm"][0]),
             knorm=np.ascontiguousarray(np.asarray(inp["ev_k_norm"][0], np.float32).T),
             posT=np.ascontiguousarray(W["ev_cmp_pos"][0].T), ckw1=W["ev_cmp_k_w1"][0], ckw2=W["ev_cmp_k_w2"][0],
             cvw1=W["ev_cmp_v_w1"][0], cvw2=W["ev_cmp_v_w2"][0])
    m.update(consts)
    return m


def d_consts():
    t_ = np.arange(128)
    return dict(identb=_bfc(np.eye(128)), identf=np.eye(128, dtype=np.float32),
                U=(t_[:, None] <= t_[None, :]).astype(np.float32),
                NM=np.where(t_[None, :] < t_[:, None], -30000.0, 0.0).astype(np.float32),
                onesf=np.ones((128, 128), np.float32))


def d_inputs(inp, W, x1T_b, g, consts):
    Wi = W["od_w_in"][0]
    cols = np.concatenate([np.arange(g * 512, (g + 1) * 512), 2048 + np.arange(g * 512, (g + 1) * 512),
                           4096 + np.arange(g * 128, (g + 1) * 128), 4096 + 512 + np.arange(g * 128, (g + 1) * 128)])
    dtcols = 2048 + 3072 + np.arange(g * 8, (g + 1) * 8)
    cch = [np.arange(g * 512 + k * 128, g * 512 + (k + 1) * 128) for k in range(4)] + \
          [2048 + np.arange(g * 128, (g + 1) * 128), 2048 + 512 + np.arange(g * 128, (g + 1) * 128)]
    cwv = np.asarray(inp["od_conv_w"][0], np.float32)
    cbv = np.asarray(inp["od_conv_b"][0], np.float32)
    cw = np.stack([cwv[j, ch] for ch in cch for j in range(4)], axis=1)
    cb = np.stack([cbv[ch] for ch in cch], axis=1)
    hs = slice(g * 8, (g + 1) * 8)
    gn = np.asarray(inp["od_norm"][0], np.float32)[g * 512:(g + 1) * 512].reshape(4, 128).T
    m = dict(xT=x1T_b, gmix=_vec8(inp["norm_mix"][1]), wD=np.ascontiguousarray(Wi[:, cols]),
             wdt=np.ascontiguousarray(Wi[:, dtcols]), cw=np.ascontiguousarray(cw), cb=np.ascontiguousarray(cb),
             dtb=_bcrow(np.asarray(inp["od_dt_bias"][0])[hs]), alog=_bcrow(np.asarray(inp["od_a_log"][0])[hs]),
             dsk=_bcrow(np.asarray(inp["od_d_skip"][0])[hs]), gn=np.ascontiguousarray(gn),
             wo=np.ascontiguousarray(W["od_w_out"][0][g * 512:(g + 1) * 512]))
    m.update(consts)
    return m


def kernel(**inp):
    NT = 2048
    S = 8192
    x = np.asarray(inp["x"], np.float32)
    mem = np.asarray(inp["mem"], np.float32)
    W = cast_weights(inp)
    xT = [np.ascontiguousarray(x[b].T) for b in range(2)]
    memT = [np.ascontiguousarray(mem[b].T) for b in range(2)]

    ca = _get("A", build_stage_a)
    maps = []
    for core in range(8):
        m = a_inputs(inp, W, core // 4, core % 4)
        m["xT"] = xT[core // 4]
        maps.append(m)
    ra = ca.run(maps).results
    cb_ = _get("B", build_stage_b)
    bc = b_consts()
    maps = [b_inputs(inp, W, x[core // 4], xT[core // 4], (core % 4) // 2, core % 2, bc) for core in range(8)]
    rb = cb_.run(maps).results
    mixT = [np.zeros((2048, S), NPBF) for _ in range(2)]
    for core in range(8):
        b, bp = core // 4, core % 4
        mixT[b][bp * 256:(bp + 1) * 256] = np.asarray(ra[core]["rgT"])
        g, qh = (core % 4) // 2, core % 2
        tok = b_tok(qh).reshape(-1)
        o = np.asarray(rb[core]["attT"])
        for r in range(4):
            h = g * 4 + r
            mixT[b][1024 + h * 128:1024 + (h + 1) * 128, tok] = o[r]

    cc = _get("C", build_stage_c, NT)
    maps = []
    for core in range(8):
        b, q = core // 4, core % 4
        sl = slice(q * NT, (q + 1) * NT)
        maps.append(dict(xT=np.ascontiguousarray(xT[b][:, sl]), mixT=np.ascontiguousarray(mixT[b][:, sl]),
                         memT=memT[b], w_out=W["ev_w_out"][0], wq=W["x_wq"][0], wkv=W["x_wkv"][0], wo=W["x_wo"][0],
                         w13=W["ff_w13"][0], w2=W["ff_w2"][0], norm_cross=_vec8(inp["norm_cross"][0]),
                         norm_mem=_vec8(inp["norm_mem"][0]), norm_ffn=_vec8(inp["norm_ffn"][0]),
                         qn=_col(inp["x_q_norm"][0]), kn=_col(inp["x_k_norm"][0])))
    rc = cc.run(maps).results
    x1T = [np.concatenate([np.asarray(rc[b * 4 + q]["outT"]) for q in range(4)], axis=1) for b in range(2)]

    cd = _get("D", build_stage_d)
    dc = d_consts()
    maps = [d_inputs(inp, W, x1T[core // 4], core % 4, dc) for core in range(8)]
    rd = cd.run(maps).results

    ce = _get("E", build_stage_e, NT)
    selm = np.zeros((8, 8 * 128), np.float32)
    for e in range(8):
        selm[e, e * 128:(e + 1) * 128] = 1
    maps = []
    for core in range(8):
        b, q = core // 4, core % 4
        sl = slice(q * NT, (q + 1) * NT)
        parts = np.stack([np.ascontiguousarray(np.asarray(rd[b * 4 + g]["outT"])[:, sl]) for g in range(4)])
        maps.append(dict(xT=np.ascontiguousarray(x1T[b][:, sl]), pT=parts, memT=memT[b],
                         wq=W["x_wq"][1], wkv=W["x_wkv"][1], wo=W["x_wo"][1],
                         norm_cross=_vec8(inp["norm_cross"][1]), norm_mem=_vec8(inp["norm_mem"][1]),
                         norm_ffn=_vec8(inp["norm_ffn"][1]), qn=_col(inp["x_q_norm"][1]), kn=_col(inp["x_k_norm"][1]),
                         router=np.ascontiguousarray(np.asarray(inp["moe_router"][0], np.float32)),
                         w13=W["moe_w13"][0], w2=W["moe_w2"][0], identf=np.eye(128, dtype=np.float32), sel=selm))
    re_ = ce.run(maps).results
    out = np.zeros((2, S, 1024), np.float32)
    for core in range(8):
        b, q = core // 4, core % 4
        out[b, q * NT:(q + 1) * NT] = np.asarray(re_[core]["outT"]).T
    return out


BF_NAMES = {
    "A": {"wA", "wa", "wx"},
    "B": {"wq", "wkv", "wg", "posT", "ckw1", "ckw2", "cvw1", "cvw2", "identb", "E", "ov", "sel12"},
    "C": {"w_out", "wq", "wkv", "wo", "w13", "w2"},
    "D": {"wD", "wdt", "wo", "identb"},
    "E": {"wq", "wkv", "wo", "w13", "w2"},
}
BIG_IN = {"xT", "xqT", "memT", "mixT", "pT"}


class Packer:
    def __init__(self):
        self.items = {}
        self.n = 0
        self.arrs = []

    def add(self, key, arr):
        arr = np.asarray(arr)
        if arr.dtype != np.float32:
            arr = arr.astype(np.float32)
        sz = arr.size
        self.items[key] = (self.n, tuple(arr.shape))
        self.arrs.append((self.n, arr.reshape(-1)))
        self.n += ((sz + 63) // 64) * 64

    def build(self, mult):
        tot = ((self.n + mult - 1) // mult) * mult
        buf = np.zeros(tot, np.float32)
        for o, a in self.arrs:
            buf[o:o + a.size] = a
        return buf


def _role_maps(inp):
    Wf = {n: np.asarray(inp[n], np.float32) for n in CAST_NAMES}
    roles = {}
    for bp in range(4):
        roles[f"A{bp}"] = ("A", a_inputs(inp, Wf, 0, bp))
    bc = b_consts()
    dummy_x = np.zeros((8192, 8), np.float32)
    for g in range(2):
        m = b_inputs(inp, Wf, dummy_x, None, g, 0, bc)
        for k in ("xT", "xqT", "tq", "tqb"):
            m.pop(k)
        roles[f"B{g}"] = ("B", m)
    for qh in range(2):
        tok = b_tok(qh)
        roles[f"Bq{qh}"] = ("B", dict(tq=np.ascontiguousarray(tok.T).astype(np.float32),
                                     tqb=np.ascontiguousarray(np.broadcast_to(tok.reshape(1, -1), (128, 4096))).astype(np.float32)))
    roles["C"] = ("C", dict(w_out=Wf["ev_w_out"][0], wq=Wf["x_wq"][0], wkv=Wf["x_wkv"][0], wo=Wf["x_wo"][0],
                           w13=Wf["ff_w13"][0], w2=Wf["ff_w2"][0], norm_cross=_vec8(inp["norm_cross"][0]),
                           norm_mem=_vec8(inp["norm_mem"][0]), norm_ffn=_vec8(inp["norm_ffn"][0]),
                           qn=_col(inp["x_q_norm"][0]), kn=_col(inp["x_k_norm"][0])))
    dc = d_consts()
    for g in range(4):
        m = d_inputs(inp, Wf, None, g, dc)
        m.pop("xT")
        roles[f"D{g}"] = ("D", m)
    selm = np.zeros((8, 8 * 128), np.float32)
    for e in range(8):
        selm[e, e * 128:(e + 1) * 128] = 1
    roles["E"] = ("E", dict(wq=Wf["x_wq"][1], wkv=Wf["x_wkv"][1], wo=Wf["x_wo"][1],
                           norm_cross=_vec8(inp["norm_cross"][1]), norm_mem=_vec8(inp["norm_mem"][1]),
                           norm_ffn=_vec8(inp["norm_ffn"][1]), qn=_col(inp["x_q_norm"][1]), kn=_col(inp["x_k_norm"][1]),
                           router=np.ascontiguousarray(np.asarray(inp["moe_router"][0], np.float32)),
                           w13=Wf["moe_w13"][0], w2=Wf["moe_w2"][0], identf=np.eye(128, dtype=np.float32), sel=selm))
    return roles


CAST_CH = 8192


CAST_CH2 = 2048


def pack_roles(roles):
    pw, pe, pp = Packer(), Packer(), Packer()
    for rk, (st, m) in roles.items():
        for name, arr in m.items():
            if name in BF_NAMES[st]:
                (pe if rk == "E" else pw).add(f"{rk}.{name}", arr)
            else:
                pp.add(f"{rk}.{name}", arr)
    w32 = pw.build(128 * CAST_CH)
    w32e = pe.build(128 * CAST_CH2)
    p32 = pp.build(64)
    return w32, w32e, p32, pw.items, pe.items, pp.items


def _view(flat, off, shape):
    n = int(np.prod(shape))
    v = flat[off:off + n]
    if len(shape) == 1:
        return v
    if len(shape) == 2:
        return v.rearrange("(a b) -> a b", b=shape[1])
    if len(shape) == 3:
        return v.rearrange("(a b c) -> a b c", b=shape[1], c=shape[2])
    raise ValueError(shape)


def build_fused(NW, NWE, NP, witems, eitems, pitems, S=8192, stages="ABCDE"):
    c = Ctx()
    nc = c.nc
    w32 = c.din("w32", [NW], F32)
    w32e = c.din("w32e", [NWE], F32)
    p32 = c.din("p32", [NP], F32)
    xT = c.din("xT", [1024, S], F32)
    xqT = c.din("xqT", [2, 1024, 4096], F32)
    memT = c.din("memT", [1024, 256], F32)
    outT = c.dout("outT", [1024, 2048], F32)
    w16 = nc.dram_tensor("w16_i", [NW], BF16, kind="Internal").ap()
    w16e = nc.dram_tensor("w16e_i", [NWE], BF16, kind="Internal").ap()
    mixT_i = nc.dram_tensor("mixT_i", [2048, S], BF16, kind="Internal").ap()
    x1T_i = nc.dram_tensor("x1T_i", [1024, S], F32, kind="Internal").ap()
    part_i = nc.dram_tensor("part_i", [4, 1024, 2048], F32, kind="Internal").ap()
    st_i = nc.dram_tensor("st_i", [4, 4, 128, 530], F32, kind="Internal").ap()
    qidx = nc.partition_id() % 4

    def role_io(rk, st):
        io = {}
        for key, (off, shape) in witems.items():
            r, name = key.split(".")
            if r == rk:
                io[name] = _view(w16, off, shape)
        for key, (off, shape) in eitems.items():
            r, name = key.split(".")
            if r == rk:
                io[name] = _view(w16e, off, shape)
        for key, (off, shape) in pitems.items():
            r, name = key.split(".")
            if r == rk:
                io[name] = _view(p32, off, shape)
        return io

    F = NW // 128
    with c.scope():
        w32v = w32.rearrange("(p f) -> p f", p=128)
        w16v = w16.rearrange("(p f) -> p f", p=128)
        a = [c.sb([128, CAST_CH], F32, name=f"ca{i}") for i in range(3)]
        b = [c.sb([128, CAST_CH], BF16, name=f"cb{i}") for i in range(3)]
        engs = ["dve", "pool", "act"]
        for i, p in enumerate(range(0, F, CAST_CH)):
            ta, tb_ = a[i % 3], b[i % 3]
            c.dma(ta.v, w32v[:, p:p + CAST_CH], q="sp")
            c.copy(tb_.v, ta.v, eng=engs[i % 3])
            c.dma(w16v[:, p:p + CAST_CH], tb_.v, q="sp")
    if "A" in stages:
        for bp in range(4):
            with c.scope():
                io = role_io(f"A{bp}", "A")
                io["xT"] = xT
                io["rgT"] = mixT_i[bp * 256:(bp + 1) * 256, :]
                build_stage_a(c0=c, io=io)
    if "B" in stages:
        kvshapes = dict(ksnT=[128, S], kwnT=[128, S], vs_tm=[128, S // 128, 128], vw_tm=[128, S // 128, 128],
                        kcmpT=[128, 512], vcmp=[128, 4, 128])
        kv_i = {nm: nc.dram_tensor("kv_" + nm, sh, BF16, kind="Internal").ap() for nm, sh in kvshapes.items()}
        for g in range(2):
            for qh in range(2):
                with c.scope():
                    io = role_io(f"B{g}", "B")
                    io.update(role_io(f"Bq{qh}", "B"))
                    io["xT"] = xT
                    io["xqT"] = xqT[qh]

                    def att_dst(k, g=g, qh=qh):
                        gt = 2 * k + qh
                        return mixT_i[1024 + g * 512:1024 + (g + 1) * 512, gt * 128:(gt + 1) * 128].rearrange(
                            "(r d) q -> d r q", d=128)
                    build_stage_b(c0=c, io=io, att_dst=att_dst, kv_save=kv_i if qh == 0 else None,
                                  kv_load=kv_i if qh == 1 else None)
    if "C" in stages:
        for qq in range(4):
            with c.scope():
                io = role_io("C", "C")
                sl = slice(qq * 2048, (qq + 1) * 2048)
                io["xT"] = xT[:, sl]
                io["mixT"] = mixT_i[:, sl]
                io["memT"] = memT
                io["outT"] = x1T_i[:, sl]
                build_stage_c(c0=c, io=io)
    if "D" in stages:
        with c.scope():
            FE_ = NWE // 128
            srcv = w32e.rearrange("(p f) -> p f", p=128)
            dstv = w16e.rearrange("(p f) -> p f", p=128)
            NB_ = 3
            ca = [c.sb([128, CAST_CH2], F32, name=f"cea{i}") for i in range(NB_)]
            cb_ = [c.sb([128, CAST_CH2], BF16, name=f"ceb{i}") for i in range(NB_)]
            nsteps = FE_ // CAST_CH2

            def cast_gen():
                def load(j):
                    if j < nsteps:
                        c.dma(ca[j % NB_].v, srcv[:, j * CAST_CH2:(j + 1) * CAST_CH2], q="sp")

                def store(j):
                    if 0 <= j < nsteps:
                        c.dma(dstv[:, j * CAST_CH2:(j + 1) * CAST_CH2], cb_[j % NB_].v, q="sp")
                load(0)
                load(1)
                for j in range(nsteps):
                    store(j - 1)
                    load(j + 2)
                    c.copy(cb_[j % NB_].v, ca[j % NB_].v, eng="pool")
                    yield
                store(nsteps - 1)
            gen = cast_gen()

            def hook():
                for _ in range(2):
                    next(gen, None)
            for g in range(4):
                with c.scope():
                    io = role_io(f"D{g}", "D")
                    io["xT"] = x1T_i
                    build_stage_d(c0=c, io=io, mode="state", st_d=st_i[g], hook=hook)
                with c.scope():
                    io = role_io(f"D{g}", "D")
                    io["xT"] = x1T_i
                    io["outT"] = part_i[g]
                    build_stage_d(c0=c, io=io, mode="own", st_d=st_i[g], tok0=qidx * 2048, qidx=qidx, hook=hook)
            for _ in gen:
                pass
    if "E" in stages:
        with c.scope():
            io = role_io("E", "E")
            io["xT"] = x1T_i
            io["pT"] = part_i
            io["memT"] = memT
            io["outT"] = outT
            build_stage_e(c0=c, io=io, tok0=qidx * 2048)
    return c


def kernel_fused(**inp):
    S = 8192
    x = np.asarray(inp["x"], np.float32)
    mem = np.asarray(inp["mem"], np.float32)
    roles = _role_maps(inp)
    w32, w32e, p32, witems, eitems, pitems = pack_roles(roles)
    key = ("F", w32.size, w32e.size, p32.size, tuple(sorted(witems.items())), tuple(sorted(eitems.items())),
           tuple(sorted(pitems.items())))
    if key not in _CACHE:
        _CACHE[key] = build_fused(w32.size, w32e.size, p32.size, witems, eitems, pitems)
    c = _CACHE[key]
    maps = []
    for b in range(2):
        xTb = np.ascontiguousarray(x[b].T)
        xq = np.stack([np.ascontiguousarray(x[b][b_tok(qh).reshape(-1)].T) for qh in range(2)])
        memTb = np.ascontiguousarray(mem[b].T)
        for q in range(4):
            maps.append(dict(w32=w32, w32e=w32e, p32=p32, xT=xTb, xqT=xq, memT=memTb))
    res = c.run(maps).results
    out = np.zeros((2, S, 1024), np.float32)
    for core in range(8):
        b, q = core // 4, core % 4
        out[b, q * 2048:(q + 1) * 2048] = np.asarray(res[core]["outT"]).T
    return out


kernel_unfused = kernel
kernel = kernel_fused
```

```python
import numpy as np
import ml_dtypes
import concourse.bass as bass
import concourse.mybir as mybir
from concourse.bass_utils import run_bass_kernel_spmd

F32 = mybir.dt.float32
BF16 = mybir.dt.bfloat16
I32 = mybir.dt.int32
AF = mybir.ActivationFunctionType
ALU = mybir.AluOpType
AX = mybir.AxisListType
NPBF = ml_dtypes.bfloat16

SEM_ROLL = 30000
NUM_DEV = None


class V:
    __slots__ = ("tile", "ap")

    def __init__(self, tile, ap):
        self.tile = tile
        self.ap = ap

    def __getitem__(self, idx):
        return V(self.tile, self.ap[idx])

    def re(self, pat, **kw):
        return V(self.tile, self.ap.rearrange(pat, **kw))

    def bc(self, shape):
        return V(self.tile, self.ap.to_broadcast(shape))


class Tile:
    def __init__(self, ctx, h, name, space):
        self.ctx = ctx
        self.h = h
        self.name = name
        self.space = space
        self.last_w = None
        self.reads = []
        self.dsem = None
        self.dcnt = 0

    def __getitem__(self, idx):
        return V(self, self.h[idx])

    @property
    def v(self):
        return V(self, self.h[:])


class Eng:
    def __init__(self, ctx, name, h):
        self.ctx = ctx
        self.name = name
        self.h = h
        self.sem = None
        self.cnt = 0
        self.waited = {}
        self.n = 0

    def newsem(self):
        self.sem = self.ctx.nc.alloc_semaphore(f"s_{self.name}_{self.ctx.nsem}")
        self.ctx.nsem += 1
        self.ctx.sems[id(self.sem)] = self.sem
        self.cnt = 0


class Ctx:
    def __init__(self):
        self.nc = bass.Bass("TRN2", target_bir_lowering=False, num_devices=NUM_DEV)
        nc = self.nc
        self.nsem = 0
        self.sems = {}
        self.E = {
            "pe": Eng(self, "pe", nc.tensor),
            "act": Eng(self, "act", nc.scalar),
            "dve": Eng(self, "dve", nc.vector),
            "pool": Eng(self, "pool", nc.gpsimd),
            "sp": Eng(self, "sp", nc.sync),
        }
        for e in self.E.values():
            e.newsem()
        self.ntile = 0
        self._all_tiles = []
        self._scopes = []
        self.free_dsems = []
        self.out_tiles = []
        self.sb_bytes = 0

    def sb(self, shape, dt, name=None):
        name = f"sb{self.ntile}_{name or 't'}"
        self.ntile += 1
        if self._scopes:
            h = self._scopes[-1][0].enter_context(self.nc.sbuf_tensor(name, list(shape), dt))
        else:
            h = self.nc.alloc_sbuf_tensor(name, list(shape), dt)
        t = Tile(self, h, name, "sb")
        self._all_tiles.append(t)
        if self._scopes:
            self._scopes[-1][1].append(t)
        return t

    def scope(self):
        from contextlib import contextmanager, ExitStack

        @contextmanager
        def _cm():
            st = ExitStack()
            tiles = []
            self._scopes.append((st, tiles))
            try:
                yield
            finally:
                self.barrier()
                self._scopes.pop()
                st.close()
                for t in tiles:
                    if t.dsem is not None:
                        self.free_dsems.append((t.dsem, t.dcnt))
                        t.dsem = None
                dead = set(id(t) for t in tiles)
                self._all_tiles = [t for t in self._all_tiles if id(t) not in dead]
                self.out_tiles = [t for t in self.out_tiles if id(t) not in dead]
        return _cm()

    def sb_scoped(self, stack, shape, dt, name=None):
        name = f"sb{self.ntile}_{name or 't'}"
        self.ntile += 1
        h = stack.enter_context(self.nc.sbuf_tensor(name, list(shape), dt))
        t = Tile(self, h, name, "sb")
        self._all_tiles.append(t)
        return t

    def ps(self, shape, dt=F32, name=None):
        name = f"ps{self.ntile}_{name or 'p'}"
        self.ntile += 1
        if self._scopes:
            h = self._scopes[-1][0].enter_context(self.nc.psum_tensor(name, list(shape), dt))
        else:
            h = self.nc.alloc_psum_tensor(name, list(shape), dt)
        return Tile(self, h, name, "ps")

    def din(self, name, shape, dt):
        return self.nc.dram_tensor(name, list(shape), dt, kind="ExternalInput").ap()

    def dout(self, name, shape, dt):
        return self.nc.dram_tensor(name, list(shape), dt, kind="ExternalOutput").ap()

    def _wait(self, eng, ev):
        sem_id, val = ev[0], ev[1]
        if eng.waited.get(sem_id, 0) >= val:
            return
        eng.h.wait_ge(self.sems[sem_id], val)
        eng.waited[sem_id] = val

    def _sync(self, eng, reads, writes, is_mm=False):
        for t in reads:
            if t.last_w is not None:
                self._wait(eng, t.last_w)
            if t.space == "ps":
                for r in t.reads:
                    if r[2] != eng.name:
                        self._wait(eng, r)
        for t in writes:
            if t.last_w is not None:
                lw = t.last_w
                if not (is_mm and lw[3] and lw[2] == eng.name):
                    if lw[2] != eng.name or True:
                        self._wait(eng, lw)
            for r in t.reads:
                if is_mm and r[2] == eng.name:
                    continue
                self._wait(eng, r)

    def _done(self, eng, ins, reads, writes, is_mm=False):
        if eng.cnt >= SEM_ROLL:
            eng.newsem()
        eng.cnt += 1
        eng.n += 1
        ins.then_inc(eng.sem, 1)
        ev = (id(eng.sem), eng.cnt, eng.name, is_mm)
        for t in writes:
            t.last_w = ev
            t.reads = []
        for t in reads:
            if t not in writes:
                t.reads.append(ev)
                if len(t.reads) > 12:
                    best = {}
                    for r in t.reads:
                        if r[0] not in best or best[r[0]][1] < r[1]:
                            best[r[0]] = r
                    t.reads = list(best.values())

    def _op(self, engname, fn, reads, writes, is_mm=False):
        eng = self.E[engname]
        reads = [v.tile for v in reads if isinstance(v, V)]
        writes = [v.tile for v in writes if isinstance(v, V)]
        self._sync(eng, reads, writes, is_mm)
        ins = fn(eng.h)
        self._done(eng, ins, reads, writes, is_mm)

    def dma(self, out, in_, q="sp", **kw):
        eng = self.E[q]
        if isinstance(out, V):
            t = out.tile
            lw = t.last_w
            if lw is not None and lw[2] == "dma":
                t.last_w = None
            self._sync(eng, [], [t])
            t.last_w = lw
            o, i = out.ap, in_
            wr = True
        else:
            t = in_.tile
            self._sync(eng, [t], [])
            o, i = out, in_.ap
            wr = False
        if t.dsem is None:
            while self.free_dsems and self.free_dsems[-1][1] >= SEM_ROLL - 2000:
                self.free_dsems.pop()
            if self.free_dsems:
                t.dsem, t.dcnt = self.free_dsems.pop()
            else:
                t.dsem = self.nc.alloc_semaphore(f"d_{t.name}")
                self.sems[id(t.dsem)] = t.dsem
        if t.dcnt >= SEM_ROLL:
            self._wait(eng, (id(t.dsem), t.dcnt))
            t.dsem = self.nc.alloc_semaphore(f"d_{t.name}_{self.nsem}")
            self.nsem += 1
            self.sems[id(t.dsem)] = t.dsem
            t.dcnt = 0
        ins = eng.h.dma_start(out=o, in_=i, **kw)
        t.dcnt += 16
        ins.then_inc(t.dsem, 16)
        ev = (id(t.dsem), t.dcnt, "dma", False)
        if wr:
            t.last_w = ev
            t.reads = []
        else:
            t.reads.append(ev)
            self.out_tiles.append(t)

    def finish(self):
        eng = self.E["sp"]
        seen = set()
        for t in self.out_tiles:
            if id(t) in seen:
                continue
            seen.add(id(t))
            self._wait(eng, (id(t.dsem), t.dcnt))

    def mm(self, out, lhsT, rhs, start=True, stop=True):
        self._op("pe", lambda h: h.matmul(out.ap, lhsT.ap, rhs.ap, start=start, stop=stop),
                 [lhsT, rhs], [out], is_mm=True)

    def tr(self, out, in_, ident):
        self._op("pe", lambda h: h.transpose(out.ap, in_.ap, ident.ap), [in_, ident], [out], is_mm=True)

    def act(self, out, in_, func, bias=None, scale=None, accum_out=None, eng="act"):
        kw = {}
        rd = [in_]
        if bias is not None:
            kw["bias"] = bias.ap if isinstance(bias, V) else bias
            rd.append(bias)
        if scale is not None:
            kw["scale"] = scale.ap if isinstance(scale, V) else scale
            rd.append(scale)
        wr = [out]
        if accum_out is not None:
            kw["accum_out"] = accum_out.ap
            wr.append(accum_out)
        self._op("act", lambda h: h.activation(out.ap, in_.ap, func, **kw), rd, wr)

    def tt(self, out, in0, in1, op, eng="dve"):
        self._op(eng, lambda h: h.tensor_tensor(out.ap, in0.ap, in1.ap, op), [in0, in1], [out])

    def ts(self, out, in0, s1, op0, s2=None, op1=None, eng="dve", accum_out=None):
        a1 = s1.ap if isinstance(s1, V) else s1
        a2 = s2.ap if isinstance(s2, V) else s2
        kw = {}
        wr = [out]
        if op1 is not None:
            kw["op1"] = op1
        if accum_out is not None:
            kw["accum_out"] = accum_out.ap
            wr.append(accum_out)
        self._op(eng, lambda h: h.tensor_scalar(out.ap, in0.ap, a1, a2, op0, **kw), [in0, s1, s2], wr)

    def stt(self, out, in0, scalar, in1, op0, op1):
        a = scalar.ap if isinstance(scalar, V) else scalar
        self._op("dve", lambda h: h.scalar_tensor_tensor(out.ap, in0.ap, a, in1.ap, op0, op1),
                 [in0, scalar, in1], [out])

    def scan(self, out, d0, d1, initial, op0=ALU.mult, op1=ALU.add):
        a = initial.ap if isinstance(initial, V) else initial
        self._op("dve", lambda h: h.tensor_tensor_scan(out.ap, d0.ap, d1.ap, a, op0, op1),
                 [d0, d1, initial], [out])

    def copy(self, out, in_, eng="dve"):
        if eng == "act":
            self._op("act", lambda h: h.copy(out.ap, in_.ap), [in_], [out])
        else:
            self._op(eng, lambda h: h.tensor_copy(out.ap, in_.ap), [in_], [out])

    def memset(self, out, val, eng="dve"):
        self._op(eng, lambda h: h.memset(out.ap, val), [], [out])

    def recip(self, out, in_):
        self._op("dve", lambda h: h.reciprocal(out.ap, in_.ap), [in_], [out])

    def max8(self, out, in_):
        self._op("dve", lambda h: h.max(out.ap, in_.ap), [in_], [out])

    def match_replace(self, out, to_rep, vals, imm):
        self._op("dve", lambda h: h.match_replace(out.ap, to_rep.ap, vals.ap, imm), [to_rep, vals], [out])

    def run(self, in_maps, n=8):
        self.finish()
        return run_bass_kernel_spmd(self.nc, in_maps, core_ids=list(range(n)))


def _barrier(self):
    evs = [(id(e.sem), e.cnt) for e in self.E.values() if e.cnt > 0]
    dm = {}
    for t in self._all_tiles:
        if t.dsem is not None and t.dcnt > 0:
            dm[id(t.dsem)] = max(dm.get(id(t.dsem), 0), t.dcnt)
    evs += list(dm.items())
    for e in self.E.values():
        for ev in evs:
            self._wait(e, ev)


Ctx.barrier = _barrier


EPS = 1e-6
TB = 512


class Pools:
    def __init__(self, c, nps=8, nw=2, wcols=28 * 512):
        self.c = c
        self.ps = [c.ps([128, 512], F32, name=f"psb{i}") for i in range(nps)]
        self.pi = 0
        self.w = [c.sb([128, wcols], BF16, name=f"wb{i}") for i in range(nw)]
        self.wi = 0

    def psum(self):
        t = self.ps[self.pi % len(self.ps)]
        self.pi += 1
        return t

    def wbuf(self):
        t = self.w[self.wi % len(self.w)]
        self.wi += 1
        return t


def load_consts(c, ident_d=None):
    K = {}
    K["ones_mean"] = c.sb([128, 128], BF16, name="ones_mean")
    c.memset(K["ones_mean"].v, 1.0 / 1024)
    K["ones_mean128"] = c.sb([128, 128], BF16, name="ones_mean128")
    c.memset(K["ones_mean128"].v, 1.0 / 128)
    K["ones"] = c.sb([128, 128], BF16, name="ones")
    c.memset(K["ones"].v, 1.0)
    K["eps"] = c.sb([128, 1], F32, name="epsc")
    c.memset(K["eps"].v, EPS)
    return K


def rmsnorm_fm(c, P, K, x, g, hn, kc_n, tb, sq, rstd, ones_key="ones_mean"):
    c.act(sq, x, AF.Square)
    ps = P.psum()
    for k in range(kc_n):
        c.mm(ps[:, :tb], K[ones_key].v, sq[:, k, :], start=(k == 0), stop=(k == kc_n - 1))
    c.act(rstd, ps[:, :tb], AF.Ln, bias=K["eps"].v)
    c.act(rstd, rstd, AF.Exp, scale=-0.5)
    for k in range(kc_n):
        c.stt(hn[:, k, :], x[:, k, :], g[:, k:k + 1], rstd, ALU.mult, ALU.mult)


def gemm(c, P, W, KC, cols, rhs_fn, out_fn, tb, NP=512, q="sp"):
    c0, n = cols
    nchunks = n // 128
    ci = 0
    p0 = 0
    while p0 < n:
        npan = min(NP, n - p0)
        wt = P.wbuf()
        wv = V(wt, wt.h[:, :KC * npan].rearrange("p (k n) -> p k n", k=KC))
        c.dma(wv, W[:, c0 + p0:c0 + p0 + npan].rearrange("(k p) n -> p k n", p=128), q=q)
        for j in range(npan // 128):
            ps = P.psum()
            for k in range(KC):
                c.mm(ps[:, :tb], wv[:, k, j * 128:(j + 1) * 128], rhs_fn(k), start=(k == 0), stop=(k == KC - 1))
            out_fn(ci, ps[:, :tb])
            ci += 1
        p0 += npan


def build_cast(F, CH=8192, c0=None, io=None):
    c = c0 or Ctx()

    def IN(name, shape, dt):
        if io is not None and name in io:
            ap = io[name]
            assert list(ap.shape) == list(shape), (name, ap.shape, shape)
            return ap
        assert io is None, name
        return c.din(name, shape, dt)

    def OUT(name, shape, dt):
        if io is not None and name in io:
            ap = io[name]
            assert list(ap.shape) == list(shape), (name, ap.shape, shape)
            return ap
        assert io is None, name
        return c.dout(name, shape, dt)
    xin = IN("w32", [128, F], F32)
    out = OUT("w16", [128, F], BF16)
    a = [c.sb([128, CH], F32, name=f"ca{i}") for i in range(2)]
    b = [c.sb([128, CH], BF16, name=f"cb{i}") for i in range(2)]
    engs = ["dve", "pool", "act"]
    i = 0
    p = 0
    while p < F:
        n = min(CH, F - p)
        ta, tb_ = a[i % 2], b[i % 2]
        c.dma(ta[:, :n], xin[:, p:p + n], q="sp")
        c.copy(tb_[:, :n], ta[:, :n], eng=engs[i % 3])
        c.dma(out[:, p:p + n], tb_[:, :n], q="act" if False else "sp")
        p += n
        i += 1
    return c


def mem_kv(c, P, K, memT_d, norm_mem_d, wkv_d, kn_d, Kt, Vt):
    with c.scope():
        _mem_kv_body(c, P, K, memT_d, norm_mem_d, wkv_d, kn_d, Kt, Vt)


def _mem_kv_body(c, P, K, memT_d, norm_mem_d, wkv_d, kn_d, Kt, Vt):
    xm = c.sb([128, 8, 256], F32, name="xm")
    c.dma(xm.v, memT_d.rearrange("(k p) m -> p k m", p=128))
    g = c.sb([128, 8], F32, name="gmem")
    c.dma(g.v, norm_mem_d)
    kn = c.sb([128, 1], F32, name="kn")
    c.dma(kn.v, kn_d)
    hm = c.sb([128, 8, 256], BF16, name="hm")
    sq = c.sb([128, 8, 256], BF16, name="sqm")
    rstd = c.sb([128, 256], F32, name="rstdm")
    rmsnorm_fm(c, P, K, xm.v, g.v, hm.v, 8, 256, sq.v, rstd.v)
    kraw = c.sb([128, 4, 256], F32, name="kraw")

    def out_k(ci, ps):
        c.copy(kraw[:, ci, :], ps, eng="act")
    gemm(c, P, wkv_d, 8, (0, 512), lambda k: hm[:, k, :], out_k, 256)
    ksq = c.sb([128, 4, 256], BF16, name="ksq")
    c.act(ksq.v, kraw.v, AF.Square)
    for h in range(4):
        ps = P.psum()
        c.mm(ps[:, :256], K["ones_mean128"].v, ksq[:, h, :])
        c.act(rstd.v, ps[:, :256], AF.Ln, bias=K["eps"].v)
        c.act(rstd.v, rstd.v, AF.Exp, scale=-0.5)
        c.stt(Kt[:, h, :], kraw[:, h, :], kn[:, 0:1], rstd.v, ALU.mult, ALU.mult)
    wt = P.wbuf()
    wv = V(wt, wt.h[:, :8 * 512].rearrange("p (k n) -> p k n", k=8))
    c.dma(wv, wkv_d[:, 512:1024].rearrange("(k p) n -> p k n", p=128))
    for mt in range(2):
        ps = P.psum()
        for k in range(8):
            c.mm(ps.v, hm[:, k, mt * 128:(mt + 1) * 128], wv[:, k, :], start=(k == 0), stop=(k == 7))
        c.copy(Vt[:, mt, :], ps.v, eng="act")


def cross_attn_blk(c, P, K, x, hn, sq, rstd, gcross, wq_d, qn, Kt, Vt, wo_d, S):
    rmsnorm_fm(c, P, K, x, gcross, hn, 8, TB, sq, rstd)
    qraw, qn_bf, pT, oT = S["qraw"], S["qn_bf"], S["pT"], S["oT"]

    def out_q(ci, ps):
        c.copy(qraw[:, ci, :], ps, eng="act")
    gemm(c, P, wq_d, 8, (0, 512), lambda k: hn[:, k, :], out_q, TB)
    c.act(S["qsq"].v, qraw.v, AF.Square)
    scale = 128 ** -0.5
    for h in range(4):
        ps = P.psum()
        c.mm(ps.v, K["ones_mean128"].v, S["qsq"][:, h, :])
        c.act(rstd, ps.v, AF.Ln, bias=K["eps"].v)
        c.act(rstd, rstd, AF.Exp, scale=-0.5)
        c.stt(qn_bf[:, h, :], qraw[:, h, :], qn[:, 0:1], rstd, ALU.mult, ALU.mult)
        for mt in range(2):
            ps2 = P.psum()
            c.mm(ps2.v, Kt[:, h, mt * 128:(mt + 1) * 128], qn_bf[:, h, :])
            c.act(pT[:, mt, :], ps2.v, AF.Exp, scale=scale)
        pso = P.psum()
        psd = P.psum()
        for mt in range(2):
            c.mm(pso.v, Vt[:, mt, h * 128:(h + 1) * 128], pT[:, mt, :], start=(mt == 0), stop=(mt == 1))
        for mt in range(2):
            c.mm(psd.v, K["ones"].v, pT[:, mt, :], start=(mt == 0), stop=(mt == 1))
        c.act(rstd, psd.v, AF.Ln)
        c.act(rstd, rstd, AF.Exp, scale=-1.0)
        c.tt(oT[:, h, :], pso.v, rstd, ALU.mult)

    def out_o(ci, ps):
        c.tt(x[:, ci, :], ps, x[:, ci, :], ALU.add)
    gemm(c, P, wo_d, 4, (0, 1024), lambda k: oT[:, k, :], out_o, TB)


def cross_scratch(c):
    S = {}
    S["qraw"] = c.sb([128, 4, TB], F32, name="qraw")
    S["qsq"] = c.sb([128, 4, TB], BF16, name="qsq")
    S["qn_bf"] = c.sb([128, 4, TB], BF16, name="qn_bf")
    S["pT"] = c.sb([128, 2, TB], BF16, name="pT")
    S["oT"] = c.sb([128, 4, TB], BF16, name="oT")
    return S


def build_stage_c(NT=2048, c0=None, io=None):
    c = c0 or Ctx()

    def IN(name, shape, dt):
        if io is not None and name in io:
            ap = io[name]
            assert list(ap.shape) == list(shape), (name, ap.shape, shape)
            return ap
        assert io is None, name
        return c.din(name, shape, dt)

    def OUT(name, shape, dt):
        if io is not None and name in io:
            ap = io[name]
            assert list(ap.shape) == list(shape), (name, ap.shape, shape)
            return ap
        assert io is None, name
        return c.dout(name, shape, dt)
    xT = IN("xT", [1024, NT], F32)
    mixT = IN("mixT", [2048, NT], BF16)
    memT = IN("memT", [1024, 256], F32)
    w_out = IN("w_out", [2048, 1024], BF16)
    wq = IN("wq", [1024, 512], BF16)
    wkv = IN("wkv", [1024, 1024], BF16)
    wo = IN("wo", [512, 1024], BF16)
    w13 = IN("w13", [1024, 5632], BF16)
    w2 = IN("w2", [2816, 1024], BF16)
    norm_cross = IN("norm_cross", [128, 8], F32)
    norm_mem = IN("norm_mem", [128, 8], F32)
    norm_ffn = IN("norm_ffn", [128, 8], F32)
    qn_d = IN("qn", [128, 1], F32)
    kn_d = IN("kn", [128, 1], F32)
    outT = OUT("outT", [1024, NT], F32)

    P = Pools(c, nw=3, wcols=22 * 512)
    K = load_consts(c)
    Kt = c.sb([128, 4, 256], BF16, name="Kt")
    Vt = c.sb([128, 2, 512], BF16, name="Vt")
    mem_kv(c, P, K, memT, norm_mem, wkv, kn_d, Kt, Vt)
    gc = c.sb([128, 8], F32, name="gc")
    c.dma(gc.v, norm_cross)
    gf = c.sb([128, 8], F32, name="gf")
    c.dma(gf.v, norm_ffn)
    qn = c.sb([128, 1], F32, name="qn")
    c.dma(qn.v, qn_d)
    S = cross_scratch(c)
    xs = [c.sb([128, 8, TB], F32, name=f"x{i}") for i in range(2)]
    mixs = [c.sb([128, 16, TB], BF16, name=f"mix{i}") for i in range(2)]
    hn = c.sb([128, 8, TB], BF16, name="hn")
    rstd = c.sb([128, TB], F32, name="rstd")
    hff = c.sb([128, 22, TB], BF16, name="hff")
    sq = hff[:, 0:8, :]
    sg = c.sb([128, 4, TB], BF16, name="sg")

    def loadC(blk):
        tsl_ = slice(blk * TB, (blk + 1) * TB)
        c.dma(xs[blk % 2].v, xT[:, tsl_].rearrange("(k p) t -> p k t", p=128), q="act")
        c.dma(mixs[blk % 2].v, mixT[:, tsl_].rearrange("(k p) t -> p k t", p=128), q="act")
    loadC(0)
    for blk in range(NT // TB):
        x = xs[blk % 2]
        mix = mixs[blk % 2]
        ts_ = slice(blk * TB, (blk + 1) * TB)
        if blk + 1 < NT // TB:
            loadC(blk + 1)

        def out_mix(ci, ps):
            c.tt(x[:, ci, :], ps, x[:, ci, :], ALU.add)
        gemm(c, P, w_out, 16, (0, 1024), lambda k: mix[:, k, :], out_mix, TB)
        cross_attn_blk(c, P, K, x.v, hn.v, sq, rstd.v, gc.v, wq, qn, Kt, Vt, wo, S)
        rmsnorm_fm(c, P, K, x.v, gf.v, hn.v, 8, TB, sq, rstd.v)
        for p0 in range(0, 2816, 512):
            n = min(512, 2816 - p0)

            def out_gate(ci, ps):
                c.act(sg[:, ci, :], ps, AF.Silu)

            def out_up(ci, ps, p0=p0):
                c.tt(hff[:, p0 // 128 + ci, :], ps, sg[:, ci, :], ALU.mult)
            gemm(c, P, w13, 8, (2816 + p0, n), lambda k: hn[:, k, :], out_gate, TB)
            gemm(c, P, w13, 8, (p0, n), lambda k: hn[:, k, :], out_up, TB)

        def out_dn(ci, ps):
            c.tt(x[:, ci, :], ps, x[:, ci, :], ALU.add)
        gemm(c, P, w2, 22, (0, 1024), lambda k: hff[:, k, :], out_dn, TB)
        c.dma(outT[:, ts_].rearrange("(k p) t -> p k t", p=128), x.v, q="act")
    return c


def build_stage_a(S=8192, CT=2048, c0=None, io=None):
    c = c0 or Ctx()

    def IN(name, shape, dt):
        if io is not None and name in io:
            ap = io[name]
            assert list(ap.shape) == list(shape), (name, ap.shape, shape)
            return ap
        assert io is None, name
        return c.din(name, shape, dt)

    def OUT(name, shape, dt):
        if io is not None and name in io:
            ap = io[name]
            assert list(ap.shape) == list(shape), (name, ap.shape, shape)
            return ap
        assert io is None, name
        return c.dout(name, shape, dt)
    xT = IN("xT", [1024, S], F32)
    gmix_d = IN("gmix", [128, 8], F32)
    wA_d = IN("wA", [1024, 512], BF16)
    cw_d = IN("cw", [128, 8], F32)
    cb_d = IN("cb", [128, 2], F32)
    wa_d = IN("wa", [128, 256], BF16)
    wx_d = IN("wx", [128, 256], BF16)
    ba_d = IN("ba", [128, 2], F32)
    bx_d = IN("bx", [128, 2], F32)
    lam_d = IN("lam", [128, 2], F32)
    rgT = OUT("rgT", [256, S], BF16)

    P = Pools(c, nw=1, wcols=8 * 512)
    K = load_consts(c)
    gm = c.sb([128, 8], F32, name="gm"); c.dma(gm.v, gmix_d)
    cw = c.sb([128, 8], F32, name="cw"); c.dma(cw.v, cw_d)
    cb = c.sb([128, 2], F32, name="cb"); c.dma(cb.v, cb_d)
    wa = c.sb([128, 256], BF16, name="wa"); c.dma(wa.v, wa_d)
    wx = c.sb([128, 256], BF16, name="wx"); c.dma(wx.v, wx_d)
    ba = c.sb([128, 2], F32, name="ba"); c.dma(ba.v, ba_d)
    bx = c.sb([128, 2], F32, name="bx"); c.dma(bx.v, bx_d)
    lam = c.sb([128, 2], F32, name="lam"); c.dma(lam.v, lam_d)
    one = c.sb([128, 1], F32, name="one"); c.memset(one.v, 1.0)
    n8 = c.sb([128, 2], F32, name="n8")
    c.act(n8.v, lam.v, AF.Exp, scale=-1.0)
    c.act(n8.v, n8.v, AF.Ln, bias=one.v)
    c.ts(n8.v, n8.v, -8.0, ALU.mult)
    wA = c.sb([128, 8, 512], BF16, name="wA")
    c.dma(wA.v, wA_d.rearrange("(k p) n -> p k n", p=128))

    xs = [c.sb([128, 8, TB], F32, name=f"x{i}") for i in range(2)]
    hns = [c.sb([128, 8, TB], BF16, name=f"hn{i}") for i in range(2)]
    sqs = [c.sb([128, 8, TB], BF16, name=f"sq{i}") for i in range(2)]
    rstd = c.sb([128, TB], F32, name="rstd")
    rgx = c.sb([128, 2, CT + 3], F32, name="rgx")
    gg = c.sb([128, 2, CT], BF16, name="gg")
    xc = c.sb([128, CT], F32, name="xc")
    xcb = c.sb([128, CT], BF16, name="xcb")
    Rb = c.sb([128, CT], F32, name="Rb")
    IG = c.sb([128, CT], F32, name="IG")
    Ab = c.sb([128, CT], F32, name="Ab")
    Ub = c.sb([128, CT], F32, name="Ub")
    Hc = [c.sb([128, CT], F32, name=f"Hc{i}") for i in range(2)]
    ob = c.sb([128, CT], BF16, name="ob")
    carry = c.sb([128, 2], F32, name="carry")
    c.memset(rgx[:, :, 0:3], 0.0)
    c.memset(carry.v, 0.0)

    def preA(blk):
        x = xs[blk % 2]
        c.dma(x.v, xT[:, blk * TB:(blk + 1) * TB].rearrange("(k p) t -> p k t", p=128), q="sp")
        rmsnorm_fm(c, P, K, x.v, gm.v, hns[blk % 2].v, 8, TB, sqs[blk % 2].v, rstd.v)
    preA(0)
    for ch in range(S // CT):
        for sb_ in range(CT // TB):
            blk = ch * (CT // TB) + sb_
            if blk + 1 < S // TB:
                preA(blk + 1)
            hn = hns[blk % 2]
            for j in range(4):
                ps = P.psum()
                for k in range(8):
                    c.mm(ps.v, wA[:, k, j * 128:(j + 1) * 128], hn[:, k, :], start=(k == 0), stop=(k == 7))
                if j < 2:
                    c.copy(rgx[:, j, 3 + sb_ * TB:3 + (sb_ + 1) * TB], ps.v, eng="act")
                else:
                    c.act(gg[:, j - 2, sb_ * TB:(sb_ + 1) * TB], ps.v, AF.Gelu)
        for b2 in range(2):
            c.act(xc.v, rgx[:, b2, 0:CT], AF.Identity, scale=cw[:, b2 * 4:b2 * 4 + 1], bias=cb[:, b2:b2 + 1])
            for j in range(1, 4):
                c.stt(xc.v, rgx[:, b2, j:j + CT], cw[:, b2 * 4 + j:b2 * 4 + j + 1], xc.v, ALU.mult, ALU.add)
            c.copy(xcb.v, xc.v, eng="pool")
            for s2 in range(CT // TB):
                sl = slice(s2 * TB, (s2 + 1) * TB)
                ps = P.psum()
                c.mm(ps.v, wa[:, b2 * 128:(b2 + 1) * 128], xcb[:, sl])
                c.act(Rb[:, sl], ps.v, AF.Sigmoid, bias=ba[:, b2:b2 + 1])
                ps = P.psum()
                c.mm(ps.v, wx[:, b2 * 128:(b2 + 1) * 128], xcb[:, sl])
                c.act(IG[:, sl], ps.v, AF.Sigmoid, bias=bx[:, b2:b2 + 1])
            c.act(Ab.v, Rb.v, AF.Exp, scale=n8[:, b2:b2 + 1])
            c.tt(Ub.v, Ab.v, Ab.v, ALU.mult, eng="pool")
            c.ts(Ub.v, Ub.v, -1.0, ALU.mult, 1.0, ALU.add, eng="pool")
            c.act(Ub.v, Ub.v, AF.Sqrt)
            c.tt(IG.v, IG.v, xc.v, ALU.mult)
            c.tt(Ub.v, Ub.v, IG.v, ALU.mult)
            H = Hc[b2]
            c.scan(H.v, Ab.v, Ub.v, carry[:, b2:b2 + 1])
            c.copy(carry[:, b2:b2 + 1], H[:, CT - 1:CT])
            c.tt(ob.v, H.v, gg[:, b2, :], ALU.mult)
            c.dma(rgT[b2 * 128:(b2 + 1) * 128, ch * CT:(ch + 1) * CT], ob.v, q="act")
        c.copy(rgx[:, :, 0:3], rgx[:, :, CT:CT + 3])
    return c


def bcl(v, n):
    shp = list(v.ap.shape) + [n]
    return V(v.tile, v.ap.unsqueeze(len(shp) - 1).to_broadcast(shp))


def build_stage_d(S=8192, dbg=False, c0=None, io=None, mode="full", st_d=None, tok0=None, qidx=None, hook=None):
    c = c0 or Ctx()

    def IN(name, shape, dt):
        if io is not None and name in io:
            ap = io[name]
            assert list(ap.shape) == list(shape), (name, ap.shape, shape)
            return ap
        assert io is None, name
        return c.din(name, shape, dt)

    def OUT(name, shape, dt):
        if io is not None and name in io:
            ap = io[name]
            assert list(ap.shape) == list(shape), (name, ap.shape, shape)
            return ap
        assert io is None, name
        return c.dout(name, shape, dt)
    DBG = {}

    def dump(name, v, shape, dt):
        if not dbg:
            return
        o = c.dout("dbg_" + name, shape, dt)
        c.dma(o, v, q="sp")
    FULL = mode != "state"
    nblk = {"full": S // TB, "state": 12, "own": 4}[mode]
    xT = IN("xT", [1024, S], F32)
    gmix_d = IN("gmix", [128, 8], F32)
    wD_d = IN("wD", [1024, 1280], BF16)
    wdt_d = IN("wdt", [1024, 8], BF16)
    cw_d = IN("cw", [128, 24], F32)
    cb_d = IN("cb", [128, 6], F32)
    dtb_d = IN("dtb", [128, 8], F32)
    alog_d = IN("alog", [128, 8], F32)
    dsk_d = IN("dsk", [128, 8], F32)
    gn_d = IN("gn", [128, 4], F32)
    wo_d = IN("wo", [512, 1024], BF16)
    identb_d = IN("identb", [128, 128], BF16)
    identf_d = IN("identf", [128, 128], F32)
    U_d = IN("U", [128, 128], F32)
    NM_d = IN("NM", [128, 128], F32)
    onesf_d = IN("onesf", [128, 128], F32)
    outT = OUT("outT", [1024, nblk * TB], F32) if FULL else None

    P = Pools(c, nps=5, nw=1, wcols=16)
    K = load_consts(c)
    pso = c.ps([128, 512], F32, name="pso")
    psy = c.ps([128, 512], F32, name="psy")

    def ld(name, d, shape, dt):
        t = c.sb(shape, dt, name=name)
        c.dma(t.v, d)
        return t
    gm = ld("gm", gmix_d, [128, 8], F32)
    cw = ld("cw", cw_d, [128, 24], F32)
    cb = ld("cb", cb_d, [128, 6], F32)
    dtb = ld("dtb", dtb_d, [128, 8], F32)
    Aneg = ld("alog", alog_d, [128, 8], F32)
    dsk = ld("dsk", dsk_d, [128, 8], F32)
    gn = ld("gn", gn_d, [128, 4], F32)
    identb = ld("identb", identb_d, [128, 128], BF16)
    identf = ld("identf", identf_d, [128, 128], F32)
    U = ld("U", U_d, [128, 128], F32)
    NM = ld("NM", NM_d, [128, 128], F32)
    onesf = ld("onesf", onesf_d, [128, 128], F32)
    one = c.sb([128, 1], F32, name="one"); c.memset(one.v, 1.0)
    eps = K["eps"]
    c.act(Aneg.v, Aneg.v, AF.Exp)
    c.ts(Aneg.v, Aneg.v, -1.0, ALU.mult)
    wD = c.sb([128, 8, 1280], BF16, name="wD")
    c.dma(wD.v, wD_d.rearrange("(k p) n -> p k n", p=128))
    wdt = c.sb([128, 8, 8], BF16, name="wdt")
    c.dma(wdt.v, wdt_d.rearrange("(k p) n -> p k n", p=128))
    wo = c.sb([128, 4, 1024], BF16, name="wo")
    c.dma(wo.v, wo_d.rearrange("(k p) n -> p k n", p=128))

    xs_in = [c.sb([128, 8, TB], F32, name=f"x{i}") for i in range(2)]
    hns = [c.sb([128, 8, TB], BF16, name=f"hn{i}") for i in range(2)]
    sqs = [c.sb([128, 8, TB], BF16, name=f"sq{i}") for i in range(2)]
    rstd = c.sb([128, TB], F32, name="rstd")
    raw = c.sb([128, 6, TB + 3], F32, name="raw")
    cacc = c.sb([128, TB], F32, name="cacc")
    xact = c.sb([128, 6, TB], BF16, name="xact")
    sz = c.sb([128, 4, 512], BF16, name="sz")
    dt_t = c.sb([128, 4, 8], F32, name="dt_t")
    a_t = c.sb([128, 4, 8], F32, name="a_t")
    xs_tm_l = [c.sb([128, 512], BF16, name=f"xs_tm{i}") for i in range(2)]
    B_tm_l = [c.sb([128, 128], BF16, name=f"B_tm{i}") for i in range(2)]
    acs_l = [c.sb([128, 8], F32, name=f"acs{i}") for i in range(2)]
    nacs_l = [c.sb([128, 8], F32, name=f"nacs{i}") for i in range(2)]
    eacs_l = [c.sb([128, 8], F32, name=f"eacs{i}") for i in range(2)]
    dst_l = [c.sb([128, 8], F32, name=f"dst{i}") for i in range(2)]
    cdec_l = [c.sb([128, 8], F32, name=f"cdec{i}") for i in range(2)]
    abc_l = [c.sb([128, 8, 128], F32, name=f"abc{i}") for i in range(2)]
    xdt_l = [c.sb([128, 512], BF16, name=f"xdt{i}") for i in range(2)]
    xd_l = [c.sb([128, 512], BF16, name=f"xd{i}") for i in range(2)]
    GT_l = [c.sb([128, 128], BF16, name=f"GT{i}") for i in range(2)]
    LT = [c.sb([128, 128], BF16, name=f"LT{i}") for i in range(3)]
    MT = [c.sb([128, 128], BF16, name=f"MT{i}") for i in range(3)]
    prev = c.sb([128, 512], F32, name="prev")
    prevb = c.sb([128, 512], BF16, name="prevb")
    t1 = c.sb([128, 512], F32, name="t1")
    t2 = c.sb([128, 512], F32, name="t2")
    ysq = c.sb([128, 512], BF16, name="ysq")
    ss = c.sb([128, 1], F32, name="ss")
    yn = c.sb([128, 512], BF16, name="yn")
    ynT = c.sb([128, 4, TB], BF16, name="ynT")
    obuf = c.sb([128, 8, TB], F32, name="obuf")
    psT = c.ps([128, 512], BF16, name="psT")
    if mode == "own":
        stv = st_d[bass.ds(qidx, 1)].rearrange("o p n -> (o p) n")
        c.dma(prev.v, stv[:, 0:512], q="sp")
        c.dma(raw[:, :, 0:3], stv[:, 512:530].rearrange("p (a b) -> p a b", b=3), q="sp")
        c.copy(prevb.v, prev.v)
    else:
        c.memset(raw[:, :, 0:3], 0.0)
        c.memset(prev.v, 0.0)
        c.memset(prevb.v, 0.0)
        if mode == "state":
            c.dma(st_d[0][:, 0:512], prev.v, q="sp")
            c.dma(st_d[0][:, 512:530].rearrange("p (a b) -> p a b", b=3), raw[:, :, 0:3], q="sp")

    def preD(blk):
        x = xs_in[blk % 2]
        xcols = slice(blk * TB, (blk + 1) * TB) if mode != "own" else bass.ds(tok0 + blk * TB, TB)
        c.dma(x.v, xT[:, xcols].rearrange("(k p) t -> p k t", p=128), q="sp")
        rmsnorm_fm(c, P, K, x.v, gm.v, hns[blk % 2].v, 8, TB, sqs[blk % 2].v, rstd.v)
    preD(0)
    for blk in range(nblk):
        if blk + 1 < nblk:
            preD(blk + 1)
        hn = hns[blk % 2]
        for c6 in range(6):
            ps = P.psum()
            for k in range(8):
                c.mm(ps.v, wD[:, k, 512 + c6 * 128:512 + (c6 + 1) * 128], hn[:, k, :], start=(k == 0), stop=(k == 7))
            c.copy(raw[:, c6, 3:3 + TB], ps.v, eng="act")
        for tt_ in range(4):
            tsl = slice(tt_ * 128, (tt_ + 1) * 128)
            if FULL:
                ps = P.psum()
                for k in range(8):
                    c.mm(ps.v, hn[:, k, tsl], wD[:, k, 0:512], start=(k == 0), stop=(k == 7))
                c.act(sz[:, tt_, :], ps.v, AF.Silu)
            ps = P.psum()
            for k in range(8):
                c.mm(ps[:, 0:8], hn[:, k, tsl], wdt[:, k, :], start=(k == 0), stop=(k == 7))
            c.tt(dt_t[:, tt_, :], ps[:, 0:8], dtb.v, ALU.add)
        c.act(dt_t.v, dt_t.v, AF.Exp)
        c.act(dt_t.v, dt_t.v, AF.Ln, bias=one.v)
        c.tt(a_t.v, dt_t.v, V(Aneg, Aneg.h[:].unsqueeze(1).to_broadcast([128, 4, 8])), ALU.mult)
        for c6 in range(6):
            c.act(cacc.v, raw[:, c6, 0:TB], AF.Identity, scale=cw[:, c6 * 4:c6 * 4 + 1], bias=cb[:, c6:c6 + 1])
            for j in range(1, 4):
                c.stt(cacc.v, raw[:, c6, j:j + TB], cw[:, c6 * 4 + j:c6 * 4 + j + 1], cacc.v, ALU.mult, ALU.add)
            c.act(xact[:, c6, :], cacc.v, AF.Silu)
        c.copy(raw[:, :, 0:3], raw[:, :, TB:TB + 3])
        def cfront(tt_):
            xs_tm, B_tm, acs, nacs, eacs, dst, cdec, abc, xdt, xd, GT = xs_tm_l[tt_ % 2], B_tm_l[tt_ % 2], acs_l[tt_ % 2], nacs_l[tt_ % 2], eacs_l[tt_ % 2], dst_l[tt_ % 2], cdec_l[tt_ % 2], abc_l[tt_ % 2], xdt_l[tt_ % 2], xd_l[tt_ % 2], GT_l[tt_ % 2]
            tsl = slice(tt_ * 128, (tt_ + 1) * 128)
            for c4 in range(4):
                c.tr(psT[:, c4 * 128:(c4 + 1) * 128], xact[:, c4, tsl], identb.v)
            c.copy(xs_tm.v, psT.v, eng="act")
            c.tr(psT[:, 0:128], xact[:, 4, tsl], identb.v)
            c.copy(B_tm.v, psT[:, 0:128], eng="act")
            a = a_t[:, tt_, :]
            dt = dt_t[:, tt_, :]
            if hook is not None:
                hook()
            ps = P.psum()
            c.mm(ps[:, 0:8], U.v, a)
            c.copy(acs.v, ps[:, 0:8])
            if FULL:
                c.ts(nacs.v, ps[:, 0:8], -1.0, ALU.mult)
                c.act(eacs.v, ps[:, 0:8], AF.Exp)
            ps2 = P.psum()
            c.mm(ps2[:, 0:8], onesf.v, a)
            c.act(cdec.v, ps2[:, 0:8], AF.Exp)
            c.tt(dst.v, ps2[:, 0:8], acs.v, ALU.subtract)
            c.act(dst.v, dst.v, AF.Exp)
            if FULL:
                c.copy(abc.v, bcl(a, 128), eng="pool")
            c.tt(xdt.v.re("p (h e) -> p h e", h=8), xs_tm.v.re("p (h e) -> p h e", h=8), bcl(dt, 64), ALU.mult)
            c.tt(xd.v.re("p (h e) -> p h e", h=8), xdt.v.re("p (h e) -> p h e", h=8), bcl(dst.v, 64), ALU.mult, eng="pool")
            if FULL:
                psg = P.psum()
                c.mm(psg[:, 0:128], xact[:, 4, tsl], xact[:, 5, tsl])
                c.copy(GT.v, psg[:, 0:128], eng="act")

        def cback(tt_):
            xs_tm, B_tm, acs, nacs, eacs, dst, cdec, abc, xdt, xd, GT = xs_tm_l[tt_ % 2], B_tm_l[tt_ % 2], acs_l[tt_ % 2], nacs_l[tt_ % 2], eacs_l[tt_ % 2], dst_l[tt_ % 2], cdec_l[tt_ % 2], abc_l[tt_ % 2], xdt_l[tt_ % 2], xd_l[tt_ % 2], GT_l[tt_ % 2]
            tsl = slice(tt_ * 128, (tt_ + 1) * 128)
            a = a_t[:, tt_, :]
            dt = dt_t[:, tt_, :]
            if FULL:
                c.mm(pso.v, xact[:, 5, tsl], prevb.v)
                def hfront(h):
                    psd = P.psum()
                    c.mm(psd[:, 0:128], abc[:, h, :], U.v, start=True, stop=False)
                    c.mm(psd[:, 0:128], identf.v, NM.v, start=False, stop=True)
                    L = LT[h % 3]
                    M = MT[h % 3]
                    c.act(L.v, psd[:, 0:128], AF.Exp, bias=nacs[:, h:h + 1])
                    c.tt(M.v, L.v, GT.v, ALU.mult)

                def hback(h):
                    c.mm(psy[:, h * 64:(h + 1) * 64], MT[h % 3].v, xdt[:, h * 64:(h + 1) * 64])
                hfront(0)
                hfront(1)
                for h in range(8):
                    hback(h)
                    if h + 2 < 8:
                        hfront(h + 2)
                c.tt(t1.v.re("p (h e) -> p h e", h=8), pso.v.re("p (h e) -> p h e", h=8), bcl(eacs.v, 64), ALU.mult)
                c.tt(t1.v, t1.v, psy.v, ALU.add)
                c.tt(t2.v.re("p (h e) -> p h e", h=8), xs_tm.v.re("p (h e) -> p h e", h=8), bcl(dsk.v, 64), ALU.mult, eng="pool")
                c.tt(t1.v, t1.v, t2.v, ALU.add)
                c.tt(t1.v, t1.v, sz[:, tt_, :], ALU.mult)
                c.act(ysq.v, t1.v, AF.Square, accum_out=ss.v)
                c.ts(ss.v, ss.v, 1.0 / 512, ALU.mult, EPS, ALU.add)
                c.act(ss.v, ss.v, AF.Sqrt)
                c.recip(ss.v, ss.v)
                c.ts(yn.v, t1.v, ss[:, 0:1], ALU.mult)
                for c4 in range(4):
                    c.tr(psT[:, c4 * 128:(c4 + 1) * 128], yn[:, c4 * 128:(c4 + 1) * 128], identb.v)
                for c4 in range(4):
                    c.ts(ynT[:, c4, tsl], psT[:, c4 * 128:(c4 + 1) * 128], gn[:, c4:c4 + 1], ALU.mult)
            pss = P.psum()
            c.mm(pss.v, B_tm.v, xd.v)
            c.tt(prev.v.re("p (h e) -> p h e", h=8), prev.v.re("p (h e) -> p h e", h=8), bcl(cdec.v, 64), ALU.mult)
            c.tt(prev.v, prev.v, pss.v, ALU.add)
            if FULL:
                c.copy(prevb.v, prev.v, eng="pool")
            if FULL and blk == 0 and tt_ == 1:
                dump("dt", dt_t.v, [128, 4, 8], F32)
                dump("a", a_t.v, [128, 4, 8], F32)
                dump("xact", xact.v, [128, 6, TB], BF16)
                dump("sz", sz.v, [128, 4, 512], BF16)
                dump("xs_tm", xs_tm.v, [128, 512], BF16)
                dump("B_tm", B_tm.v, [128, 128], BF16)
                dump("acs", acs.v, [128, 8], F32)
                dump("dst", dst.v, [128, 8], F32)
                dump("cdec", cdec.v, [128, 8], F32)
                dump("GT", GT.v, [128, 128], BF16)
                dump("LT", LT[1].v, [128, 128], BF16)
                dump("MT", MT[1].v, [128, 128], BF16)
                dump("t1", t1.v, [128, 512], F32)
                dump("yn", yn.v, [128, 512], BF16)
                dump("prev", prev.v, [128, 512], F32)
                dump("xdt", xdt.v, [128, 512], BF16)
                dump("ss", ss.v, [128, 1], F32)
        cfront(0)
        for tt_ in range(4):
            if tt_ + 1 < 4:
                cfront(tt_ + 1)
            cback(tt_)
        if mode == "state" and blk % 4 == 3:
            qq = (blk + 1) // 4
            c.dma(st_d[qq][:, 0:512], prev.v, q="sp")
            c.dma(st_d[qq][:, 512:530].rearrange("p (a b) -> p a b", b=3), raw[:, :, 0:3], q="sp")
        if not FULL:
            continue
        for n in range(8):
            ps = P.psum()
            for k in range(4):
                c.mm(ps.v, wo[:, k, n * 128:(n + 1) * 128], ynT[:, k, :], start=(k == 0), stop=(k == 3))
            c.copy(obuf[:, n, :], ps.v, eng="act")
        c.dma(outT[:, blk * TB:(blk + 1) * TB].rearrange("(k p) t -> p k t", p=128), obuf.v, q="act")
    return c


def build_stage_e(NT=2048, NE=8, FE=3584, c0=None, io=None, tok0=None):
    c = c0 or Ctx()

    def IN(name, shape, dt):
        if io is not None and name in io:
            ap = io[name]
            assert list(ap.shape) == list(shape), (name, ap.shape, shape)
            return ap
        assert io is None, name
        return c.din(name, shape, dt)

    def OUT(name, shape, dt):
        if io is not None and name in io:
            ap = io[name]
            assert list(ap.shape) == list(shape), (name, ap.shape, shape)
            return ap
        assert io is None, name
        return c.dout(name, shape, dt)
    SX = NT if tok0 is None else 8192
    xT = IN("xT", [1024, SX], F32)
    pT_d = IN("pT", [4, 1024, NT], F32)
    memT = IN("memT", [1024, 256], F32)
    wq = IN("wq", [1024, 512], BF16)
    wkv = IN("wkv", [1024, 1024], BF16)
    wo = IN("wo", [512, 1024], BF16)
    norm_cross = IN("norm_cross", [128, 8], F32)
    norm_mem = IN("norm_mem", [128, 8], F32)
    norm_ffn = IN("norm_ffn", [128, 8], F32)
    qn_d = IN("qn", [128, 1], F32)
    kn_d = IN("kn", [128, 1], F32)
    router_d = IN("router", [1024, NE], F32)
    w13 = IN("w13", [NE, 1024, 2 * FE], BF16)
    w2 = IN("w2", [NE, FE, 1024], BF16)
    identf_d = IN("identf", [128, 128], F32)
    sel_d = IN("sel", [NE, NE * 128], F32)
    outT = OUT("outT", [1024, NT], F32)
    KF = FE // 128

    P = Pools(c, nw=3, wcols=KF * 512)
    K = load_consts(c)
    Kt = c.sb([128, 4, 256], BF16, name="Kt")
    Vt = c.sb([128, 2, 512], BF16, name="Vt")
    mem_kv(c, P, K, memT, norm_mem, wkv, kn_d, Kt, Vt)
    gc = c.sb([128, 8], F32, name="gc"); c.dma(gc.v, norm_cross)
    gf = c.sb([128, 8], F32, name="gf"); c.dma(gf.v, norm_ffn)
    qn = c.sb([128, 1], F32, name="qn"); c.dma(qn.v, qn_d)
    identf = c.sb([128, 128], F32, name="identf"); c.dma(identf.v, identf_d)
    sel = c.sb([NE, NE * 128], F32, name="sel"); c.dma(sel.v, sel_d)
    rt = c.sb([128, 8, NE], F32, name="rt"); c.dma(rt.v, router_d.rearrange("(k p) e -> p k e", p=128))
    S = cross_scratch(c)
    x = c.sb([128, 8, TB], F32, name="x")
    pin = [c.sb([128, 8, TB], F32, name=f"pin{i}") for i in range(1)]
    hn = c.sb([128, 8, TB], BF16, name="hn")
    hn32 = pin[0]
    rstd = c.sb([128, TB], F32, name="rstd")
    hff = c.sb([128, KF, TB], BF16, name="hff")
    sq = hff[:, 0:8, :]
    sg = c.sb([128, 4, TB], BF16, name="sg")
    lg = c.sb([128, NE], F32, name="lg")
    m8 = c.sb([128, 8], F32, name="m8")
    w1 = c.sb([128, 1], F32, name="w1")
    w2s = c.sb([128, 1], F32, name="w2s")
    g1 = c.sb([128, NE], F32, name="g1")
    g2 = c.sb([128, NE], F32, name="g2")
    gT = c.sb([NE, TB], F32, name="gT")
    gb = c.sb([128, TB], F32, name="gb")
    tmp = c.sb([128, TB], F32, name="tmp")
    assert NE == 8

    for blk in range(NT // TB):
        ts_ = slice(blk * TB, (blk + 1) * TB)
        tin = ts_ if tok0 is None else bass.ds(tok0 + blk * TB, TB)
        qin = "act" if tok0 is None else "sp"
        c.dma(x.v, xT[:, tin].rearrange("(k p) t -> p k t", p=128), q=qin)
        for i in range(4):
            pb = pin[0]
            c.dma(pb.v, pT_d[i, :, ts_].rearrange("(k p) t -> p k t", p=128), q=qin)
            c.tt(x.v, x.v, pb.v, ALU.add, eng="pool" if i % 2 else "dve")
        cross_attn_blk(c, P, K, x.v, hn.v, sq, rstd.v, gc.v, wq, qn, Kt, Vt, wo, S)
        rmsnorm_fm(c, P, K, x.v, gf.v, hn.v, 8, TB, sq, rstd.v)
        for k in range(8):
            c.stt(hn32[:, k, :], x[:, k, :], gf[:, k:k + 1], rstd.v, ALU.mult, ALU.mult)
        for tt_ in range(TB // 128):
            tsl = slice(tt_ * 128, (tt_ + 1) * 128)
            ps = P.psum()
            for k in range(8):
                c.mm(ps[:, 0:NE], hn32[:, k, tsl], rt[:, k, :], start=(k == 0), stop=(k == 7))
            c.copy(lg.v, ps[:, 0:NE])
            c.max8(m8.v, lg.v)
            c.tt(w1.v, m8[:, 1:2], m8[:, 0:1], ALU.subtract)
            c.act(w1.v, w1.v, AF.Exp)
            c.ts(w1.v, w1.v, 1.0, ALU.add)
            c.recip(w1.v, w1.v)
            c.ts(w2s.v, w1.v, -1.0, ALU.mult, 1.0, ALU.add)
            c.ts(g1.v, lg.v, m8[:, 0:1], ALU.is_equal, w1[:, 0:1], ALU.mult)
            c.ts(g2.v, lg.v, m8[:, 1:2], ALU.is_equal, w2s[:, 0:1], ALU.mult)
            c.tt(g1.v, g1.v, g2.v, ALU.add)
            pst = P.psum()
            c.tr(pst[0:NE, 0:128], g1.v, identf.v)
            c.copy(gT[:, tsl], pst[0:NE, 0:128], eng="act")
        for e in range(NE):
            psg = P.psum()
            c.mm(psg.v, sel[:, e * 128:(e + 1) * 128], gT.v)
            c.copy(gb.v, psg.v, eng="act")
            for p0 in range(0, FE, 512):
                def out_gate(ci, ps):
                    c.act(sg[:, ci, :], ps, AF.Silu)

                def out_up(ci, ps, p0=p0):
                    c.tt(hff[:, p0 // 128 + ci, :], ps, sg[:, ci, :], ALU.mult)
                gemm(c, P, w13[e], 8, (FE + p0, 512), lambda k: hn[:, k, :], out_gate, TB)
                gemm(c, P, w13[e], 8, (p0, 512), lambda k: hn[:, k, :], out_up, TB)

            def out_dn(ci, ps):
                c.tt(tmp.v, ps, gb.v, ALU.mult)
                c.tt(x[:, ci, :], tmp.v, x[:, ci, :], ALU.add, eng="pool")
            gemm(c, P, w2[e], KF, (0, 1024), lambda k: hff[:, k, :], out_dn, TB)
        c.dma(outT[:, ts_].rearrange("(k p) t -> p k t", p=128), x.v, q="act")
    return c


def bcm(v, n):
    shp = [v.ap.shape[0], n] + list(v.ap.shape[1:])
    return V(v.tile, v.ap.unsqueeze(1).to_broadcast(shp))


def build_stage_b(S=8192, NQ=4096, dbg=False, lim=99, nqt_lim=None, c0=None, io=None, att_dst=None,
                  kv_save=None, kv_load=None):
    from contextlib import ExitStack
    c = c0 or Ctx()

    def IN(name, shape, dt):
        if io is not None and name in io:
            ap = io[name]
            assert list(ap.shape) == list(shape), (name, ap.shape, shape)
            return ap
        assert io is None, name
        return c.din(name, shape, dt)

    def OUT(name, shape, dt):
        if io is not None and name in io:
            ap = io[name]
            assert list(ap.shape) == list(shape), (name, ap.shape, shape)
            return ap
        assert io is None, name
        return c.dout(name, shape, dt)
    NKT = S // 128
    NQT = NQ // 128
    xT = IN("xT", [1024, S], F32)
    xqT = IN("xqT", [1024, NQ], F32)
    tq_d = IN("tq", [128, NQT], F32)
    tqb_d = IN("tqb", [128, NQ], F32)
    gmix_d = IN("gmix", [128, 8], F32)
    wq_d = IN("wq", [1024, 512], BF16)
    wkv_d = IN("wkv", [1024, 768], BF16)
    wg_d = IN("wg", [1024, 12], BF16)
    gateb_d = IN("gateb", [12, 1], F32)
    qnorm_d = IN("qnorm", [128, 1], F32)
    knorm_d = IN("knorm", [128, 3], F32)
    posT_d = IN("posT", [128, 32], BF16)
    ckw1_d = IN("ckw1", [4096, 256], BF16)
    ckw2_d = IN("ckw2", [256, 128], BF16)
    cvw1_d = IN("cvw1", [4096, 256], BF16)
    cvw2_d = IN("cvw2", [256, 128], BF16)
    identb_d = IN("identb", [128, 128], BF16)
    E_d = IN("E", [128, S], BF16)
    ov_d = IN("ov", [128, 4, 129], BF16)
    sel12_d = IN("sel12", [12, 12 * 128], BF16)
    keypos_d = IN("keypos", [128, NKT], F32)
    cmpend_d = IN("cmpend", [128, 4], F32)
    j64_d = IN("j64", [128, 128], F32)
    e0_d = IN("e0", [128, 128], F32)
    attT = OUT("attT", [4, 128, NQ], BF16) if att_dst is None else None
    scale = 128 ** -0.5

    def dump(name, v, shape, dt):
        if dbg:
            o = c.dout("dbg_" + name, shape, dt)
            c.dma(o, v, q="sp")

    P = Pools(c, nps=3, nw=1, wcols=16)
    K = load_consts(c)
    psO = c.ps([128, 512], F32, name="psO")
    psD = c.ps([128, 512], F32, name="psD")
    psM = [c.ps([128, 512], F32, name=f"psM{i}") for i in range(2)]
    psX = c.ps([128, 512], F32, name="psX")

    def ld(name, d, shape, dt, q="sp"):
        t = c.sb(shape, dt, name=name)
        c.dma(t.v, d, q=q)
        return t
    gm = ld("gm", gmix_d, [128, 8], F32)
    gateb = ld("gateb", gateb_d, [12, 1], F32)
    qnorm = ld("qnorm", qnorm_d, [128, 1], F32)
    knorm = ld("knorm", knorm_d, [128, 3], F32)
    posT = ld("posT", posT_d, [128, 32], BF16)
    identb = ld("identb", identb_d, [128, 128], BF16)
    ov = ld("ov", ov_d, [128, 4, 129], BF16)
    sel12 = ld("sel12", sel12_d, [12, 12 * 128], BF16)
    keypos = ld("keypos", keypos_d, [128, NKT], F32)
    cmpend = ld("cmpend", cmpend_d, [128, 4], F32)
    j64 = ld("j64", j64_d, [128, 128], F32)
    e0 = ld("e0", e0_d, [128, 128], F32)
    tq = ld("tq", tq_d, [128, NQT], F32)
    wq = c.sb([128, 8, 512], BF16, name="wq"); c.dma(wq.v, wq_d.rearrange("(k p) n -> p k n", p=128))
    wkv = c.sb([128, 8, 768], BF16, name="wkv"); c.dma(wkv.v, wkv_d.rearrange("(k p) n -> p k n", p=128))
    wg = c.sb([128, 8, 12], BF16, name="wg"); c.dma(wg.v, wg_d.rearrange("(k p) n -> p k n", p=128))
    ckw2 = c.sb([128, 2, 128], BF16, name="ckw2"); c.dma(ckw2.v, ckw2_d.rearrange("(k p) n -> p k n", p=128))
    cvw2 = c.sb([128, 2, 128], BF16, name="cvw2"); c.dma(cvw2.v, cvw2_d.rearrange("(k p) n -> p k n", p=128))

    ksnT = c.sb([128, S], BF16, name="ksnT")
    kwnT = c.sb([128, S], BF16, name="kwnT")
    vs_tm = c.sb([128, NKT, 128], BF16, name="vs_tm")
    vw_tm = c.sb([128, NKT, 128], BF16, name="vw_tm")
    qnT = c.sb([128, 4, NQ], BF16, name="qnT")
    Gs = c.sb([12, NQ], BF16, name="Gs")
    kcmpT = c.sb([128, 512], BF16, name="kcmpT")
    vcmp = c.sb([128, 4, 128], BF16, name="vcmp")
    rstd = c.sb([128, TB], F32, name="rstd")
    rstdn = c.sb([128, TB], F32, name="rstdn")
    tmpf = c.sb([128, TB], F32, name="tmpf")
    tmpb = c.sb([128, TB], BF16, name="tmpb")

    with ExitStack() as st:
        kcT = c.sb_scoped(st, [128, S + 16], BF16, name="kcT")
        vcT = c.sb_scoped(st, [128, S + 16], BF16, name="vcT")
        st2 = ExitStack()
        x = c.sb_scoped(st2, [128, 8, TB], F32, name="x")
        hn = c.sb_scoped(st2, [128, 8, TB], BF16, name="hn")
        sq = c.sb_scoped(st2, [128, 8, TB], BF16, name="sq")
        c.memset(kcT[:, S:S + 16], 0.0)
        c.memset(vcT[:, S:S + 16], 0.0)
        qflat = qnT.h[:].rearrange("p a b -> p (a b)")
        xB = [x.v, V(qnT, qflat[:, 0:8192].bitcast(F32).rearrange("p (k t) -> p k t", k=8))]
        hB = [hn.v, V(qnT, qflat[:, 8192:12288].rearrange("p (k t) -> p k t", k=8))]
        sB = [sq.v, V(qnT, qflat[:, 12288:16384].rearrange("p (k t) -> p k t", k=8))]

        def pre1(blk):
            bs_ = slice(blk * TB, (blk + 1) * TB)
            c.dma(xB[blk % 2], xT[:, bs_].rearrange("(k p) t -> p k t", p=128), q="sp")
            rmsnorm_fm(c, P, K, xB[blk % 2], gm.v, hB[blk % 2], 8, TB, sB[blk % 2], rstdn.v)
        if kv_load is None:
            pre1(0)
        for blk in range(S // TB if kv_load is None else 0):
            bs = slice(blk * TB, (blk + 1) * TB)
            if blk + 1 < S // TB:
                pre1(blk + 1)
            hn = hB[blk % 2]
            for j in range(4):
                ps = P.psum()
                for k in range(8):
                    c.mm(ps.v, wkv[:, k, j * 128:(j + 1) * 128], hn[:, k, :], start=(k == 0), stop=(k == 7))
                if j == 0:
                    c.copy(kcT[:, bs], ps.v, eng="act")
                elif j == 1:
                    c.copy(vcT[:, bs], ps.v, eng="act")
                else:
                    dst = ksnT if j == 2 else kwnT
                    c.copy(tmpf.v, ps.v, eng="act")
                    c.act(tmpb.v, ps.v, AF.Square)
                    ps2 = P.psum()
                    c.mm(ps2.v, K["ones_mean128"].v, tmpb.v)
                    c.act(rstd.v, ps2.v, AF.Ln, bias=K["eps"].v)
                    c.act(rstd.v, rstd.v, AF.Exp, scale=-0.5)
                    c.stt(dst[:, bs], tmpf.v, knorm[:, j - 1:j], rstd.v, ALU.mult, ALU.mult)
            for tt_ in range(4):
                tsl = slice(tt_ * 128, (tt_ + 1) * 128)
                ps = P.psum()
                for k in range(8):
                    c.mm(ps[:, 0:256], hn[:, k, tsl], wkv[:, k, 512:768], start=(k == 0), stop=(k == 7))
                kt = blk * 4 + tt_
                c.copy(vs_tm[:, kt, :], ps[:, 0:128], eng="act")
                c.copy(vw_tm[:, kt, :], ps[:, 128:256])
        hn = hB[0]
        for blk in range(NQ // TB if lim >= 2 else 0):
            bs = slice(blk * TB, (blk + 1) * TB)
            c.dma(x.v, xqT[:, bs].rearrange("(k p) t -> p k t", p=128), q="sp")
            rmsnorm_fm(c, P, K, x.v, gm.v, hn, 8, TB, sq.v, rstd.v)
            for j in range(4):
                ps = P.psum()
                for k in range(8):
                    c.mm(ps.v, wq[:, k, j * 128:(j + 1) * 128], hn[:, k, :], start=(k == 0), stop=(k == 7))
                c.copy(tmpf.v, ps.v, eng="act")
                c.act(tmpb.v, ps.v, AF.Square)
                ps2 = P.psum()
                c.mm(ps2.v, K["ones_mean128"].v, tmpb.v)
                c.act(rstd.v, ps2.v, AF.Ln, bias=K["eps"].v)
                c.act(rstd.v, rstd.v, AF.Exp, scale=-0.5)
                c.stt(qnT[:, j, bs], tmpf.v, qnorm[:, 0:1], rstd.v, ALU.mult, ALU.mult)
            ps = P.psum()
            for k in range(8):
                c.mm(ps[0:12, :], wg[:, k, :], hn[:, k, :], start=(k == 0), stop=(k == 7))
            c.act(Gs[:, bs], ps[0:12, :], AF.Sigmoid, bias=gateb.v)
        c.barrier()
        st2.close()
        w1 = c.sb_scoped(st, [128, 32, 256], BF16, name="w1")
        hk = c.sb_scoped(st, [128, 2, 512], BF16, name="hk")
        pb = c.sb_scoped(st, [128, 2], F32, name="pb")
        for which in range(2):
            src = kcT if which == 0 else vcT
            if lim < 3 or kv_load is not None:
                break
            w1src = (ckw1_d if which == 0 else cvw1_d).rearrange("(l p) h -> p l h", p=128)
            for l4 in range(4):
                c.dma(w1[:, l4 * 8:(l4 + 1) * 8, :], w1src[:, l4 * 8:(l4 + 1) * 8, :], q="sp")
            srcv = src.v.re("p (n s) -> p n s", s=16)
            for hc in range(2):
                ps = P.psum()
                for l in range(32):
                    c.mm(ps[:, 0:1], w1[:, l, hc * 128:(hc + 1) * 128], posT[:, l:l + 1], start=(l == 0), stop=(l == 31))
                c.copy(pb[:, hc:hc + 1], ps[:, 0:1])
            for hc in range(2):
                ps = P.psum()
                for l in range(32):
                    c.mm(ps.v, w1[:, l, hc * 128:(hc + 1) * 128], srcv[:, l // 16:l // 16 + 512, l % 16],
                         start=(l == 0), stop=(l == 31))
                c.act(hk[:, hc, :], ps.v, AF.Gelu, bias=pb[:, hc:hc + 1])
            if which == 0:
                ps = P.psum()
                for hc in range(2):
                    c.mm(ps.v, ckw2[:, hc, :], hk[:, hc, :], start=(hc == 0), stop=(hc == 1))
                c.copy(tmpf.v, ps.v, eng="act")
                c.act(tmpb.v, ps.v, AF.Square)
                ps2 = P.psum()
                c.mm(ps2.v, K["ones_mean128"].v, tmpb.v)
                c.act(rstd.v, ps2.v, AF.Ln, bias=K["eps"].v)
                c.act(rstd.v, rstd.v, AF.Exp, scale=-0.5)
                c.stt(kcmpT.v, tmpf.v, knorm[:, 0:1], rstd.v, ALU.mult, ALU.mult)
            else:
                for nt in range(4):
                    ps = P.psum()
                    for hc in range(2):
                        c.mm(ps[:, 0:128], hk[:, hc, nt * 128:(nt + 1) * 128], cvw2[:, hc, :], start=(hc == 0), stop=(hc == 1))
                    c.copy(vcmp[:, nt, :], ps[:, 0:128], eng="act")
        kvt = dict(ksnT=ksnT, kwnT=kwnT, vs_tm=vs_tm, vw_tm=vw_tm, kcmpT=kcmpT, vcmp=vcmp)
        if kv_load is not None:
            for i_, (nm, t_) in enumerate(kvt.items()):
                c.dma(t_.v, kv_load[nm], q="sp" if i_ % 2 == 0 else "act")
        if kv_save is not None:
            for i_, (nm, t_) in enumerate(kvt.items()):
                c.dma(kv_save[nm], t_.v, q="sp" if i_ % 2 == 0 else "act")
        dump("kcmpT", kcmpT.v, [128, 512], BF16)
        dump("vcmp", vcmp.v, [128, 4, 128], BF16)
        dump("ksnT", ksnT.v, [128, S], BF16)
        dump("qnT", qnT.v, [128, 4, NQ], BF16)
        dump("Gs", Gs.v, [12, NQ], BF16)
        dump("vw_tm", vw_tm.v, [128, NKT, 128], BF16)
        c.barrier()

    E = ld("E", E_d, [128, S], BF16)
    tqb = ld("tqb", tqb_d, [128, NQ], F32, q="act")
    eC = c.sb([128, 4, 512], BF16, name="eC")
    NR = 4
    eS = [c.sb([128, 512], BF16, name=f"eS{i}") for i in range(NR)]
    pS = [c.sb([128, 512], BF16, name=f"pS{i}") for i in range(NR)]
    mk = [c.sb([128, 128], F32, name=f"mk{i}") for i in range(6)]
    mk2 = [c.sb([128, 128], F32, name=f"mkb{i}") for i in range(6)]
    mkb = [c.sb([128, 128], BF16, name=f"mkc{i}") for i in range(6)]
    imp = c.sb([128, 128], F32, name="imp")
    rec1 = [c.sb([128, 1], F32, name=f"rec1{i}") for i in range(2)]
    nd = c.sb([128, 128], F32, name="nd")
    vv = c.sb([128, 128], F32, name="vv")
    ff = c.sb([128, 128], F32, name="ff")
    sc = c.sb([128, 128], F32, name="sc")
    sc2 = c.sb([128, 128], F32, name="sc2")
    m8a = c.sb([128, 8], F32, name="m8a")
    m8b = c.sb([128, 8], F32, name="m8b")
    selb = c.sb([128, 128], BF16, name="selb")
    selT = c.sb([128, 128], BF16, name="selT")
    rec = c.sb([128, 512], F32, name="rec")
    oacc = c.sb([128, 512], F32, name="oacc")
    otmp = c.sb([128, 512], F32, name="otmp")
    osb = c.sb([128, 512], F32, name="osb")
    obf = [c.sb([128, 512], BF16, name=f"obf{i}") for i in range(2)]
    XT = V(psX, psX.h[:].bitcast(BF16))
    cnt = [0, 0]

    def pipe(n, front, back, depth=2):
        for i in range(min(depth, n)):
            front(i)
        for i in range(n):
            back(i)
            if i + depth < n:
                front(i + depth)

    def finish_branch(k, br, first):
        qs = slice(k * 128, (k + 1) * 128)
        for r in range(4):
            cidx = r * 3 + br
            c.mm(psX[:, r * 128:(r + 1) * 128], sel12[:, cidx * 128:(cidx + 1) * 128], Gs[:, qs])
        c.copy(osb.v, psO.v, eng="act")
        if br == 0 and k == 0:
            c.ts(rec.v, psD.v, 1e-30, ALU.max)
            c.act(rec.v, rec.v, AF.Ln)
        else:
            c.act(rec.v, psD.v, AF.Ln)
        c.act(rec.v, rec.v, AF.Exp, scale=-1.0)
        c.tt(rec.v, rec.v, psX.v, ALU.mult)
        if first:
            c.tt(oacc.v, osb.v, rec.v, ALU.mult)
        else:
            c.tt(otmp.v, osb.v, rec.v, ALU.mult)
            c.tt(oacc.v, oacc.v, otmp.v, ALU.add, eng="pool")

    for k in range(NQT if lim >= 4 else 0):
        if nqt_lim is not None and k >= nqt_lim:
            break
        qs = slice(k * 128, (k + 1) * 128)
        Q = qnT[:, :, qs]
        tqk = tq[:, k:k + 1]

        def cfront(nt):
            ps = P.psum()
            c.mm(ps.v.re("p (r q) -> p r q", r=4), kcmpT[:, nt * 128:(nt + 1) * 128], Q)
            c.act(eC[:, nt, :], ps.v, AF.Exp, scale=scale)
            m = mk[nt % 6]
            c.ts(m.v, tqb[:, qs], cmpend[:, nt:nt + 1], ALU.is_ge)
            c.tt(eC[:, nt, :].re("p (r q) -> p r q", r=4), eC[:, nt, :].re("p (r q) -> p r q", r=4), bcm(m.v, 4), ALU.mult)

        def cback(nt):
            c.mm(psO.v, vcmp[:, nt, :], eC[:, nt, :], start=(nt == 0), stop=(nt == 3))
            c.mm(psD.v, K["ones"].v, eC[:, nt, :], start=(nt == 0), stop=(nt == 3))
        pipe(4, cfront, cback)
        for r in range(4):
            psI = P.psum()
            for nt in range(4):
                c.mm(psI[:, 0:129], eC[:, nt, r * 128:(r + 1) * 128], ov[:, nt, :], start=(nt == 0), stop=(nt == 3))
            r1 = rec1[r % 2]
            c.ts(r1.v, psI[:, 128:129], 1e-30, ALU.max)
            c.recip(r1.v, r1.v)
            if r == 0:
                c.ts(imp.v, psI[:, 0:128], r1[:, 0:1], ALU.mult)
            else:
                c.stt(imp.v, psI[:, 0:128], r1[:, 0:1], imp.v, ALU.mult, ALU.add)
        finish_branch(k, 0, True)
        c.ts(nd.v, j64.v, tqk, ALU.subtract)
        c.ts(vv.v, nd.v, 0.0, ALU.is_le)
        c.ts(ff.v, nd.v, -128.0, ALU.is_gt)
        c.tt(ff.v, ff.v, vv.v, ALU.mult)
        c.tt(ff.v, ff.v, e0.v, ALU.add)
        c.stt(sc.v, ff.v, 100.0, imp.v, ALU.mult, ALU.add)
        c.tt(sc.v, sc.v, vv.v, ALU.mult)
        c.ts(ff.v, vv.v, -1.0, ALU.add)
        c.tt(sc.v, sc.v, ff.v, ALU.add)
        c.max8(m8a.v, sc.v)
        c.match_replace(sc2.v, m8a.v, sc.v, -2.0)
        c.max8(m8b.v, sc2.v)
        c.ts(sc2.v, sc.v, m8b[:, 7:8], ALU.is_ge)
        c.tt(selb.v, sc2.v, vv.v, ALU.mult)
        c.tr(XT[:, 0:128], selb.v, identb.v)
        c.copy(selT.v, XT[:, 0:128], eng="act")
        if dbg and k in (1, 20):
            dump(f"imp{k}", imp.v, [128, 128], F32)
            dump(f"sel{k}", selb.v, [128, 128], BF16)
        wkts = [kt for kt in range(2 * k - 4, 2 * k + 2) if kt >= 0]
        nkt = 2 * k + 2
        items = [("win", kt, ii, len(wkts)) for ii, kt in enumerate(wkts)] + \
                [("slc", kt, kt, nkt) for kt in range(nkt)]
        bufs = {}

        def front(i):
            kind, kt, ii, nn = items[i]
            ks_ = slice(kt * 128, (kt + 1) * 128)
            b3 = cnt[0] % NR
            cnt[0] += 1
            bufs[i] = b3
            ps = P.psum()
            c.mm(ps.v.re("p (r q) -> p r q", r=4), (kwnT if kind == "win" else ksnT)[:, ks_], Q)
            c.act(eS[b3].v, ps.v, AF.Exp, scale=scale)
            m3 = cnt[1] % 6
            cnt[1] += 1
            if kind == "win":
                m, m2, mb = mk[m3], mk2[m3], mkb[m3]
                c.ts(m.v, tqb[:, qs], keypos[:, kt:kt + 1], ALU.subtract)
                c.ts(m2.v, m.v, 0.0, ALU.is_ge)
                c.ts(m.v, m.v, 512.0, ALU.is_lt)
                c.tt(mb.v, m.v, m2.v, ALU.mult)
                msk = mb.v
            else:
                pm = psM[m3 % 2]
                c.mm(pm[:, 0:128], E[:, ks_], selT.v)
                if kt >= 2 * k:
                    m = mk[m3]
                    c.ts(m.v, tqb[:, qs], keypos[:, kt:kt + 1], ALU.is_ge)
                    c.tt(m.v, m.v, pm[:, 0:128], ALU.mult)
                    msk = m.v
                else:
                    msk = pm[:, 0:128]
            c.tt(pS[b3].v.re("p (r q) -> p r q", r=4), eS[b3].v.re("p (r q) -> p r q", r=4), bcm(msk, 4), ALU.mult)

        def back(i):
            kind, kt, ii, nn = items[i]
            b3 = bufs[i]
            vt = vw_tm if kind == "win" else vs_tm
            c.mm(psO.v, vt[:, kt, :], pS[b3].v, start=(ii == 0), stop=(ii == nn - 1))
            c.mm(psD.v, K["ones"].v, pS[b3].v, start=(ii == 0), stop=(ii == nn - 1))
            if ii == nn - 1:
                finish_branch(k, 2 if kind == "win" else 1, False)
        pipe(len(items), front, back, depth=3)
        ob = obf[k % 2]
        c.copy(ob.v, oacc.v, eng="act")
        dst = attT.rearrange("r d q -> d r q")[:, :, qs] if att_dst is None else att_dst(k)
        c.dma(dst, ob.v.re("p (r q) -> p r q", r=4), q="act")
    return c


_CACHE = {}


def _get(name, fn, *a):
    key = (name,) + tuple(a)
    if key not in _CACHE:
        _CACHE[key] = fn(*a)
    return _CACHE[key]


def _vec8(v):
    return np.ascontiguousarray(np.asarray(v, np.float32).reshape(8, 128).T)


def _col(v):
    return np.ascontiguousarray(np.asarray(v, np.float32).reshape(128, 1))


def _bcrow(v):
    v = np.asarray(v, np.float32)
    return np.ascontiguousarray(np.broadcast_to(v[None, :], (128, v.shape[0])))


def _bfc(a):
    return np.asarray(a, dtype=np.float32).astype(NPBF)


CAST_NAMES = ["ev_w_in", "ev_rg_wa", "ev_rg_wx", "ev_cmp_pos", "ev_cmp_k_w1", "ev_cmp_k_w2", "ev_cmp_v_w1",
              "ev_cmp_v_w2", "ev_w_out", "od_w_in", "od_w_out", "x_wq", "x_wkv", "x_wo", "ff_w13", "ff_w2",
              "moe_w13", "moe_w2"]


def cast_weights(inp):
    flats = [np.asarray(inp[n], np.float32).reshape(-1) for n in CAST_NAMES]
    tot = sum(f.size for f in flats)
    per = 8 * 128
    F = (tot + per - 1) // per
    F = ((F + 7) // 8) * 8
    buf = np.zeros(per * F, np.float32)
    o = 0
    offs = {}
    for n, f in zip(CAST_NAMES, flats):
        buf[o:o + f.size] = f
        offs[n] = (o, f.size)
        o += f.size
    buf = buf.reshape(8, 128, F)
    c = _get("cast", build_cast, F)
    res = c.run([{"w32": buf[i]} for i in range(8)])
    out = np.concatenate([np.asarray(res.results[i]["w16"]).reshape(-1) for i in range(8)])
    W = {}
    for n in CAST_NAMES:
        o, sz = offs[n]
        W[n] = out[o:o + sz].reshape(np.asarray(inp[n]).shape)
    return W


def a_inputs(inp, W, b, bp):
    Wi = W["ev_w_in"][0]
    blks = [2 * bp, 2 * bp + 1]
    cols = np.concatenate([np.arange(k * 128, (k + 1) * 128) for k in blks] +
                          [1024 + np.arange(k * 128, (k + 1) * 128) for k in blks])

    def pb(v):
        v = np.asarray(v, np.float32)
        return np.ascontiguousarray(np.stack([v[k * 128:(k + 1) * 128] for k in blks], axis=1))
    cwv = np.asarray(inp["ev_rg_conv_w"][0], np.float32)
    cw = np.stack([cwv[j, k * 128:(k + 1) * 128] for k in blks for j in range(4)], axis=1)
    return dict(gmix=_vec8(inp["norm_mix"][0]), wA=np.ascontiguousarray(Wi[:, cols]),
                cw=np.ascontiguousarray(cw), cb=pb(inp["ev_rg_conv_b"][0]),
                wa=np.ascontiguousarray(np.concatenate([W["ev_rg_wa"][0][k] for k in blks], axis=1)),
                wx=np.ascontiguousarray(np.concatenate([W["ev_rg_wx"][0][k] for k in blks], axis=1)),
                ba=pb(inp["ev_rg_ba"][0]), bx=pb(inp["ev_rg_bx"][0]), lam=pb(inp["ev_rg_lambda"][0]))


def b_consts(S=8192):
    j = np.arange(128)
    n = np.arange(512)
    key = np.arange(S)
    E = (key[None, :] // 64 == j[:, None]).astype(np.float32)
    cs = n * 16
    ce = cs + 31
    ss = j * 64
    ovm = ((cs[:, None] <= ss[None, :] + 63) & (ce[:, None] >= ss[None, :])).astype(np.float32)
    ovm[511] = 0
    ov = np.concatenate([ovm, np.ones((512, 1), np.float32)], axis=1).reshape(4, 128, 129).transpose(1, 0, 2)
    sel12 = np.zeros((12, 12 * 128), np.float32)
    for i in range(12):
        sel12[i, i * 128:(i + 1) * 128] = 1
    keypos = (np.arange(64)[None, :] * 128 + np.arange(128)[:, None]).astype(np.float32)
    cmpend = (16 * (np.arange(4)[None, :] * 128 + np.arange(128)[:, None]) + 31).astype(np.float32)
    cmpend[127, 3] = 1e9
    j64 = np.broadcast_to((64 * j)[None, :], (128, 128)).astype(np.float32)
    e0 = np.zeros((128, 128), np.float32)
    e0[:, 0] = 1
    return dict(identb=_bfc(np.eye(128)), E=_bfc(E), ov=_bfc(np.ascontiguousarray(ov)), sel12=_bfc(sel12),
                keypos=np.ascontiguousarray(keypos), cmpend=np.ascontiguousarray(cmpend),
                j64=np.ascontiguousarray(j64), e0=e0)


def b_tok(qh):
    tiles = np.arange(32) * 2 + qh
    return tiles[:, None] * 128 + np.arange(128)[None, :]


def b_inputs(inp, W, xb, xbT, g, qh, consts):
    Wi = W["ev_w_in"][0]
    qcols = 2048 + np.arange(g * 512, (g + 1) * 512)
    base = 2048 + 1024

    def kvc(i):
        return base + i * 256 + np.arange(g * 128, (g + 1) * 128)
    kvcols = np.concatenate([kvc(0), kvc(1), kvc(2), kvc(4), kvc(3), kvc(5)])
    gcols = base + 6 * 256 + np.arange(g * 12, (g + 1) * 12)
    tok = b_tok(qh)
    m = dict(xT=xbT, xqT=np.ascontiguousarray(xb[tok.reshape(-1)].T),
             tq=np.ascontiguousarray(tok.T).astype(np.float32),
             tqb=np.ascontiguousarray(np.broadcast_to(tok.reshape(1, -1), (128, 4096))).astype(np.float32),
             gmix=_vec8(inp["norm_mix"][0]), wq=np.ascontiguousarray(Wi[:, qcols]),
             wkv=np.ascontiguousarray(Wi[:, kvcols]), wg=np.ascontiguousarray(Wi[:, gcols]),
             gateb=np.ascontiguousarray(np.asarray(inp["ev_nsa_gate_b"][0], np.float32)[g * 12:(g + 1) * 12].reshape(12, 1)),
             qnorm=_col(inp["ev_q_norm"][0]),
             knorm=np.ascontiguousarray(np.asarray(inp["ev_k_norm"][0], np.float32).T),
             posT=np.ascontiguousarray(W["ev_cmp_pos"][0].T), ckw1=W["ev_cmp_k_w1"][0], ckw2=W["ev_cmp_k_w2"][0],
             cvw1=W["ev_cmp_v_w1"][0], cvw2=W["ev_cmp_v_w2"][0])
    m.update(consts)
    return m


def d_consts():
    t_ = np.arange(128)
    return dict(identb=_bfc(np.eye(128)), identf=np.eye(128, dtype=np.float32),
                U=(t_[:, None] <= t_[None, :]).astype(np.float32),
                NM=np.where(t_[None, :] < t_[:, None], -30000.0, 0.0).astype(np.float32),
                onesf=np.ones((128, 128), np.float32))


def d_inputs(inp, W, x1T_b, g, consts):
    Wi = W["od_w_in"][0]
    cols = np.concatenate([np.arange(g * 512, (g + 1) * 512), 2048 + np.arange(g * 512, (g + 1) * 512),
                           4096 + np.arange(g * 128, (g + 1) * 128), 4096 + 512 + np.arange(g * 128, (g + 1) * 128)])
    dtcols = 2048 + 3072 + np.arange(g * 8, (g + 1) * 8)
    cch = [np.arange(g * 512 + k * 128, g * 512 + (k + 1) * 128) for k in range(4)] + \
          [2048 + np.arange(g * 128, (g + 1) * 128), 2048 + 512 + np.arange(g * 128, (g + 1) * 128)]
    cwv = np.asarray(inp["od_conv_w"][0], np.float32)
    cbv = np.asarray(inp["od_conv_b"][0], np.float32)
    cw = np.stack([cwv[j, ch] for ch in cch for j in range(4)], axis=1)
    cb = np.stack([cbv[ch] for ch in cch], axis=1)
    hs = slice(g * 8, (g + 1) * 8)
    gn = np.asarray(inp["od_norm"][0], np.float32)[g * 512:(g + 1) * 512].reshape(4, 128).T
    m = dict(xT=x1T_b, gmix=_vec8(inp["norm_mix"][1]), wD=np.ascontiguousarray(Wi[:, cols]),
             wdt=np.ascontiguousarray(Wi[:, dtcols]), cw=np.ascontiguousarray(cw), cb=np.ascontiguousarray(cb),
             dtb=_bcrow(np.asarray(inp["od_dt_bias"][0])[hs]), alog=_bcrow(np.asarray(inp["od_a_log"][0])[hs]),
             dsk=_bcrow(np.asarray(inp["od_d_skip"][0])[hs]), gn=np.ascontiguousarray(gn),
             wo=np.ascontiguousarray(W["od_w_out"][0][g * 512:(g + 1) * 512]))
    m.update(consts)
    return m


def kernel(**inp):
    NT = 2048
    S = 8192
    x = np.asarray(inp["x"], np.float32)
    mem = np.asarray(inp["mem"], np.float32)
    W = cast_weights(inp)
    xT = [np.ascontiguousarray(x[b].T) for b in range(2)]
    memT = [np.ascontiguousarray(mem[b].T) for b in range(2)]

    ca = _get("A", build_stage_a)
    maps = []
    for core in range(8):
        m = a_inputs(inp, W, core // 4, core % 4)
        m["xT"] = xT[core // 4]
        maps.append(m)
    ra = ca.run(maps).results
    cb_ = _get("B", build_stage_b)
    bc = b_consts()
    maps = [b_inputs(inp, W, x[core // 4], xT[core // 4], (core % 4) // 2, core % 2, bc) for core in range(8)]
    rb = cb_.run(maps).results
    mixT = [np.zeros((2048, S), NPBF) for _ in range(2)]
    for core in range(8):
        b, bp = core // 4, core % 4
        mixT[b][bp * 256:(bp + 1) * 256] = np.asarray(ra[core]["rgT"])
        g, qh = (core % 4) // 2, core % 2
        tok = b_tok(qh).reshape(-1)
        o = np.asarray(rb[core]["attT"])
        for r in range(4):
            h = g * 4 + r
            mixT[b][1024 + h * 128:1024 + (h + 1) * 128, tok] = o[r]

    cc = _get("C", build_stage_c, NT)
    maps = []
    for core in range(8):
        b, q = core // 4, core % 4
        sl = slice(q * NT, (q + 1) * NT)
        maps.append(dict(xT=np.ascontiguousarray(xT[b][:, sl]), mixT=np.ascontiguousarray(mixT[b][:, sl]),
                         memT=memT[b], w_out=W["ev_w_out"][0], wq=W["x_wq"][0], wkv=W["x_wkv"][0], wo=W["x_wo"][0],
                         w13=W["ff_w13"][0], w2=W["ff_w2"][0], norm_cross=_vec8(inp["norm_cross"][0]),
                         norm_mem=_vec8(inp["norm_mem"][0]), norm_ffn=_vec8(inp["norm_ffn"][0]),
                         qn=_col(inp["x_q_norm"][0]), kn=_col(inp["x_k_norm"][0])))
    rc = cc.run(maps).results
    x1T = [np.concatenate([np.asarray(rc[b * 4 + q]["outT"]) for q in range(4)], axis=1) for b in range(2)]

    cd = _get("D", build_stage_d)
    dc = d_consts()
    maps = [d_inputs(inp, W, x1T[core // 4], core % 4, dc) for core in range(8)]
    rd = cd.run(maps).results

    ce = _get("E", build_stage_e, NT)
    selm = np.zeros((8, 8 * 128), np.float32)
    for e in range(8):
        selm[e, e * 128:(e + 1) * 128] = 1
    maps = []
    for core in range(8):
        b, q = core // 4, core % 4
        sl = slice(q * NT, (q + 1) * NT)
        parts = np.stack([np.ascontiguousarray(np.asarray(rd[b * 4 + g]["outT"])[:, sl]) for g in range(4)])
        maps.append(dict(xT=np.ascontiguousarray(x1T[b][:, sl]), pT=parts, memT=memT[b],
                         wq=W["x_wq"][1], wkv=W["x_wkv"][1], wo=W["x_wo"][1],
                         norm_cross=_vec8(inp["norm_cross"][1]), norm_mem=_vec8(inp["norm_mem"][1]),
                         norm_ffn=_vec8(inp["norm_ffn"][1]), qn=_col(inp["x_q_norm"][1]), kn=_col(inp["x_k_norm"][1]),
                         router=np.ascontiguousarray(np.asarray(inp["moe_router"][0], np.float32)),
                         w13=W["moe_w13"][0], w2=W["moe_w2"][0], identf=np.eye(128, dtype=np.float32), sel=selm))
    re_ = ce.run(maps).results
    out = np.zeros((2, S, 1024), np.float32)
    for core in range(8):
        b, q = core // 4, core % 4
        out[b, q * NT:(q + 1) * NT] = np.asarray(re_[core]["outT"]).T
    return out


BF_NAMES = {
    "A": {"wA", "wa", "wx"},
    "B": {"wq", "wkv", "wg", "posT", "ckw1", "ckw2", "cvw1", "cvw2", "identb", "E", "ov", "sel12"},
    "C": {"w_out", "wq", "wkv", "wo", "w13", "w2"},
    "D": {"wD", "wdt", "wo", "identb"},
    "E": {"wq", "wkv", "wo", "w13", "w2"},
}
BIG_IN = {"xT", "xqT", "memT", "mixT", "pT"}


class Packer:
    def __init__(self):
        self.items = {}
        self.n = 0
        self.arrs = []

    def add(self, key, arr):
        arr = np.asarray(arr)
        if arr.dtype != np.float32:
            arr = arr.astype(np.float32)
        sz = arr.size
        self.items[key] = (self.n, tuple(arr.shape))
        self.arrs.append((self.n, arr.reshape(-1)))
        self.n += ((sz + 63) // 64) * 64

    def build(self, mult):
        tot = ((self.n + mult - 1) // mult) * mult
        buf = np.zeros(tot, np.float32)
        for o, a in self.arrs:
            buf[o:o + a.size] = a
        return buf


def _role_maps(inp):
    Wf = {n: np.asarray(inp[n], np.float32) for n in CAST_NAMES}
    roles = {}
    for bp in range(4):
        roles[f"A{bp}"] = ("A", a_inputs(inp, Wf, 0, bp))
    bc = b_consts()
    dummy_x = np.zeros((8192, 8), np.float32)
    for g in range(2):
        m = b_inputs(inp, Wf, dummy_x, None, g, 0, bc)
        for k in ("xT", "xqT", "tq", "tqb"):
            m.pop(k)
        roles[f"B{g}"] = ("B", m)
    for qh in range(2):
        tok = b_tok(qh)
        roles[f"Bq{qh}"] = ("B", dict(tq=np.ascontiguousarray(tok.T).astype(np.float32),
                                     tqb=np.ascontiguousarray(np.broadcast_to(tok.reshape(1, -1), (128, 4096))).astype(np.float32)))
    roles["C"] = ("C", dict(w_out=Wf["ev_w_out"][0], wq=Wf["x_wq"][0], wkv=Wf["x_wkv"][0], wo=Wf["x_wo"][0],
                           w13=Wf["ff_w13"][0], w2=Wf["ff_w2"][0], norm_cross=_vec8(inp["norm_cross"][0]),
                           norm_mem=_vec8(inp["norm_mem"][0]), norm_ffn=_vec8(inp["norm_ffn"][0]),
                           qn=_col(inp["x_q_norm"][0]), kn=_col(inp["x_k_norm"][0])))
    dc = d_consts()
    for g in range(4):
        m = d_inputs(inp, Wf, None, g, dc)
        m.pop("xT")
        roles[f"D{g}"] = ("D", m)
    selm = np.zeros((8, 8 * 128), np.float32)
    for e in range(8):
        selm[e, e * 128:(e + 1) * 128] = 1
    roles["E"] = ("E", dict(wq=Wf["x_wq"][1], wkv=Wf["x_wkv"][1], wo=Wf["x_wo"][1],
                           norm_cross=_vec8(inp["norm_cross"][1]), norm_mem=_vec8(inp["norm_mem"][1]),
                           norm_ffn=_vec8(inp["norm_ffn"][1]), qn=_col(inp["x_q_norm"][1]), kn=_col(inp["x_k_norm"][1]),
                           router=np.ascontiguousarray(np.asarray(inp["moe_router"][0], np.float32)),
                           w13=Wf["moe_w13"][0], w2=Wf["moe_w2"][0], identf=np.eye(128, dtype=np.float32), sel=selm))
    return roles


CAST_CH = 8192


CAST_CH2 = 2048


def pack_roles(roles):
    pw, pe, pp = Packer(), Packer(), Packer()
    for rk, (st, m) in roles.items():
        for name, arr in m.items():
            if name in BF_NAMES[st]:
                (pe if rk == "E" else pw).add(f"{rk}.{name}", arr)
            else:
                pp.add(f"{rk}.{name}", arr)
    w32 = pw.build(128 * CAST_CH)
    w32e = pe.build(128 * CAST_CH2)
    p32 = pp.build(64)
    return w32, w32e, p32, pw.items, pe.items, pp.items


def _view(flat, off, shape):
    n = int(np.prod(shape))
    v = flat[off:off + n]
    if len(shape) == 1:
        return v
    if len(shape) == 2:
        return v.rearrange("(a b) -> a b", b=shape[1])
    if len(shape) == 3:
        return v.rearrange("(a b c) -> a b c", b=shape[1], c=shape[2])
    raise ValueError(shape)


def build_fused(NW, NWE, NP, witems, eitems, pitems, S=8192, stages="ABCDE"):
    c = Ctx()
    nc = c.nc
    w32 = c.din("w32", [NW], F32)
    w32e = c.din("w32e", [NWE], F32)
    p32 = c.din("p32", [NP], F32)
    xT = c.din("xT", [1024, S], F32)
    xqT = c.din("xqT", [2, 1024, 4096], F32)
    memT = c.din("memT", [1024, 256], F32)
    outT = c.dout("outT", [1024, 2048], F32)
    w16 = nc.dram_tensor("w16_i", [NW], BF16, kind="Internal").ap()
    w16e = nc.dram_tensor("w16e_i", [NWE], BF16, kind="Internal").ap()
    mixT_i = nc.dram_tensor("mixT_i", [2048, S], BF16, kind="Internal").ap()
    x1T_i = nc.dram_tensor("x1T_i", [1024, S], F32, kind="Internal").ap()
    part_i = nc.dram_tensor("part_i", [4, 1024, 2048], F32, kind="Internal").ap()
    st_i = nc.dram_tensor("st_i", [4, 4, 128, 530], F32, kind="Internal").ap()
    qidx = nc.partition_id() % 4

    def role_io(rk, st):
        io = {}
        for key, (off, shape) in witems.items():
            r, name = key.split(".")
            if r == rk:
                io[name] = _view(w16, off, shape)
        for key, (off, shape) in eitems.items():
            r, name = key.split(".")
            if r == rk:
                io[name] = _view(w16e, off, shape)
        for key, (off, shape) in pitems.items():
            r, name = key.split(".")
            if r == rk:
                io[name] = _view(p32, off, shape)
        return io

    F = NW // 128
    with c.scope():
        w32v = w32.rearrange("(p f) -> p f", p=128)
        w16v = w16.rearrange("(p f) -> p f", p=128)
        a = [c.sb([128, CAST_CH], F32, name=f"ca{i}") for i in range(3)]
        b = [c.sb([128, CAST_CH], BF16, name=f"cb{i}") for i in range(3)]
        engs = ["dve", "pool", "act"]
        for i, p in enumerate(range(0, F, CAST_CH)):
            ta, tb_ = a[i % 3], b[i % 3]
            c.dma(ta.v, w32v[:, p:p + CAST_CH], q="sp")
            c.copy(tb_.v, ta.v, eng=engs[i % 3])
            c.dma(w16v[:, p:p + CAST_CH], tb_.v, q="sp")
    if "A" in stages:
        for bp in range(4):
            with c.scope():
                io = role_io(f"A{bp}", "A")
                io["xT"] = xT
                io["rgT"] = mixT_i[bp * 256:(bp + 1) * 256, :]
                build_stage_a(c0=c, io=io)
    if "B" in stages:
        kvshapes = dict(ksnT=[128, S], kwnT=[128, S], vs_tm=[128, S // 128, 128], vw_tm=[128, S // 128, 128],
                        kcmpT=[128, 512], vcmp=[128, 4, 128])
        kv_i = {nm: nc.dram_tensor("kv_" + nm, sh, BF16, kind="Internal").ap() for nm, sh in kvshapes.items()}
        for g in range(2):
            for qh in range(2):
                with c.scope():
                    io = role_io(f"B{g}", "B")
                    io.update(role_io(f"Bq{qh}", "B"))
                    io["xT"] = xT
                    io["xqT"] = xqT[qh]

                    def att_dst(k, g=g, qh=qh):
                        gt = 2 * k + qh
                        return mixT_i[1024 + g * 512:1024 + (g + 1) * 512, gt * 128:(gt + 1) * 128].rearrange(
                            "(r d) q -> d r q", d=128)
                    build_stage_b(c0=c, io=io, att_dst=att_dst, kv_save=kv_i if qh == 0 else None,
                                  kv_load=kv_i if qh == 1 else None)
    if "C" in stages:
        for qq in range(4):
            with c.scope():
                io = role_io("C", "C")
                sl = slice(qq * 2048, (qq + 1) * 2048)
                io["xT"] = xT[:, sl]
                io["mixT"] = mixT_i[:, sl]
                io["memT"] = memT
                io["outT"] = x1T_i[:, sl]
                build_stage_c(c0=c, io=io)
    if "D" in stages:
        with c.scope():
            FE_ = NWE // 128
            srcv = w32e.rearrange("(p f) -> p f", p=128)
            dstv = w16e.rearrange("(p f) -> p f", p=128)
            NB_ = 3
            ca = [c.sb([128, CAST_CH2], F32, name=f"cea{i}") for i in range(NB_)]
            cb_ = [c.sb([128, CAST_CH2], BF16, name=f"ceb{i}") for i in range(NB_)]
            nsteps = FE_ // CAST_CH2

            def cast_gen():
                def load(j):
                    if j < nsteps:
                        c.dma(ca[j % NB_].v, srcv[:, j * CAST_CH2:(j + 1) * CAST_CH2], q="sp")

                def store(j):
                    if 0 <= j < nsteps:
                        c.dma(dstv[:, j * CAST_CH2:(j + 1) * CAST_CH2], cb_[j % NB_].v, q="sp")
                load(0)
                load(1)
                for j in range(nsteps):
                    store(j - 1)
                    load(j + 2)
                    c.copy(cb_[j % NB_].v, ca[j % NB_].v, eng="pool")
                    yield
                store(nsteps - 1)
            gen = cast_gen()

            def hook():
                for _ in range(2):
                    next(gen, None)
            for g in range(4):
                with c.scope():
                    io = role_io(f"D{g}", "D")
                    io["xT"] = x1T_i
                    build_stage_d(c0=c, io=io, mode="state", st_d=st_i[g], hook=hook)
                with c.scope():
                    io = role_io(f"D{g}", "D")
                    io["xT"] = x1T_i
                    io["outT"] = part_i[g]
                    build_stage_d(c0=c, io=io, mode="own", st_d=st_i[g], tok0=qidx * 2048, qidx=qidx, hook=hook)
            for _ in gen:
                pass
    if "E" in stages:
        with c.scope():
            io = role_io("E", "E")
            io["xT"] = x1T_i
            io["pT"] = part_i
            io["memT"] = memT
            io["outT"] = outT
            build_stage_e(c0=c, io=io, tok0=qidx * 2048)
    return c


def kernel_fused(**inp):
    S = 8192
    x = np.asarray(inp["x"], np.float32)
    mem = np.asarray(inp["mem"], np.float32)
    roles = _role_maps(inp)
    w32, w32e, p32, witems, eitems, pitems = pack_roles(roles)
    key = ("F", w32.size, w32e.size, p32.size, tuple(sorted(witems.items())), tuple(sorted(eitems.items())),
           tuple(sorted(pitems.items())))
    if key not in _CACHE:
        _CACHE[key] = build_fused(w32.size, w32e.size, p32.size, witems, eitems, pitems)
    c = _CACHE[key]
    maps = []
    for b in range(2):
        xTb = np.ascontiguousarray(x[b].T)
        xq = np.stack([np.ascontiguousarray(x[b][b_tok(qh).reshape(-1)].T) for qh in range(2)])
        memTb = np.ascontiguousarray(mem[b].T)
        for q in range(4):
            maps.append(dict(w32=w32, w32e=w32e, p32=p32, xT=xTb, xqT=xq, memT=memTb))
    res = c.run(maps).results
    out = np.zeros((2, S, 1024), np.float32)
    for core in range(8):
        b, q = core // 4, core % 4
        out[b, q * 2048:(q + 1) * 2048] = np.asarray(res[core]["outT"]).T
    return out


kernel_unfused = kernel
kernel = kernel_fused
```
